# Optimizing a Trainium2 kernel written in Bass

```python
import jax, jax.numpy as jnp
from jax import lax
import numpy as np

D_MODEL = 1024
BATCH = 8
SEQ = 2048
DEPTH = 1

MEM_LEN = 256

GDN_DK = 128
GDN_DV = 128
GDN_HEADS = D_MODEL // GDN_DV
GDN_CONV = 4
GDN_CHUNK = 64

NSA_DH = 128
NSA_HEADS = D_MODEL // NSA_DH
NSA_KV_HEADS = 2
CMP_BLOCK = 32
CMP_STRIDE = 16
CMP_HIDDEN = 256
SEL_BLOCK = 64
SEL_TOPK = 8
WINDOW = 256
NSA_QBLOCK = 64
ROPE_THETA = 10000.0

XA_HEADS = 4
XA_DH = D_MODEL // XA_HEADS

N_GROUPS = 4
EXPERTS_PER_GROUP = 8
N_EXPERTS = N_GROUPS * EXPERTS_PER_GROUP
TOPK_IN_GROUP = 2
D_FF_EXPERT = D_MODEL // 4

DN_ALPHA = (2.0 * DEPTH) ** 0.25
DN_BETA = (8.0 * DEPTH) ** -0.25
LN_EPS = 1e-5
RMS_EPS = 1e-6

IN_SPLITS = (
    GDN_HEADS * GDN_DK,
    GDN_HEADS * GDN_DK,
    GDN_HEADS * GDN_DV,
    GDN_HEADS * GDN_DV,
    GDN_HEADS,
    GDN_HEADS,
    NSA_HEADS * NSA_DH,
    NSA_KV_HEADS * NSA_DH,
    NSA_KV_HEADS * NSA_DH,
    NSA_KV_HEADS * NSA_DH,
    NSA_KV_HEADS * NSA_DH,
    NSA_KV_HEADS * NSA_DH,
    NSA_KV_HEADS * NSA_DH,
    NSA_HEADS * 3,
    2 * D_MODEL,
)
IN_WIDTH = sum(IN_SPLITS)

kernel_name = "hybrid_gdn_nsa_memxattn_hmoe_deepnorm"


def layer_norm(x, g, b):
    xf = x.astype(jnp.float32)
    mu = jnp.mean(xf, -1, keepdims=True)
    var = jnp.mean(jnp.square(xf - mu), -1, keepdims=True)
    return ((xf - mu) * lax.rsqrt(var + LN_EPS) * g + b).astype(x.dtype)


def rms_norm(x, g):
    xf = x.astype(jnp.float32)
    return xf * lax.rsqrt(jnp.mean(jnp.square(xf), -1, keepdims=True) + RMS_EPS) * g


def l2_normalize(x):
    return x * lax.rsqrt(jnp.sum(jnp.square(x), -1, keepdims=True) + RMS_EPS)


def rope_tables(positions, dim):
    half = dim // 2
    inv_freq = ROPE_THETA ** (-jnp.arange(half, dtype=jnp.float32) / half)
    ang = positions.astype(jnp.float32)[..., None] * inv_freq
    return jnp.cos(ang)[:, :, None, :], jnp.sin(ang)[:, :, None, :]


def apply_rope(x, cos, sin):
    x1, x2 = jnp.split(x.astype(jnp.float32), 2, axis=-1)
    return jnp.concatenate([x1 * cos - x2 * sin, x2 * cos + x1 * sin], -1).astype(x.dtype)


def causal_dwconv(x, w):
    K, C = w.shape
    return lax.conv_general_dilated(x, w[:, None, :].astype(x.dtype), window_strides=(1,),
                                    padding=[(K - 1, 0)], dimension_numbers=('NWC', 'WIO', 'NWC'),
                                    feature_group_count=C)


def masked_softmax(s, mask):
    s = jnp.where(mask, s.astype(jnp.float32), -jnp.inf)
    m = jnp.max(s, axis=-1, keepdims=True)
    m = jnp.where(jnp.isfinite(m), m, 0.0)
    e = jnp.exp(s - m)
    return e / jnp.maximum(jnp.sum(e, axis=-1, keepdims=True), 1e-30)


def gated_deltanet(q, k, v, a_logit, b_logit, z, a_log, dt_bias, norm_w):
    B, S, H, dk = q.shape
    dv = v.shape[-1]
    C = GDN_CHUNK
    N = S // C
    f32 = jnp.float32
    q = l2_normalize(q.astype(f32)) * (dk ** -0.5)
    k = l2_normalize(k.astype(f32))
    v = v.astype(f32)
    beta = jax.nn.sigmoid(b_logit.astype(f32))
    g = -jnp.exp(a_log.astype(f32)) * jax.nn.softplus(a_logit.astype(f32) + dt_bias.astype(f32))

    def chunks(t):
        return t.reshape(B, N, C, H, -1).transpose(0, 3, 1, 2, 4)

    q, k, v = chunks(q), chunks(k), chunks(v)
    beta = beta.reshape(B, N, C, H).transpose(0, 3, 1, 2)
    gc = jnp.cumsum(g.reshape(B, N, C, H).transpose(0, 3, 1, 2), axis=-1)
    diff = gc[..., :, None] - gc[..., None, :]
    causal = jnp.tril(jnp.ones((C, C), dtype=bool))
    strict = jnp.tril(jnp.ones((C, C), dtype=bool), -1)
    decay = jnp.exp(jnp.where(causal, diff, -jnp.inf))
    a_mat = jnp.where(strict, jnp.einsum('bhncd,bhnmd->bhncm', k, k) * decay, 0.0) * beta[..., None]
    lhs = a_mat + jnp.eye(C, dtype=f32)
    rhs = jnp.concatenate([v * beta[..., None], k * (beta * jnp.exp(gc))[..., None]], -1)
    sol = lax.linalg.triangular_solve(lhs, rhs, left_side=True, lower=True, unit_diagonal=True)
    u, w = sol[..., :dv], sol[..., dv:]
    qk = jnp.einsum('bhncd,bhnmd->bhncm', q, k) * decay
    q_dec = q * jnp.exp(gc)[..., None]
    g_last = gc[..., -1]
    k_dec = k * jnp.exp(g_last[..., None] - gc)[..., None]

    def step(state, inp):
        u_c, w_c, qk_c, qd_c, kd_c, gl_c = inp
        v_new = u_c - jnp.einsum('bhcd,bhde->bhce', w_c, state)
        o_c = jnp.einsum('bhcd,bhde->bhce', qd_c, state) + jnp.einsum('bhcm,bhme->bhce', qk_c, v_new)
        state = state * jnp.exp(gl_c)[..., None, None] + jnp.einsum('bhcd,bhce->bhde', kd_c, v_new)
        return state, o_c

    xs = tuple(jnp.moveaxis(t, 2, 0) for t in (u, w, qk, q_dec, k_dec, g_last))
    _, o = lax.scan(step, jnp.zeros((B, H, dk, dv), f32), xs)
    o = o.transpose(1, 0, 3, 2, 4).reshape(B, S, H, dv)
    o = rms_norm(o, norm_w) * jax.nn.silu(z.astype(f32))
    return o.reshape(B, S, H * dv)


def compress_kv(k, v, pe, w1, b1, w2):
    B, S, G, dh = k.shape
    nc = (S - CMP_BLOCK) // CMP_STRIDE + 1
    idx = jnp.arange(nc)[:, None] * CMP_STRIDE + jnp.arange(CMP_BLOCK)[None, :]

    def one(t, j):
        blk = t[:, idx] + pe[j][None, None, :, None, :]
        blk = blk.transpose(0, 1, 3, 2, 4).reshape(B, nc, G, CMP_BLOCK * dh)
        return jax.nn.gelu(blk @ w1[j] + b1[j]) @ w2[j]

    return one(k, 0), one(v, 1)


def block_overlap(nc, nb):
    c0 = jnp.arange(nc) * CMP_STRIDE
    s0 = jnp.arange(nb) * SEL_BLOCK
    ov = jnp.minimum(c0[:, None] + CMP_BLOCK, s0[None, :] + SEL_BLOCK) - jnp.maximum(c0[:, None], s0[None, :])
    return (jnp.maximum(ov, 0) / CMP_BLOCK).astype(jnp.float32)


def nsa_attention(q, kc, vc, ks, vs, kw, vw, gates):
    f32 = jnp.float32
    B, S, H, dh = q.shape
    G = kc.shape[2]
    hpg = H // G
    nc = kc.shape[1]
    nb = S // SEL_BLOCK
    n_sel = min(SEL_TOPK, nb)
    QB = NSA_QBLOCK
    q = q.astype(f32) * (dh ** -0.5)
    kc, vc, gates = kc.astype(f32), vc.astype(f32), gates.astype(f32)
    cmp_end = jnp.arange(nc) * CMP_STRIDE + CMP_BLOCK - 1
    overlap = block_overlap(nc, nb)
    ks_blk = ks.astype(f32).reshape(B, nb, SEL_BLOCK, G, dh).transpose(0, 3, 1, 2, 4)
    vs_blk = vs.astype(f32).reshape(B, nb, SEL_BLOCK, G, dh).transpose(0, 3, 1, 2, 4)
    kw_pad = jnp.pad(kw.astype(f32), ((0, 0), (WINDOW, 0), (0, 0), (0, 0)))
    vw_pad = jnp.pad(vw.astype(f32), ((0, 0), (WINDOW, 0), (0, 0), (0, 0)))
    blk_ids = jnp.arange(nb)
    offs = jnp.arange(SEL_BLOCK)
    gather = jax.vmap(jax.vmap(lambda blocks, ids: blocks[ids]))

    def one_block(i):
        t0 = i * QB
        t = t0 + jnp.arange(QB)
        qb = lax.dynamic_slice_in_dim(q, t0, QB, axis=1).reshape(B, QB, G, hpg, dh)
        gb = lax.dynamic_slice_in_dim(gates, t0, QB, axis=1).reshape(B, QB, G, hpg, 3)
        p_c = masked_softmax(jnp.einsum('bqghd,bngd->bghqn', qb, kc), cmp_end[None, :] <= t[:, None])
        o_c = jnp.einsum('bghqn,bngd->bqghd', p_c, vc)
        imp = jnp.einsum('bghqn,nj->bgqj', p_c, overlap)
        cur = t // SEL_BLOCK
        valid = blk_ids[None, :] * SEL_BLOCK <= t[:, None]
        forced = (blk_ids[None, :] == 0) | (blk_ids[None, :] == cur[:, None]) | (blk_ids[None, :] == cur[:, None] - 1)
        imp = jnp.where(forced, jnp.inf, jnp.where(valid, imp, -jnp.inf))
        _, sel = lax.top_k(imp, n_sel)
        flat = sel.reshape(B, G, QB * n_sel)
        k_sel = gather(ks_blk, flat).reshape(B, G, QB, n_sel * SEL_BLOCK, dh)
        v_sel = gather(vs_blk, flat).reshape(B, G, QB, n_sel * SEL_BLOCK, dh)
        kpos = (sel[..., None] * SEL_BLOCK + offs).reshape(B, G, QB, n_sel * SEL_BLOCK)
        p_s = masked_softmax(jnp.einsum('bqghd,bgqkd->bghqk', qb, k_sel), (kpos <= t[:, None])[:, :, None])
        o_s = jnp.einsum('bghqk,bgqkd->bqghd', p_s, v_sel)
        k_w = lax.dynamic_slice_in_dim(kw_pad, t0, QB + WINDOW, axis=1)
        v_w = lax.dynamic_slice_in_dim(vw_pad, t0, QB + WINDOW, axis=1)
        wpos = t0 - WINDOW + jnp.arange(QB + WINDOW)
        rel = t[:, None] - wpos[None, :]
        wmask = (rel >= 0) & (rel < WINDOW) & (wpos[None, :] >= 0)
        p_w = masked_softmax(jnp.einsum('bqghd,bkgd->bghqk', qb, k_w), wmask)
        o_w = jnp.einsum('bghqk,bkgd->bqghd', p_w, v_w)
        o = gb[..., 0:1] * o_c + gb[..., 1:2] * o_s + gb[..., 2:3] * o_w
        return o.reshape(B, QB, H * dh)

    out = lax.map(one_block, jnp.arange(S // QB))
    return out.transpose(1, 0, 2, 3).reshape(B, S, H * dh)


def token_mixer(h, cos, sin, w_in, conv_w, a_log, dt_bias, norm_w, cmp_pe, cmp_w1, cmp_b1, cmp_w2, w_out):
    B, S, _ = h.shape
    proj = h @ w_in
    points = np.cumsum(IN_SPLITS)[:-1].tolist()
    (g_q, g_k, g_v, g_z, g_b, g_a, n_q, c_k, c_v, s_k, s_v, w_k, w_v, n_g, m_g) = jnp.split(proj, points, axis=-1)
    qkv = jax.nn.silu(causal_dwconv(jnp.concatenate([g_q, g_k, g_v], -1), conv_w))
    qkw = GDN_HEADS * GDN_DK
    y_a = gated_deltanet(qkv[..., :qkw].reshape(B, S, GDN_HEADS, GDN_DK),
                         qkv[..., qkw:2 * qkw].reshape(B, S, GDN_HEADS, GDN_DK),
                         qkv[..., 2 * qkw:].reshape(B, S, GDN_HEADS, GDN_DV),
                         g_a, g_b, g_z.reshape(B, S, GDN_HEADS, GDN_DV), a_log, dt_bias, norm_w)
    def kvh(t):
        return t.reshape(B, S, NSA_KV_HEADS, NSA_DH)
    q = apply_rope(n_q.reshape(B, S, NSA_HEADS, NSA_DH), cos, sin)
    k_cmp, v_cmp = compress_kv(apply_rope(kvh(c_k), cos, sin), kvh(c_v), cmp_pe, cmp_w1, cmp_b1, cmp_w2)
    y_b = nsa_attention(q, k_cmp, v_cmp, apply_rope(kvh(s_k), cos, sin), kvh(s_v),
                        apply_rope(kvh(w_k), cos, sin), kvh(w_v),
                        jax.nn.sigmoid(n_g.reshape(B, S, NSA_HEADS, 3)))
    gate = jax.nn.sigmoid(m_g)
    merged = gate[..., :D_MODEL] * y_a.astype(h.dtype) + gate[..., D_MODEL:] * y_b.astype(h.dtype)
    return merged @ w_out


def memory_xattn(h, mem, wq, wkv, wo):
    B, S, _ = h.shape
    M = mem.shape[1]
    q = (h @ wq).reshape(B, S, XA_HEADS, XA_DH)
    kv = mem @ wkv
    k = kv[..., :D_MODEL].reshape(B, M, XA_HEADS, XA_DH)
    v = kv[..., D_MODEL:].reshape(B, M, XA_HEADS, XA_DH)
    s = jnp.einsum('bshd,bmhd->bhsm', q, k).astype(jnp.float32) * (XA_DH ** -0.5)
    p = jax.nn.softmax(s, axis=-1).astype(v.dtype)
    o = jnp.einsum('bhsm,bmhd->bshd', p, v).reshape(B, S, XA_HEADS * XA_DH)
    return o @ wo


def hier_moe(h, w_group, b_group, w_expert, b_expert, w_gate, w_up, w_down):
    B, S, D = h.shape
    xf = h.reshape(B * S, D)
    n = xf.shape[0]
    p_group = jax.nn.softmax((xf @ w_group).astype(jnp.float32) + b_group, axis=-1)
    p_top, g_idx = lax.top_k(p_group, 1)
    g_onehot = jax.nn.one_hot(g_idx[:, 0], N_GROUPS, dtype=jnp.float32)
    e_logits = ((xf @ w_expert).astype(jnp.float32) + b_expert).reshape(n, N_GROUPS, EXPERTS_PER_GROUP)
    e_logits = jnp.einsum('ng,nge->ne', g_onehot, e_logits)
    e_top, e_idx = lax.top_k(jax.nn.softmax(e_logits, axis=-1), TOPK_IN_GROUP)
    wts = p_top * e_top / jnp.sum(e_top, -1, keepdims=True)
    expert_id = g_idx * EXPERTS_PER_GROUP + e_idx
    combine = jnp.einsum('nk,nke->ne', wts, jax.nn.one_hot(expert_id, N_EXPERTS, dtype=jnp.float32))
    out = jnp.zeros((n, D), jnp.float32)
    for grp in range(N_GROUPS):
        sl = slice(grp * EXPERTS_PER_GROUP, (grp + 1) * EXPERTS_PER_GROUP)
        hg = jax.nn.silu(jnp.einsum('nd,edf->nef', xf, w_gate[sl])) * jnp.einsum('nd,edf->nef', xf, w_up[sl])
        out = out + jnp.einsum('nef,efd->nd', hg * combine[:, sl, None].astype(hg.dtype), w_down[sl])
    return out.astype(h.dtype).reshape(B, S, D)


def setup_inputs(seed: int = 0) -> dict:
    key = jax.random.key(seed)
    ks = jax.random.split(key, 32)
    f32 = jnp.float32
    L = DEPTH

    def nrm(i, shape, scale):
        return jax.random.normal(ks[i], shape, f32) * scale

    x = nrm(0, (BATCH, SEQ, D_MODEL), 1.0)
    mem = nrm(1, (BATCH, MEM_LEN, D_MODEL), 1.0)
    positions = (jnp.arange(SEQ, dtype=jnp.int32)[None, :]
                 + jax.random.randint(ks[2], (BATCH, 1), 0, 1024, dtype=jnp.int32))
    w_in = nrm(3, (L, D_MODEL, IN_WIDTH), D_MODEL ** -0.5)
    gdn_conv_w = nrm(4, (L, GDN_CONV, 2 * GDN_HEADS * GDN_DK + GDN_HEADS * GDN_DV), GDN_CONV ** -0.5)
    gdn_a_log = jnp.log(jax.random.uniform(ks[5], (L, GDN_HEADS), f32, 1.0, 16.0))
    dt = jnp.exp(jax.random.uniform(ks[6], (L, GDN_HEADS), f32, jnp.log(1e-3), jnp.log(1e-1)))
    gdn_dt_bias = dt + jnp.log(-jnp.expm1(-dt))
    gdn_norm_w = 1.0 + nrm(7, (L, GDN_DV), 0.02)
    cmp_pe = nrm(8, (L, 2, CMP_BLOCK, NSA_DH), 0.02)
    cmp_w1 = nrm(9, (L, 2, CMP_BLOCK * NSA_DH, CMP_HIDDEN), (CMP_BLOCK * NSA_DH) ** -0.5)
    cmp_b1 = nrm(10, (L, 2, CMP_HIDDEN), 0.02)
    cmp_w2 = nrm(11, (L, 2, CMP_HIDDEN, NSA_DH), CMP_HIDDEN ** -0.5)
    w_out = nrm(12, (L, D_MODEL, D_MODEL), D_MODEL ** -0.5 * DN_BETA)
    ln1_g = 1.0 + nrm(13, (L, D_MODEL), 0.02)
    ln1_b = nrm(14, (L, D_MODEL), 0.02)
    xa_wq = nrm(15, (L, D_MODEL, XA_HEADS * XA_DH), D_MODEL ** -0.5)
    xa_wkv = nrm(16, (L, D_MODEL, 2 * XA_HEADS * XA_DH), D_MODEL ** -0.5)
    xa_wo = nrm(17, (L, XA_HEADS * XA_DH, D_MODEL), D_MODEL ** -0.5 * DN_BETA)
    ln2_g = 1.0 + nrm(18, (L, D_MODEL), 0.02)
    ln2_b = nrm(19, (L, D_MODEL), 0.02)
    moe_w_group = nrm(20, (L, D_MODEL, N_GROUPS), D_MODEL ** -0.5)
    moe_b_group = nrm(21, (L, N_GROUPS), 0.01)
    moe_w_expert = nrm(22, (L, D_MODEL, N_EXPERTS), D_MODEL ** -0.5)
    moe_b_expert = nrm(23, (L, N_EXPERTS), 0.01)
    moe_w_gate = nrm(24, (L, N_EXPERTS, D_MODEL, D_FF_EXPERT), D_MODEL ** -0.5)
    moe_w_up = nrm(25, (L, N_EXPERTS, D_MODEL, D_FF_EXPERT), D_MODEL ** -0.5)
    moe_w_down = nrm(26, (L, N_EXPERTS, D_FF_EXPERT, D_MODEL), D_FF_EXPERT ** -0.5 * DN_BETA)
    ln3_g = 1.0 + nrm(27, (L, D_MODEL), 0.02)
    ln3_b = nrm(28, (L, D_MODEL), 0.02)
    return {"x": x, "mem": mem, "positions": positions, "w_in": w_in, "gdn_conv_w": gdn_conv_w,
            "gdn_a_log": gdn_a_log, "gdn_dt_bias": gdn_dt_bias, "gdn_norm_w": gdn_norm_w,
            "cmp_pe": cmp_pe, "cmp_w1": cmp_w1, "cmp_b1": cmp_b1, "cmp_w2": cmp_w2, "w_out": w_out,
            "ln1_g": ln1_g, "ln1_b": ln1_b, "xa_wq": xa_wq, "xa_wkv": xa_wkv, "xa_wo": xa_wo,
            "ln2_g": ln2_g, "ln2_b": ln2_b, "moe_w_group": moe_w_group, "moe_b_group": moe_b_group,
            "moe_w_expert": moe_w_expert, "moe_b_expert": moe_b_expert, "moe_w_gate": moe_w_gate,
            "moe_w_up": moe_w_up, "moe_w_down": moe_w_down, "ln3_g": ln3_g, "ln3_b": ln3_b}


def reference(x, mem, positions, w_in, gdn_conv_w, gdn_a_log, gdn_dt_bias, gdn_norm_w,
              cmp_pe, cmp_w1, cmp_b1, cmp_w2, w_out, ln1_g, ln1_b, xa_wq, xa_wkv, xa_wo,
              ln2_g, ln2_b, moe_w_group, moe_b_group, moe_w_expert, moe_b_expert,
              moe_w_gate, moe_w_up, moe_w_down, ln3_g, ln3_b):
    cos, sin = rope_tables(positions, NSA_DH)
    h = x
    for l in range(DEPTH):
        mix = token_mixer(h, cos, sin, w_in[l], gdn_conv_w[l], gdn_a_log[l], gdn_dt_bias[l], gdn_norm_w[l],
                          cmp_pe[l], cmp_w1[l], cmp_b1[l], cmp_w2[l], w_out[l])
        h = layer_norm(DN_ALPHA * h + mix, ln1_g[l], ln1_b[l])
        h = layer_norm(DN_ALPHA * h + memory_xattn(h, mem, xa_wq[l], xa_wkv[l], xa_wo[l]), ln2_g[l], ln2_b[l])
        ffn = hier_moe(h, moe_w_group[l], moe_b_group[l], moe_w_expert[l], moe_b_expert[l],
                       moe_w_gate[l], moe_w_up[l], moe_w_down[l])
        h = layer_norm(DN_ALPHA * h + ffn, ln3_g[l], ln3_b[l])
    return h
```

```python
import contextlib
import numpy as np
import concourse.bass as bass
import concourse.mybir as mybir
from concourse.bass_utils import run_bass_kernel_spmd

F32 = mybir.dt.float32
BF16 = mybir.dt.bfloat16
F32R = mybir.dt.float32r
I32 = mybir.dt.int32
AF = mybir.ActivationFunctionType
ALU = mybir.AluOpType
AX = mybir.AxisListType

ENGS = ("pe", "act", "dve", "pool", "sp")
EPOCH = 12000
CHECK_PSUM = False
INST_LABELS = None

S = 2048
D = 1024
NT = 16
DN_ALPHA = 2.0 ** 0.25
LN_EPS = 1e-5
RMS_EPS = 1e-6


class Reg:
    __slots__ = ("key", "psum", "last_w", "readers")

    def __init__(self, key, psum):
        self.key = key
        self.psum = psum
        self.last_w = None
        self.readers = []


class Op:
    __slots__ = ("eng", "fn", "deps", "sem", "target", "dma", "needs_inc", "idx", "pbanks", "label")

    def __init__(self, eng, fn, dma):
        self.eng = eng
        self.fn = fn
        self.deps = []
        self.sem = None
        self.target = 0
        self.dma = dma
        self.needs_inc = dma
        self.idx = 0


class Prog:
    def __init__(self, nc, n_dma_sems=40):
        self.nc = nc
        self.ops = {e: [] for e in ENGS}
        self.regs = {}
        self.n_dma_sems = n_dma_sems
        self.dma_last = [None] * n_dma_sems
        self.dma_count = [0] * n_dma_sems
        self.dma_rr = 0
        self.dma_rr_pool = 0
        self.fence_ops = []

    def R(self, *key):
        r = self.regs.get(key)
        if r is None:
            r = Reg(key, len(key) > 0 and key[0] == "psum")
            self.regs[key] = r
        return r

    def _regs(self, lst):
        out = []
        for x in lst:
            if isinstance(x, Reg):
                out.append(x)
            elif isinstance(x, tuple):
                out.append(self.R(*x))
            else:
                out.append(self.R(x))
        return out

    def fence(self):
        f = []
        for e in ENGS:
            for o in reversed(self.ops[e]):
                if not o.dma:
                    f.append(o)
                    break
        for o in self.dma_last:
            if o is not None:
                f.append(o)
        for o in f:
            o.needs_inc = True
        self.fence_ops = f
        self.regs = {}

    def op(self, eng, fn, reads=(), writes=(), dma=False):
        o = Op(eng, fn, dma)
        deps = {}
        reads = self._regs(reads)
        writes = self._regs(writes)
        o.pbanks = set(r.key[1] for r in reads + writes if r.psum)
        o.label = getattr(self, "label", "")
        for r in reads:
            if r.psum:
                writes.append(r)
                continue
            if r.last_w is not None:
                deps[id(r.last_w)] = r.last_w
            r.readers.append(o)
        for r in writes:
            if r.last_w is not None:
                deps[id(r.last_w)] = r.last_w
            for q in r.readers:
                if q is not o:
                    deps[id(q)] = q
            r.readers = []
            r.last_w = o
        for q in self.fence_ops:
            deps[id(q)] = q
        if dma:
            half = self.n_dma_sems // 2
            if eng == "pool":
                s = half + self.dma_rr_pool
                self.dma_rr_pool = (self.dma_rr_pool + 1) % (self.n_dma_sems - half)
            else:
                s = self.dma_rr
                self.dma_rr = (self.dma_rr + 1) % half
            prev = self.dma_last[s]
            if prev is not None:
                deps[id(prev)] = prev
            self.dma_last[s] = o
            self.dma_count[s] += 1
            o.sem = ("dma", s)
            o.target = 16 * self.dma_count[s]
        o.deps = list(deps.values())
        for d in o.deps:
            d.needs_inc = True
        o.idx = len(self.ops[eng])
        self.ops[eng].append(o)
        return o

    def dma(self, out, in_, reads=(), writes=(), eng="sp", **kw):
        return self.op(eng, lambda e: e.dma_start(out=out, in_=in_, **kw), reads, writes, dma=True)

    def emit(self):
        nc = self.nc
        n_eng_sems = {}
        for e in ENGS:
            cnt = 0
            ep = 0
            for o in self.ops[e]:
                if o.dma:
                    continue
                if o.needs_inc:
                    cnt += 1
                    if cnt > EPOCH:
                        ep += 1
                        cnt = 1
                o.sem = (e, ep)
                o.target = cnt
            n_eng_sems[e] = ep + 1
        with contextlib.ExitStack() as st:
            sems = {}
            for e in ENGS:
                for ep in range(n_eng_sems[e]):
                    sems[(e, ep)] = st.enter_context(nc.semaphore(f"s_{e}_{ep}"))
            for s in range(self.n_dma_sems):
                if self.dma_count[s] > 0:
                    sems[("dma", s)] = st.enter_context(nc.semaphore(f"s_dma_{s}"))
            block = st.enter_context(nc.Block())

            def make(e):
                ops = self.ops[e]

                def body(eng):
                    waited = {}
                    for o in ops:
                        for d in o.deps:
                            if d.eng == e and not d.dma and not o.dma:
                                if e == "pe":
                                    continue
                                if e != "pool" and o.idx - d.idx > 3:
                                    continue
                            if waited.get(d.sem, 0) < d.target:
                                eng.wait_ge(sems[d.sem], d.target)
                                waited[d.sem] = d.target
                        ins = o.fn(eng)
                        if INST_LABELS is not None:
                            INST_LABELS[ins.ins.name] = o.label
                        if CHECK_PSUM:
                            touched = set()
                            for pap in tuple(ins.ins.ins) + tuple(ins.ins.outs):
                                if getattr(pap, "memref", None) == "psum_all":
                                    epb = 1024 if pap.dtype == BF16 else 512
                                    off = int(pap.offset) % (8 * epb)
                                    ext = 0
                                    for st, nn in list(pap.ap)[1:]:
                                        ext += abs(int(st)) * (int(nn) - 1)
                                    touched.update(range(off // epb, (off + ext) // epb + 1))
                            if not touched <= o.pbanks:
                                raise AssertionError(f"PSUM banks touched {touched} not declared {o.pbanks} in {ins.ins.concise()}")
                        if o.needs_inc:
                            ins.then_inc(sems[o.sem], 16 if o.dma else 1)
                    for o in ops:
                        if o.dma and self.dma_last[o.sem[1]] is o:
                            if waited.get(o.sem, 0) < o.target:
                                eng.wait_ge(sems[o.sem], o.target)
                                waited[o.sem] = o.target

                return body

            block.tensor(make("pe"))
            block.scalar(make("act"))
            block.vector(make("dve"))
            block.gpsimd(make("pool"))
            block.sync(make("sp"))


class Ctx:
    SB_BASE = 16640
    SB_LIMIT = 228800

    def __init__(self, nc):
        self.nc = nc
        self.P = Prog(nc)
        self.ptr = self.SB_BASE
        self.nalloc = 0
        self.rr = 0
        ps = nc.alloc_psum_tensor("psum_all", [128, 4096], F32).ap()
        self.ps = ps
        self.psb = ps.bitcast(BF16)

    def bank(self, b, n=1):
        return self.ps[:, b * 512:(b + n) * 512]

    def bankb(self, b, n=1):
        return self.psb[:, b * 1024:(b + n) * 1024]

    def sb(self, shape, dtype=F32):
        nbytes = int(np.prod(shape[1:])) * (2 if dtype == BF16 else 4)
        nbytes = (nbytes + 31) // 32 * 32
        off = self.ptr
        self.ptr += nbytes
        assert self.ptr <= self.SB_LIMIT, f"SBUF overflow {self.ptr}"
        self.nalloc += 1
        import sys as _sys
        if not hasattr(self, "names"):
            self.names = {}
        self.names[self.nalloc] = (list(shape), str(dtype), _sys._getframe(1).f_lineno, off)
        return self.nc.alloc_sbuf_tensor_at(f"t{self.nalloc}", list(shape), dtype, offset=off).ap()

    def mark(self):
        return self.ptr

    def release(self, m):
        self.P.fence()
        self.ptr = m

    def dbg(self, name, ap, reads):
        if not getattr(self, "debug", False):
            return
        shape = list(ap.shape)
        dt_ = F32 if ap.dtype == F32R else ap.dtype
        d = self.nc.dram_tensor("dbg_" + name, shape, dt_, kind="ExternalOutput").ap()
        self.P.dma(d, ap.bitcast(F32) if ap.dtype == F32R else ap, reads=reads)

    def evac_eng(self):
        self.rr += 1
        return "act" if self.rr % 2 else "dve"


def copy_op(P, eng, out, in_, reads, writes):
    if eng == "act":
        return P.op("act", lambda e: e.copy(out, in_), reads, writes)
    return P.op(eng, lambda e: e.tensor_copy(out, in_), reads, writes)


def build_consts(C):
    P = C.P
    ident = C.sb([128, 128], F32)
    P.op("pool", lambda e: e.memset(ident, 0.0), writes=["ident"])
    P.op("pool", lambda e: e.affine_select(out=ident, in_=ident, pattern=[[-1, 128]], compare_op=ALU.not_equal,
                                            fill=1.0, base=0, channel_multiplier=1), reads=["ident"], writes=["ident"])
    onesr = C.sb([128, 128], F32R)
    onesf = C.sb([128, 128], F32)
    P.op("pool", lambda e: e.memset(onesf, 1.0), writes=["onesf"])
    P.op("pool", lambda e: e.tensor_copy(onesr, onesf), reads=["onesf"], writes=["onesr"])
    eps_ln = C.sb([128, 1], F32)
    P.op("pool", lambda e: e.memset(eps_ln, LN_EPS), writes=["eps_ln"])
    identb = C.sb([128, 128], BF16)
    P.op("pool", lambda e: e.tensor_copy(identb, ident), reads=["ident"], writes=["identb"])
    C.identb = identb
    C.ident = ident
    C.onesr = onesr
    C.eps_ln = eps_ln


def to_featmajor(C, src, hT, name, ntiles=NT, xt_bufs=None):
    P = C.P
    m = C.mark()
    xt = xt_bufs if xt_bufs is not None else [C.sb([128, 1024], F32) for _ in range(2)]
    for t in range(ntiles):
        b = t % 2
        P.dma(xt[b], src[t * 128:(t + 1) * 128, :], writes=[(name + "_xt", b)])
        for half in range(2):
            bk = 2 * b + half
            for j in range(4):
                kc = half * 4 + j
                P.op("pe", lambda e, bk=bk, j=j, kc=kc, b=b: e.transpose(C.bank(bk)[:, j * 128:(j + 1) * 128],
                                                                        xt[b][:, kc * 128:(kc + 1) * 128], C.ident),
                     reads=[(name + "_xt", b), "ident"], writes=[("psum", bk)])
            copy_op(P, C.evac_eng(), hT[:, half * 4:(half + 1) * 4, t * 128:(t + 1) * 128],
                    C.bank(bk).rearrange("p (k n) -> p k n", k=4), reads=[("psum", bk)], writes=[(name, t, half)])
    if xt_bufs is None:
        C.release(m)
    return [(name, t, half) for t in range(ntiles) for half in range(2)]


def fm_gen(C, src, hT, name, ntiles, xt):
    P = C.P
    for t in range(ntiles):
        b = t % 2
        P.dma(xt[b], src[t * 128:(t + 1) * 128, :], writes=[(name + "_xt", b)])
        for half in range(2):
            bk = 2 * b + half
            for j in range(4):
                kc = half * 4 + j
                P.op("pe", lambda e, bk=bk, j=j, kc=kc, b=b: e.transpose(C.bank(bk)[:, j * 128:(j + 1) * 128],
                                                                        xt[b][:, kc * 128:(kc + 1) * 128], C.ident),
                     reads=[(name + "_xt", b), "ident"], writes=[("psum", bk)])
            copy_op(P, C.evac_eng(), hT[:, half * 4:(half + 1) * 4, t * 128:(t + 1) * 128],
                    C.bank(bk).rearrange("p (k n) -> p k n", k=4), reads=[("psum", bk)], writes=[(name, t, half)])
        yield


class LNPipe:
    def __init__(self, C, g_bc, b_bc, tag, scratch):
        self.C, self.g_bc, self.b_bc, self.tag, self.scratch = C, g_bc, b_bc, tag, scratch
        self.pending = None

    def push(self, pre, out, rd_pre, wr_out, par, after_fn=None):
        C, P, tag = self.C, self.C.P, self.tag
        stats, mv, rstd, nmr = self.scratch[par]
        tg = (tag, par)
        for c in range(2):
            P.op("dve", lambda e, c=c: e.bn_stats(stats[:, c, :], pre[:, c * 512:(c + 1) * 512]), reads=rd_pre, writes=[(tg, "st", c)])
        P.op("dve", lambda e: e.bn_aggr(mv, stats.rearrange("p a b -> p (a b)")), reads=[(tg, "st", 0), (tg, "st", 1)], writes=[(tg, "mv")])
        P.op("act", lambda e: e.activation(rstd, mv[:, 1:2], AF.Sqrt, bias=C.eps_ln), reads=[(tg, "mv"), "eps_ln"], writes=[(tg, "sd")])
        g_bc, b_bc = self.g_bc, self.b_bc

        def stage2():
            P.op("dve", lambda e: e.reciprocal(rstd, rstd), reads=[(tg, "sd")], writes=[(tg, "rstd")])
            P.op("dve", lambda e: e.tensor_scalar(nmr, mv[:, 0:1], rstd, -1.0, ALU.mult, ALU.mult), reads=[(tg, "mv"), (tg, "rstd")], writes=[(tg, "nmr")])
            P.op("act", lambda e: e.activation(out, pre, AF.Identity, bias=nmr, scale=rstd), reads=list(rd_pre) + [(tg, "rstd"), (tg, "nmr")], writes=wr_out)
            P.op("dve", lambda e: e.tensor_tensor(out, out, g_bc, ALU.mult), reads=list(wr_out) + [(tag, "g")], writes=wr_out)
            P.op("pool", lambda e: e.tensor_tensor(out, out, b_bc, ALU.add), reads=list(wr_out) + [(tag, "b")], writes=wr_out)
            if after_fn is not None:
                after_fn()

        prev = self.pending
        self.pending = stage2
        if prev is not None:
            prev()

    def flush(self):
        if self.pending is not None:
            self.pending()
        self.pending = None


def ln_setup(C, g_dram, b_dram, tag, nbuf=2):
    P = C.P
    g_bc = C.sb([128, 1024], F32)
    b_bc = C.sb([128, 1024], F32)
    P.dma(g_bc, g_dram.partition_broadcast(128), writes=[(tag, "g")])
    P.dma(b_bc, b_dram.partition_broadcast(128), writes=[(tag, "b")])
    scratch = [(C.sb([128, 2, 6], F32), C.sb([128, 2], F32), C.sb([128, 1], F32), C.sb([128, 1], F32)) for _ in range(nbuf)]
    return g_bc, b_bc, scratch


def phase_B(C, A):
    P = C.P
    P.label = "B"
    P.fence()
    m0 = C.mark()
    mT = C.sb([128, 8, S], F32R)
    wo = C.sb([128, 8, 1024], F32R)
    P.dma(wo, A["w_out"], writes=["wo"], eng="pool")
    g_bc, b_bc, scratch = ln_setup(C, A["ln1_g"], A["ln1_b"], "ln1", nbuf=4)
    xt = [C.sb([128, 1024], F32) for _ in range(4)]
    pre = [C.sb([128, 1024], F32) for _ in range(4)]
    xtm = [C.sb([128, 1024], F32) for _ in range(2)]
    gen = fm_gen(C, A["merged"], mT, "mT", NT, xtm)
    next(gen)
    next(gen)
    lnp = LNPipe(C, g_bc, b_bc, "ln1", scratch)
    for t in range(NT):
        b = t % 4
        pb = t % 2
        next(gen, None)
        P.dma(xt[b], A["x"][t * 128:(t + 1) * 128, :], writes=[("Bx", b)])
        for half in range(2):
            bk = 4 + 2 * pb + half
            for kc in range(8):
                P.op("pe", lambda e, bk=bk, kc=kc, t=t, half=half: e.matmul(C.bank(bk), mT[:, kc, t * 128:(t + 1) * 128],
                                                                          wo[:, kc, half * 512:(half + 1) * 512], start=(kc == 0), stop=(kc == 7)),
                     reads=[("mT", t, kc // 4), "wo"], writes=[("psum", bk)])
        P.op("dve", lambda e, b=b, pb=pb: e.scalar_tensor_tensor(pre[b], xt[b], DN_ALPHA, C.bank(4 + 2 * pb, 2), ALU.mult, ALU.add),
             reads=[("Bx", b), ("psum", 4 + 2 * pb), ("psum", 5 + 2 * pb)], writes=[("Bpre", b)])
        lnp.push(pre[b], xt[b], [("Bpre", b)], [("Bx", b)], b,
                 after_fn=(lambda t=t, b=b: P.dma(A["h1"][t * 128:(t + 1) * 128, :], xt[b], reads=[("Bx", b)], eng="pool")))
    lnp.flush()
    C.release(m0)


def phase_C(C, A):
    P = C.P
    P.label = "C"
    P.fence()
    m0 = C.mark()
    h1Tb = [C.sb([128, 8, 512], F32R) for _ in range(2)]
    memT = C.sb([128, 8, 256], F32R)
    kT = C.sb([128, 8, 256], F32R)
    V = C.sb([128, 2, 1024], F32R)
    wq = C.sb([128, 8, 1024], F32R)
    wkv = C.sb([128, 8, 1024], F32R)
    wo = wkv
    P.dma(wkv, A["xa_wk"], writes=["wkv"], eng="pool")
    P.dma(wq, A["xa_wq"], writes=["wq"], eng="pool")
    to_featmajor(C, A["mem"], memT, "memT", ntiles=2)
    rd_memT = [("memT", t, hf) for t in range(2) for hf in range(2)]
    for j in range(8):
        bk = j % 2
        for kc in range(8):
            P.op("pe", lambda e, bk=bk, kc=kc, j=j: e.matmul(C.bank(bk)[:, 0:256], wkv[:, kc, j * 128:(j + 1) * 128], memT[:, kc, :],
                                                          start=(kc == 0), stop=(kc == 7)),
                 reads=["wkv"] + rd_memT, writes=[("psum", bk)])
        copy_op(P, C.evac_eng(), kT[:, j, :], C.bank(bk)[:, 0:256], reads=[("psum", bk)], writes=[("kT", j)])
    P.dma(wkv, A["xa_wv"], writes=["wkv"], eng="pool")
    for mt in range(2):
        for half in range(2):
            bk = 2 + half
            for kc in range(8):
                P.op("pe", lambda e, bk=bk, kc=kc, mt=mt, half=half: e.matmul(C.bank(bk), memT[:, kc, mt * 128:(mt + 1) * 128],
                                                                            wkv[:, kc, half * 512:(half + 1) * 512], start=(kc == 0), stop=(kc == 7)),
                     reads=["wkv"] + rd_memT, writes=[("psum", bk)])
            copy_op(P, C.evac_eng(), V[:, mt, half * 512:(half + 1) * 512], C.bank(bk), reads=[("psum", bk)], writes=[("V", mt, half)])
    C.dbg("kT", kT, [("kT", j) for j in range(8)])
    C.dbg("V", V, [("V", a, b_) for a in range(2) for b_ in range(2)])
    C.dbg("memT", memT, rd_memT)
    P.dma(wo, A["xa_wo"], writes=["wkv"], eng="pool")
    g_bc, b_bc, scratch = ln_setup(C, A["ln2_g"], A["ln2_b"], "ln2", nbuf=4)
    xtf = [C.sb([128, 1024], F32) for _ in range(2)]
    qT = [C.sb([128, 2, 512], F32R) for _ in range(2)]
    ex = [C.sb([128, 2, 512], F32R) for _ in range(2)]
    rden = [C.sb([128, 512], F32) for _ in range(2)]
    oT = C.sb([128, 8, 512], F32R)
    xt = [C.sb([128, 1024], F32) for _ in range(4)]
    pre = [C.sb([128, 1024], F32) for _ in range(4)]
    it = 0
    lnp = LNPipe(C, g_bc, b_bc, "ln2", scratch)
    for tb in range(4):
        h1T = h1Tb[tb % 2]
        hname = "h1T%d" % (tb % 2)
        rd_h = to_featmajor(C, A["h1"][tb * 512:(tb + 1) * 512, :], h1T, hname, ntiles=4, xt_bufs=xtf)
        for h in range(4):
            b = it % 2
            it += 1
            for c in range(2):
                bk = c
                for kc in range(8):
                    P.op("pe", lambda e, bk=bk, kc=kc, h=h, c=c, h1T=h1T: e.matmul(C.bank(bk), wq[:, kc, h * 256 + c * 128:h * 256 + (c + 1) * 128],
                                                                               h1T[:, kc, :], start=(kc == 0), stop=(kc == 7)),
                         reads=["wq"] + rd_h, writes=[("psum", bk)])
                P.op("act", lambda e, bk=bk, b=b, c=c: e.activation(qT[b][:, c, :], C.bank(bk), AF.Identity, scale=1.0 / 16.0),
                     reads=[("psum", bk)], writes=[("qT", b, c)])
            for mt in range(2):
                bk = 2 + mt
                for c in range(2):
                    P.op("pe", lambda e, bk=bk, c=c, mt=mt, h=h, b=b: e.matmul(C.bank(bk), kT[:, h * 2 + c, mt * 128:(mt + 1) * 128], qT[b][:, c, :],
                                                                             start=(c == 0), stop=(c == 1)),
                         reads=[("kT", h * 2 + c), ("qT", b, c)], writes=[("psum", bk)])
                P.op("act", lambda e, bk=bk, b=b, mt=mt: e.activation(ex[b][:, mt, :], C.bank(bk), AF.Exp), reads=[("psum", bk)], writes=[("ex", b, mt)])
            for mt in range(2):
                P.op("pe", lambda e, mt=mt, b=b: e.matmul(C.bank(4), C.onesr, ex[b][:, mt, :], start=(mt == 0), stop=(mt == 1)),
                     reads=["onesr", ("ex", b, mt)], writes=[("psum", 4)])
            P.op("dve", lambda e, b=b: e.reciprocal(rden[b], C.bank(4)), reads=[("psum", 4)], writes=[("rden", b)])
            for c in range(2):
                bk = 5 + c
                for mt in range(2):
                    P.op("pe", lambda e, bk=bk, mt=mt, c=c, h=h, b=b: e.matmul(C.bank(bk), V[:, mt, h * 256 + c * 128:h * 256 + (c + 1) * 128], ex[b][:, mt, :],
                                                                             start=(mt == 0), stop=(mt == 1)),
                         reads=[("V", mt, (h * 256 + c * 128) // 512), ("ex", b, mt)], writes=[("psum", bk)])
                P.op("dve", lambda e, bk=bk, b=b, h=h, c=c: e.tensor_tensor(oT[:, h * 2 + c, :], C.bank(bk), rden[b], ALU.mult),
                     reads=[("psum", bk), ("rden", b)], writes=[("oT", h * 2 + c)])
        if tb == 0:
            C.dbg("oT", oT, [("oT", j) for j in range(8)])
            C.dbg("qT", qT[1], [("qT", 1, 0), ("qT", 1, 1)])
            C.dbg("ex", ex[1], [("ex", 1, 0), ("ex", 1, 1)])
            C.dbg("rden", rden[1], [("rden", 1)])
            C.dbg("h1T", h1T, rd_h)
        for tt in range(4):
            t = tb * 4 + tt
            b = t % 4
            pb = t % 2
            P.dma(xt[b], A["h1"][t * 128:(t + 1) * 128, :], writes=[("Cx", b)])
            for half in range(2):
                bk = 0 + half if pb == 0 else 2 + half
                for j in range(8):
                    P.op("pe", lambda e, bk=bk, j=j, tt=tt, half=half: e.matmul(C.bank(bk), oT[:, j, tt * 128:(tt + 1) * 128],
                                                                              wo[:, j, half * 512:(half + 1) * 512], start=(j == 0), stop=(j == 7)),
                         reads=[("oT", j), "wkv"], writes=[("psum", bk)])
            b0 = 0 if pb == 0 else 2
            P.op("dve", lambda e, b=b, b0=b0: e.scalar_tensor_tensor(pre[b], xt[b], DN_ALPHA, C.bank(b0, 2), ALU.mult, ALU.add),
                 reads=[("Cx", b), ("psum", b0), ("psum", b0 + 1)], writes=[("Cpre", b)])
            lnp.push(pre[b], xt[b], [("Cpre", b)], [("Cx", b)], b,
                     after_fn=(lambda t=t, b=b: P.dma(A["h2"][t * 128:(t + 1) * 128, :], xt[b], reads=[("Cx", b)], eng="pool")))
    lnp.flush()
    C.release(m0)


def phase_D(C, A):
    P = C.P
    P.label = "Drouter"
    P.fence()
    m0 = C.mark()
    h2T = C.sb([128, 8, S], F32R)
    acc = C.sb([128, NT, 1024], F32)
    comb = C.sb([128, NT, 32], F32)
    m1 = C.mark()
    wr = C.sb([128, 8, 36], F32R)
    P.dma(wr, A["moe_wr"], writes=["wr"], eng="pool")
    br = C.sb([128, 36], F32)
    P.dma(br, A["moe_br"].partition_broadcast(128), writes=["br"])
    rd_all = to_featmajor(C, A["h2"], h2T, "h2T")
    lg = C.sb([128, NT, 36], F32)
    for t in range(NT):
        bk = t // 8
        for kc in range(8):
            P.op("pe", lambda e, bk=bk, kc=kc, t=t: e.matmul(C.bank(bk)[:, (t % 8) * 64:(t % 8) * 64 + 36], h2T[:, kc, t * 128:(t + 1) * 128], wr[:, kc, :],
                                                          start=(kc == 0), stop=(kc == 7)),
                 reads=[("h2T", t, kc // 4), "wr"], writes=[("psum", bk)])
    for bk in range(2):
        P.op("dve", lambda e, bk=bk: e.tensor_tensor(lg[:, bk * 8:(bk + 1) * 8, :], C.bank(bk).rearrange("p (t c) -> p t c", c=64)[:, :, 0:36],
                                                     br.unsqueeze(1).to_broadcast([128, 8, 36]), ALU.add),
             reads=[("psum", bk), "br"], writes=[("lg", bk)])
    rd_lg = [("lg", 0), ("lg", 1)]
    gmx = C.sb([128, NT], F32)
    gex = C.sb([128, NT, 4], F32)
    gsum = C.sb([128, NT], F32)
    ptop = C.sb([128, NT], F32)
    ohg = C.sb([128, NT, 4], F32)
    sel = C.sb([128, NT, 4, 8], F32)
    el = C.sb([128, NT, 8], F32)
    e1 = C.sb([128, NT], F32)
    e2 = C.sb([128, NT], F32)
    oh1 = C.sb([128, NT, 8], F32)
    oh2 = C.sb([128, NT, 8], F32)
    el2 = C.sb([128, NT, 8], F32)
    dd = C.sb([128, NT], F32)
    w1 = C.sb([128, NT], F32)
    w2 = C.sb([128, NT], F32)
    c8 = C.sb([128, NT, 8], F32)
    lgg = lg[:, :, 0:4]
    lge = lg[:, :, 4:36].rearrange("p t (g e) -> p t g e", g=4)
    V = "dve"
    P.op(V, lambda e: e.tensor_reduce(gmx, lgg, AX.X, ALU.max), reads=rd_lg, writes=["gmx"])
    P.op(V, lambda e: e.tensor_tensor(gex, lgg, gmx.unsqueeze(2).to_broadcast([128, NT, 4]), ALU.subtract), reads=rd_lg + ["gmx"], writes=["gex"])
    P.op(V, lambda e: e.tensor_tensor(ohg, lgg, gmx.unsqueeze(2).to_broadcast([128, NT, 4]), ALU.is_equal), reads=rd_lg + ["gmx"], writes=["ohg"])
    P.op("act", lambda e: e.activation(gex, gex, AF.Exp), reads=["gex"], writes=["gex"])
    P.op(V, lambda e: e.tensor_reduce(gsum, gex, AX.X, ALU.add), reads=["gex"], writes=["gsum"])
    P.op(V, lambda e: e.reciprocal(ptop, gsum), reads=["gsum"], writes=["ptop"])
    P.op(V, lambda e: e.tensor_tensor(sel, lge, ohg.unsqueeze(3).to_broadcast([128, NT, 4, 8]), ALU.mult), reads=rd_lg + ["ohg"], writes=["sel"])
    P.op(V, lambda e: e.tensor_reduce(el, sel.rearrange("p t g e -> p t e g"), AX.X, ALU.add), reads=["sel"], writes=["el"])
    P.op(V, lambda e: e.tensor_reduce(e1, el, AX.X, ALU.max), reads=["el"], writes=["e1"])
    P.op(V, lambda e: e.tensor_tensor(oh1, el, e1.unsqueeze(2).to_broadcast([128, NT, 8]), ALU.is_equal), reads=["el", "e1"], writes=["oh1"])
    P.op(V, lambda e: e.scalar_tensor_tensor(el2, oh1, -1e30, el, ALU.mult, ALU.add), reads=["oh1", "el"], writes=["el2"])
    P.op(V, lambda e: e.tensor_reduce(e2, el2, AX.X, ALU.max), reads=["el2"], writes=["e2"])
    P.op(V, lambda e: e.tensor_tensor(oh2, el2, e2.unsqueeze(2).to_broadcast([128, NT, 8]), ALU.is_equal), reads=["el2", "e2"], writes=["oh2"])
    P.op(V, lambda e: e.tensor_tensor(dd, e2, e1, ALU.subtract), reads=["e1", "e2"], writes=["dd"])
    P.op("act", lambda e: e.activation(dd, dd, AF.Exp), reads=["dd"], writes=["dd"])
    P.op(V, lambda e: e.tensor_scalar(w1, dd, 1.0, None, ALU.add), reads=["dd"], writes=["w1"])
    P.op(V, lambda e: e.reciprocal(w1, w1), reads=["w1"], writes=["w1"])
    P.op(V, lambda e: e.tensor_tensor(w1, w1, ptop, ALU.mult), reads=["w1", "ptop"], writes=["w1"])
    P.op(V, lambda e: e.tensor_tensor(w2, w1, dd, ALU.mult), reads=["w1", "dd"], writes=["w2"])
    P.op(V, lambda e: e.tensor_tensor(oh1, oh1, w1.unsqueeze(2).to_broadcast([128, NT, 8]), ALU.mult), reads=["oh1", "w1"], writes=["oh1"])
    P.op(V, lambda e: e.tensor_tensor(oh2, oh2, w2.unsqueeze(2).to_broadcast([128, NT, 8]), ALU.mult), reads=["oh2", "w2"], writes=["oh2"])
    P.op(V, lambda e: e.tensor_tensor(c8, oh1, oh2, ALU.add), reads=["oh1", "oh2"], writes=["c8"])
    P.op(V, lambda e: e.tensor_tensor(comb.rearrange("p t (g e) -> p t g e", g=4), ohg.unsqueeze(3).to_broadcast([128, NT, 4, 8]),
                                      c8.unsqueeze(2).to_broadcast([128, NT, 4, 8]), ALU.mult), reads=["ohg", "c8"], writes=["comb"])
    P.op("pool", lambda e: e.memset(acc, 0.0), writes=[("acc", t) for t in range(NT)])
    P.fence()
    C.release(m1)
    P.label = "Dexperts"
    wg = [C.sb([128, 8, 256], F32R) for _ in range(2)]
    wu = [C.sb([128, 8, 256], F32R) for _ in range(2)]
    wd = [C.sb([128, 2, 1024], F32R) for _ in range(2)]
    sg = [C.sb([128, 512], F32) for _ in range(2)]
    hg = [C.sb([128, 2, 512], F32R) for _ in range(2)]
    it = 0
    dn = 0
    for ex in range(32):
        wb = ex % 2
        P.dma(wg[wb], A["moe_wg"][ex], writes=[("wg", wb)], eng="pool")
        P.dma(wu[wb], A["moe_wu"][ex], writes=[("wu", wb)], eng="pool")
        P.dma(wd[wb], A["moe_wd"][ex], writes=[("wd", wb)], eng="pool")
        for tb in range(4):
            hb = it % 2
            it += 1
            rd_h = [("h2T", t, hf) for t in range(tb * 4, tb * 4 + 4) for hf in range(2)]
            for fc in range(2):
                for kc in range(8):
                    P.op("pe", lambda e, fc=fc, kc=kc, wb=wb, tb=tb: e.matmul(C.bank(fc), wg[wb][:, kc, fc * 128:(fc + 1) * 128], h2T[:, kc, tb * 512:(tb + 1) * 512],
                                                                            start=(kc == 0), stop=(kc == 7)),
                         reads=[("wg", wb)] + rd_h, writes=[("psum", fc)])
                for kc in range(8):
                    P.op("pe", lambda e, fc=fc, kc=kc, wb=wb, tb=tb: e.matmul(C.bank(2 + fc), wu[wb][:, kc, fc * 128:(fc + 1) * 128], h2T[:, kc, tb * 512:(tb + 1) * 512],
                                                                            start=(kc == 0), stop=(kc == 7)),
                         reads=[("wu", wb)] + rd_h, writes=[("psum", 2 + fc)])
                P.op("act", lambda e, fc=fc: e.activation(sg[fc], C.bank(fc), AF.Silu), reads=[("psum", fc)], writes=[("sg", fc)])
                P.op("dve", lambda e, fc=fc, hb=hb: e.tensor_tensor(hg[hb][:, fc, :], C.bank(2 + fc), sg[fc], ALU.mult),
                     reads=[("psum", 2 + fc), ("sg", fc)], writes=[("hg", hb, fc)])
            for tt in range(4):
                t = tb * 4 + tt
                b0 = 4 + 2 * (dn % 2)
                dn += 1
                for half in range(2):
                    for fc in range(2):
                        P.op("pe", lambda e, b0=b0, half=half, fc=fc, hb=hb, tt=tt, wb=wb: e.matmul(C.bank(b0 + half), hg[hb][:, fc, tt * 128:(tt + 1) * 128],
                                                                                                 wd[wb][:, fc, half * 512:(half + 1) * 512], start=(fc == 0), stop=(fc == 1)),
                             reads=[("hg", hb, fc), ("wd", wb)], writes=[("psum", b0 + half)])
                P.op("dve", lambda e, b0=b0, t=t, ex=ex: e.scalar_tensor_tensor(acc[:, t, :], C.bank(b0, 2), comb[:, t, ex:ex + 1], acc[:, t, :], ALU.mult, ALU.add),
                     reads=[("psum", b0), ("psum", b0 + 1), "comb", ("acc", t)], writes=[("acc", t)])
    P.fence()
    C.release(m1)
    P.label = "Dln3"
    g_bc, b_bc, scratch = ln_setup(C, A["ln3_g"], A["ln3_b"], "ln3", nbuf=4)
    xt = [C.sb([128, 1024], F32) for _ in range(4)]
    lnp = LNPipe(C, g_bc, b_bc, "ln3", scratch)
    for t in range(NT):
        b = t % 4
        P.dma(xt[b], A["h2"][t * 128:(t + 1) * 128, :], writes=[("Dx", b)])
        P.op("dve", lambda e, b=b, t=t: e.scalar_tensor_tensor(acc[:, t, :], xt[b], DN_ALPHA, acc[:, t, :], ALU.mult, ALU.add),
             reads=[("Dx", b), ("acc", t)], writes=[("acc", t)])
        lnp.push(acc[:, t, :], xt[b], [("acc", t)], [("Dx", b)], b,
                 after_fn=(lambda t=t, b=b: P.dma(A["out"][t * 128:(t + 1) * 128, :], xt[b], reads=[("Dx", b)], eng="pool")))
    lnp.flush()
    C.release(m0)


SCALE_DH = 128.0 ** -0.5
BIGM = 1.0e4
TWO_PI = 6.283185307179586
RC1 = 6.28125
RC2 = TWO_PI - RC1


def cast_copy(P, eng, out, in_, reads, writes):
    return P.op(eng, lambda e: e.tensor_copy(out, in_), reads, writes)


def phase_A0(C, A):
    P = C.P
    P.label = "A0"
    P.fence()
    C.mA0 = C.mark()
    C.xT = C.sb([128, 8, S], F32R)
    C.rd_xT = [("xT", t, half) for t in range(NT) for half in range(2)]
    C.m_xT = C.mark()
    C.xtm = [C.sb([128, 1024], F32) for _ in range(2)]
    C.fmgen = fm_gen(C, A["x"], C.xT, "xT", NT, C.xtm)
    next(C.fmgen)
    next(C.fmgen)
    C.rd_xT_tb = [[("xT", t, hf) for t in range(tb * 4, tb * 4 + 4) for hf in range(2)] for tb in range(4)]


def proj_tok_to_dram(C, W_dram, ncols, func, dst, col0):
    P = C.P
    P.label = "A1"
    m = C.mark()
    w = C.sb([128, 8, ncols], F32R)
    P.dma(w, W_dram, writes=["ptw"], eng="pool")
    ot = [C.sb([128, ncols], F32) for _ in range(2)]
    nb = ncols // 512
    for t in range(NT):
        b = t % 2
        next(C.fmgen, None)
        for j in range(nb):
            bk = 4 + (t % 2) * 2 + j
            for kc in range(8):
                P.op("pe", lambda e, bk=bk, kc=kc, t=t, j=j: e.matmul(C.bank(bk), C.xT[:, kc, t * 128:(t + 1) * 128], w[:, kc, j * 512:(j + 1) * 512],
                                                                    start=(kc == 0), stop=(kc == 7)),
                     reads=[("xT", t, kc // 4), "ptw"], writes=[("psum", bk)])
            P.op("act", lambda e, bk=bk, b=b, j=j: e.activation(ot[b][:, j * 512:(j + 1) * 512], C.bank(bk), func), reads=[("psum", bk)], writes=[("pto", b, j)])
        P.dma(dst[t * 128:(t + 1) * 128, col0:col0 + ncols], ot[b], reads=[("pto", b, j) for j in range(nb)])
    P.fence()
    C.release(m)


def phase_A1(C, A):
    P = C.P
    proj_tok_to_dram(C, A["w_mgb"], 1024, AF.Sigmoid, A["gates"], 1024)
    P.label = "A1"
    m = C.mark()
    wa = C.sb([128, 8, 1024], F32R)
    wz = C.sb([128, 8, 1024], F32R)
    P.dma(wa, A["w_mga"], writes=["wa"], eng="pool")
    P.dma(wz, A["w_z"], writes=["wz"], eng="pool")
    nwb = C.sb([128, 128], F32)
    P.dma(nwb, A["gdn_norm_w"].partition_broadcast(128), writes=["nwb"])
    ga_t = [C.sb([128, 1024], F32) for _ in range(2)]
    z_t = [C.sb([128, 1024], F32) for _ in range(2)]
    for t in range(NT):
        b = t % 2
        for wi_, (w_, wreg, func, dst, dreg) in enumerate(((wa, "wa", AF.Sigmoid, ga_t, "ga_t"), (wz, "wz", AF.Silu, z_t, "z_t"))):
            for j in range(2):
                bk = b * 4 + wi_ * 2 + j
                for kc in range(8):
                    P.op("pe", lambda e, bk=bk, kc=kc, t=t, j=j, w_=w_: e.matmul(C.bank(bk), C.xT[:, kc, t * 128:(t + 1) * 128], w_[:, kc, j * 512:(j + 1) * 512],
                                                                               start=(kc == 0), stop=(kc == 7)),
                         reads=[("xT", t, kc // 4), wreg], writes=[("psum", bk)])
                P.op("act", lambda e, bk=bk, b=b, j=j, func=func, dst=dst: e.activation(dst[b][:, j * 512:(j + 1) * 512], C.bank(bk), func),
                     reads=[("psum", bk)], writes=[(dreg, b, j)])
        P.op("pool", lambda e, b=b: e.tensor_tensor(z_t[b], z_t[b], ga_t[b], ALU.mult), reads=[("ga_t", b, 0), ("ga_t", b, 1), ("z_t", b, 0), ("z_t", b, 1)],
             writes=[("z_t", b, 0), ("z_t", b, 1)])
        P.op("pool", lambda e, b=b: e.tensor_tensor(z_t[b].rearrange("p (h d) -> p h d", h=8), z_t[b].rearrange("p (h d) -> p h d", h=8),
                                                   nwb.unsqueeze(1).to_broadcast([128, 8, 128]), ALU.mult), reads=[("z_t", b, 0), ("z_t", b, 1), "nwb"],
             writes=[("z_t", b, 0), ("z_t", b, 1)])
        P.dma(A["zs"][t * 128:(t + 1) * 128, :], z_t[b], reads=[("z_t", b, 0), ("z_t", b, 1)])
    C.release(C.m_xT)


def nsa_consts(C, A):
    P = C.P
    K = {}
    ident = C.ident
    K["R"] = C.sb([128, 128], F32R)
    K["cos"] = C.sb([128, S], F32)
    K["sin"] = C.sb([128, S], F32)
    K["cmask"] = C.sb([128, S], BF16)
    K["tri"] = C.sb([128, 128], BF16)
    K["wmask"] = C.sb([128, 3, 128], BF16)
    K["Eall"] = C.sb([32, 16, 128], BF16)
    K["M1"] = C.sb([128, 16, 32], F32)
    K["M2"] = C.sb([128, 16, 32], F32)
    K["ov"] = C.sb([128, 32], F32)
    K["zeros"] = C.sb([128, 32], F32)
    P.op("pool", lambda e: e.memset(K["zeros"], 0.0), writes=["zeros"])
    mtmp = C.mark()
    r32 = C.sb([128, 128], F32)
    P.op("pool", lambda e: e.memset(r32, 0.0), writes=["r32"])
    P.op("pool", lambda e: e.tensor_scalar(r32[64:128, 0:64], ident[64:128, 64:128], -1.0, None, ALU.mult), reads=["r32"], writes=["r32"])
    P.op("pool", lambda e: e.tensor_copy(r32[0:64, 64:128], ident[0:64, 0:64]), reads=["r32"], writes=["r32"])
    P.op("pool", lambda e: e.tensor_copy(K["R"], r32), reads=["r32"], writes=["R"])
    posi = C.sb([128, S], I32)
    P.dma(posi, A["positions"].partition_broadcast(128), writes=["posi"])
    invf = C.sb([128, 1], F32)
    P.dma(invf, A["inv_freq"], writes=["invf"])
    ang = C.sb([128, S], F32)
    u = C.sb([128, S], F32)
    ni = C.sb([128, S], I32)
    P.op("dve", lambda e: e.tensor_copy(ang, posi), reads=["posi"], writes=["ang"])
    P.op("dve", lambda e: e.tensor_scalar(ang, ang, invf, None, ALU.mult), reads=["ang", "invf"], writes=["ang"])
    for nm, shift in (("sin", 0.0), ("cos", np.pi / 2)):
        dst = K[nm]
        P.op("dve", lambda e, shift=shift: e.tensor_scalar(u, ang, shift, 1.0 / TWO_PI, ALU.add, ALU.mult), reads=["ang"], writes=["u"])
        P.op("dve", lambda e: e.tensor_copy(ni, u), reads=["u"], writes=["ni"])
        P.op("dve", lambda e: e.tensor_copy(u, ni), reads=["ni"], writes=["u"])
        P.op("dve", lambda e, dst=dst, shift=shift: e.tensor_scalar(dst, ang, shift, None, ALU.add), reads=["ang"], writes=[nm])
        P.op("dve", lambda e, dst=dst: e.scalar_tensor_tensor(dst, u, -RC1, dst, ALU.mult, ALU.add), reads=["u", nm], writes=[nm])
        P.op("dve", lambda e, dst=dst: e.scalar_tensor_tensor(dst, u, -RC2, dst, ALU.mult, ALU.add), reads=["u", nm], writes=[nm])
        P.op("dve", lambda e, dst=dst: e.tensor_scalar(dst, dst, -3.1415925, 3.1415925, ALU.max, ALU.min), reads=[nm], writes=[nm])
        P.op("act", lambda e, dst=dst: e.activation(dst, dst, AF.Sin), reads=[nm], writes=[nm])
    cm32 = C.sb([128, S], F32)
    P.op("pool", lambda e: e.memset(cm32, 1.0), writes=["cm32"])
    P.op("pool", lambda e: e.affine_select(out=cm32, in_=cm32, pattern=[[1, S]], compare_op=ALU.is_ge, fill=0.0, base=-31, channel_multiplier=-16),
         reads=["cm32"], writes=["cm32"])
    P.op("pool", lambda e: e.tensor_copy(K["cmask"], cm32), reads=["cm32"], writes=["cmask"])
    tri32 = C.sb([128, 128], F32)
    P.op("pool", lambda e: e.memset(tri32, 1.0), writes=["tri32"])
    P.op("pool", lambda e: e.affine_select(out=tri32, in_=tri32, pattern=[[1, 128]], compare_op=ALU.is_ge, fill=0.0, base=0, channel_multiplier=-1),
         reads=["tri32"], writes=["tri32"])
    P.op("pool", lambda e: e.tensor_copy(K["tri"], tri32), reads=["tri32"], writes=["tri"])
    P.op("pool", lambda e: e.tensor_scalar(K["wmask"][:, 0, :], tri32, -1.0, 1.0, ALU.mult, ALU.add), reads=["tri32"], writes=["wmask"])
    P.op("pool", lambda e: e.memset(K["wmask"][:, 1, :], 1.0), reads=["wmask"], writes=["wmask"])
    P.op("pool", lambda e: e.tensor_copy(K["wmask"][:, 2, :], tri32), reads=["wmask", "tri32"], writes=["wmask"])
    e32 = C.sb([32, 16, 2, 64], F32)
    P.op("pool", lambda e: e.memset(e32, 0.0), writes=["e32"])
    P.op("pool", lambda e: e.affine_select(out=e32, in_=e32, pattern=[[-2, 16], [-1, 2], [0, 64]], compare_op=ALU.not_equal, fill=1.0, base=0, channel_multiplier=1),
         reads=["e32"], writes=["e32"])
    P.op("pool", lambda e: e.tensor_copy(K["Eall"], e32.rearrange("p a b c -> p a (b c)")), reads=["e32"], writes=["Eall"])
    dI = C.sb([128, 16, 32], F32)
    P.op("pool", lambda e: e.iota(dI[0:64], pattern=[[-2, 16], [1, 32]], base=0, channel_multiplier=0, allow_small_or_imprecise_dtypes=True), writes=["dI"])
    P.op("pool", lambda e: e.iota(dI[64:128], pattern=[[-2, 16], [1, 32]], base=-1, channel_multiplier=0, allow_small_or_imprecise_dtypes=True), reads=["dI"], writes=["dI"])
    f1 = C.sb([128, 16, 32], F32)
    f2 = C.sb([128, 16, 32], F32)
    P.op("dve", lambda e: e.tensor_scalar(f1, dI, 0.0, None, ALU.is_equal), reads=["dI"], writes=["f1"])
    P.op("dve", lambda e: e.tensor_scalar(f2, dI, -1.0, None, ALU.is_equal), reads=["dI"], writes=["f2"])
    P.op("dve", lambda e: e.tensor_tensor(f1, f1, f2, ALU.max), reads=["f1", "f2"], writes=["f1"])
    P.op("dve", lambda e: e.memset(f1[:, :, 0:1], 1.0), reads=["f1"], writes=["f1"])
    P.op("dve", lambda e: e.tensor_scalar(f2, dI, 0.0, None, ALU.is_gt), reads=["dI", "f1"], writes=["f2"])
    P.op("dve", lambda e: e.tensor_tensor(K["M2"], f1, f2, ALU.subtract), reads=["f1", "f2"], writes=["M2"])
    P.op("dve", lambda e: e.tensor_tensor(K["M1"], f1, f2, ALU.add), reads=["f1", "f2"], writes=["M1"])
    P.op("dve", lambda e: e.tensor_scalar(K["M1"], K["M1"], -1.0, 1.0, ALU.mult, ALU.add), reads=["M1"], writes=["M1"])
    P.op("dve", lambda e: e.tensor_scalar(K["M2"], K["M2"], BIGM, None, ALU.mult), reads=["M2"], writes=["M2"])
    a1 = C.sb([128, 32], F32)
    a2 = C.sb([128, 32], F32)
    ov = K["ov"]
    P.op("pool", lambda e: e.iota(a1, pattern=[[0, 32]], base=32, channel_multiplier=16, allow_small_or_imprecise_dtypes=True), writes=["a1"])
    P.op("pool", lambda e: e.iota(a2, pattern=[[64, 32]], base=64, channel_multiplier=0, allow_small_or_imprecise_dtypes=True), writes=["a2"])
    P.op("dve", lambda e: e.tensor_tensor(ov, a1, a2, ALU.min), reads=["a1", "a2"], writes=["ov"])
    P.op("dve", lambda e: e.tensor_scalar(a1, a1, -32.0, None, ALU.add), reads=["a1", "ov"], writes=["a1"])
    P.op("dve", lambda e: e.tensor_scalar(a2, a2, -64.0, None, ALU.add), reads=["a2", "ov"], writes=["a2"])
    P.op("dve", lambda e: e.tensor_tensor(a1, a1, a2, ALU.max), reads=["a1", "a2"], writes=["a1"])
    P.op("dve", lambda e: e.tensor_tensor(ov, ov, a1, ALU.subtract), reads=["ov", "a1"], writes=["ov"])
    P.op("dve", lambda e: e.tensor_scalar(ov, ov, 0.0, 1.0 / 32.0, ALU.max, ALU.mult), reads=["ov"], writes=["ov"])
    C.dbg("cos", K["cos"], ["cos"])
    C.dbg("sin", K["sin"], ["sin"])
    C.dbg("M1", K["M1"], ["M1"])
    C.dbg("M2", K["M2"], ["M2"])
    C.dbg("ov", ov, ["ov"])
    P.fence()
    C.release(mtmp)
    return K


def proj_feat(C, w, wreg, col0, dst_fn, rope=None, K=None, tag="pf"):
    P = C.P
    pend = None
    for tb in range(4):
        bk = tb % 2
        for kc in range(8):
            P.op("pe", lambda e, bk=bk, kc=kc, tb=tb: e.matmul(C.bank(bk), w[:, kc, col0:col0 + 128], C.xT[:, kc, tb * 512:(tb + 1) * 512],
                                                             start=(kc == 0), stop=(kc == 7)),
                 reads=[wreg] + C.rd_xT_tb[tb], writes=[("psum", bk)])
        out, oreg = dst_fn(tb)
        if rope is None:
            copy_op(P, C.evac_eng(), out, C.bank(bk), reads=[("psum", bk)], writes=oreg)
        else:
            raw, t1, t2 = rope
            b = tb % 2
            P.op("act", lambda e, bk=bk, b=b: e.copy(raw[b], C.bank(bk)), reads=[("psum", bk)], writes=[("rraw", b)])

            def epi(bk=bk, b=b, tb=tb, out=out, oreg=oreg):
                P.op("pe", lambda e: e.matmul(C.bank(2 + bk), K["R"], raw[b], start=True, stop=True), reads=["R", ("rraw", b)], writes=[("psum", 2 + bk)])
                P.op("pool", lambda e: e.tensor_tensor(t1[b], raw[b].bitcast(F32), K["cos"][:, tb * 512:(tb + 1) * 512], ALU.mult),
                     reads=[("rraw", b), "cos"], writes=[("rt1", b)])
                P.op("dve", lambda e: e.tensor_tensor(t2[b], C.bank(2 + bk), K["sin"][:, tb * 512:(tb + 1) * 512], ALU.mult),
                     reads=[("psum", 2 + bk), "sin"], writes=[("rt2", b)])
                P.op("dve", lambda e: e.tensor_tensor(out, t1[b], t2[b], ALU.add), reads=[("rt1", b), ("rt2", b)], writes=oreg)

            if pend is not None:
                pend()
            pend = epi
    if pend is not None:
        pend()


def gelu_tanh(C, out, in_psum, bias, tmp, rd, wr, tag):
    P = C.P
    x, x2 = tmp
    c2 = 2.0 * 0.7978845608028654
    P.op("act", lambda e: e.activation(x, in_psum, AF.Identity, bias=bias), reads=rd, writes=[(tag, "x")])
    P.op("dve", lambda e: e.tensor_tensor(x2, x, x, ALU.mult), reads=[(tag, "x")], writes=[(tag, "x2")])
    P.op("dve", lambda e: e.tensor_scalar(x2, x2, 0.044715 * c2, c2, ALU.mult, ALU.add), reads=[(tag, "x2")], writes=[(tag, "x2")])
    P.op("dve", lambda e: e.tensor_tensor(x2, x2, x, ALU.mult), reads=[(tag, "x2"), (tag, "x")], writes=[(tag, "x2")])
    P.op("act", lambda e: e.activation(x2, x2, AF.Sigmoid), reads=[(tag, "x2")], writes=[(tag, "x2")])
    P.op("dve", lambda e: e.tensor_tensor(out, x2, x, ALU.mult), reads=[(tag, "x2"), (tag, "x")], writes=wr)


def phase_A2(C, A):
    P = C.P
    P.label = "A2c"
    P.fence()
    m0 = C.mark()
    K = nsa_consts(C, A)
    ng = C.sb([128, NT, 24], F32)
    wng = C.sb([128, 8, 24], F32R)
    P.dma(wng, A["w_ng"], writes=["wng"], eng="pool")
    for t in range(NT):
        for kc in range(8):
            P.op("pe", lambda e, kc=kc, t=t: e.matmul(C.bank(7)[:, t * 24:(t + 1) * 24], C.xT[:, kc, t * 128:(t + 1) * 128], wng[:, kc, :],
                                                    start=(kc == 0), stop=(kc == 7)),
                 reads=[("xT", t, kc // 4), "wng"], writes=[("psum", 7)])
    P.op("act", lambda e: e.activation(ng.rearrange("p t c -> p (t c)"), C.bank(7)[:, 0:NT * 24], AF.Sigmoid), reads=[("psum", 7)], writes=["ng"])
    wbuf = C.sb([128, 8, 512], F32R)
    vcx = C.sb([128, 162], F32R)
    vcx32 = C.sb([128, 162], F32)
    kcc = C.sb([128, 128], F32R)
    raw = [C.sb([128, 512], F32R) for _ in range(2)]
    t1 = [C.sb([128, 512], F32) for _ in range(2)]
    t2 = [C.sb([128, 512], F32) for _ in range(2)]
    rope = (raw, t1, t2)
    m1 = C.mark()
    def nsa_group(g):
        P.label = "A2s1"
        P.fence()
        C.release(m1)
        ms = C.mark()
        xk = C.sb([128, S], F32)
        xv = C.sb([128, S], F32)
        P.dma(wbuf[:, :, 0:256], A["w_nsa_c"][g], writes=[("wbuf", 0)], eng="pool")
        P.dma(wbuf[:, :, 256:512], A["w_nsa_k"][g], writes=[("wbuf", 1)], eng="pool")
        proj_feat(C, wbuf, ("wbuf", 0), 0, lambda tb: (xk[:, tb * 512:(tb + 1) * 512], [("xk", tb)]), rope=rope, K=K)
        proj_feat(C, wbuf, ("wbuf", 0), 128, lambda tb: (xv[:, tb * 512:(tb + 1) * 512], [("xv", tb)]))
        pe_sb = C.sb([32, 2, 128], F32)
        P.dma(pe_sb, A["cmp_pe"].rearrange("j l d -> l j d"), writes=["pe_sb"])
        peT = C.sb([128, 2, 32], F32)
        for j in range(2):
            P.op("pe", lambda e, j=j: e.transpose(C.bank(6)[:, j * 32:(j + 1) * 32], pe_sb[:, j, :], C.ident[0:32, 0:32]), reads=["pe_sb", "ident"], writes=[("psum", 6)])
        P.op("dve", lambda e: e.tensor_copy(peT.rearrange("p j l -> p (j l)"), C.bank(6)[:, 0:64]), reads=[("psum", 6)], writes=["peT"])
        b1 = C.sb([128, 2, 2], F32)
        P.dma(b1, A["cmp_b1"], writes=["b1"])
        w2 = C.sb([128, 2, 2, 128], F32R)
        P.dma(w2, A["cmp_w2"].rearrange("j (c p) d -> p j c d", p=128), writes=["w2"], eng="pool")
        xim = C.sb([128, 32, 128], F32R)
        P.op("pool", lambda e: e.tensor_copy(xim[:, :, 127:128], K["zeros"].unsqueeze(2)), reads=["zeros"], writes=[("xim", l) for l in range(32)])
        w1c = [C.sb([128, 8, 256], F32R) for _ in range(2)]
        hid = C.sb([128, 2, 128], F32R)
        gx = (C.sb([128, 128], F32), C.sb([128, 128], F32))
        for j, src in ((0, xk), (1, xv)):
            for l in range(32):
                P.op("pool" if l % 2 else "dve", lambda e, l=l, j=j, src=src: e.tensor_scalar(xim[:, l, 0:127], src[:, l:l + 16 * 126 + 1:16], peT[:, j, l:l + 1], None, ALU.add),
                     reads=[("xk" if j == 0 else "xv", tb) for tb in range(4)] + ["peT", ("xim", l)], writes=[("xim", l)])
            for ch in range(4):
                wb = ch % 2
                P.dma(w1c[wb], A["cmp_w1"][j, :, ch * 8:(ch + 1) * 8, :], writes=[("w1c", wb)], eng="pool")
                for ll in range(8):
                    l = ch * 8 + ll
                    for fc in range(2):
                        P.op("pe", lambda e, l=l, ll=ll, fc=fc, wb=wb: e.matmul(C.bank(4 + fc)[:, 0:128], w1c[wb][:, ll, fc * 128:(fc + 1) * 128], xim[:, l, :],
                                                                              start=(l == 0), stop=(l == 31), skip_group_check=True),
                             reads=[("w1c", wb), ("xim", l)], writes=[("psum", 4 + fc)])
            for fc in range(2):
                gelu_tanh(C, hid[:, fc, :], C.bank(4 + fc)[:, 0:128], b1[:, j, fc:fc + 1], gx, [("psum", 4 + fc), "b1"], [("hid", fc)], "gl")
            if j == 0:
                for fc in range(2):
                    P.op("pe", lambda e, fc=fc: e.matmul(C.bank(6)[:, 0:128], w2[:, 0, fc, :], hid[:, fc, :], start=(fc == 0), stop=(fc == 1)),
                         reads=["w2", ("hid", fc)], writes=[("psum", 6)])
                P.op("act", lambda e: e.copy(kcc, C.bank(6)[:, 0:128]), reads=[("psum", 6)], writes=["kcc"])
            else:
                for fc in range(2):
                    P.op("pe", lambda e, fc=fc: e.matmul(C.bank(6)[:, 0:128], hid[:, fc, :], w2[:, 1, fc, :], start=(fc == 0), stop=(fc == 1)),
                         reads=["w2", ("hid", fc)], writes=[("psum", 6)])
                P.op("pool", lambda e: e.memset(vcx32, 0.0), writes=["vcx32"])
                P.op("act", lambda e: e.copy(vcx32[:, 0:128], C.bank(6)[:, 0:128]), reads=[("psum", 6), "vcx32"], writes=["vcx32"])
                P.op("pool", lambda e: e.memset(vcx32[:, 128:129], 1.0), reads=["vcx32"], writes=["vcx32"])
                P.op("pool", lambda e: e.tensor_copy(vcx32[:, 129:161], K["ov"]), reads=["vcx32", "ov"], writes=["vcx32"])
                P.op("pool", lambda e: e.tensor_copy(vcx, vcx32), reads=["vcx32"], writes=["vcx"])
        C.dbg("kcc%d" % g, kcc, ["kcc"])
        C.dbg("vcx%d" % g, vcx, ["vcx"])
        P.label = "A2s2"
        P.fence()
        C.release(ms)
        qTb = C.sb([128, 4, S], BF16)
        ksT = C.sb([128, S], BF16)
        kwT = C.sb([128, S], BF16)
        vs = C.sb([128, NT, 130], BF16)
        vw = C.sb([128, NT, 130], BF16)
        impacc = C.sb([128, NT, 32], F32)
        selT = C.sb([32, S], BF16)
        ycs = [C.sb([128, 2, 128], F32) for _ in range(2)]
        ybt = [C.sb([128, 512], F32) for _ in range(2)]
        gbt = [C.sb([128, 512], F32) for _ in range(2)]
        qf = [C.sb([128, S], F32R) for _ in range(2)]
        ec = [C.sb([128, 512], F32) for _ in range(2)]
        ecr = [C.sb([128, 512], F32R) for _ in range(2)]
        rdn = C.sb([128, 8], F32)
        wgt = C.sb([128, 8], F32)
        itmp = C.sb([128, 2, 32], F32)
        P.dma(wbuf[:, :, 0:256], A["w_nsa_v"][g], writes=[("wbuf", 0)], eng="pool")
        proj_feat(C, wbuf, ("wbuf", 1), 256, lambda tb: (ksT[:, tb * 512:(tb + 1) * 512], [("ksT", tb)]), rope=rope, K=K)
        proj_feat(C, wbuf, ("wbuf", 1), 384, lambda tb: (kwT[:, tb * 512:(tb + 1) * 512], [("kwT", tb)]), rope=rope, K=K)
        P.op("pool", lambda e: e.memset(vs[:, :, 128:130], 1.0), writes=[("vs1")])
        P.op("pool", lambda e: e.memset(vw[:, :, 128:130], 1.0), writes=[("vw1")])
        for t in range(NT):
            bk = t % 2
            for kc in range(8):
                P.op("pe", lambda e, bk=bk, kc=kc, t=t: e.matmul(C.bank(bk)[:, 0:256], C.xT[:, kc, t * 128:(t + 1) * 128], wbuf[:, kc, 0:256], start=(kc == 0), stop=(kc == 7)),
                     reads=[("xT", t, kc // 4), ("wbuf", 0)], writes=[("psum", bk)])
            P.op("act", lambda e, bk=bk, t=t: e.copy(vs[:, t, 0:128], C.bank(bk)[:, 0:128]), reads=[("psum", bk)], writes=[("vs", t)])
            P.op("dve", lambda e, bk=bk, t=t: e.tensor_copy(vw[:, t, 0:128], C.bank(bk)[:, 128:256]), reads=[("psum", bk)], writes=[("vw", t)])
        P.dma(wbuf, A["w_nsa_q"][g], writes=[("wbuf", 0), ("wbuf", 1)], eng="pool")
        rd_ks = [("ksT", tb) for tb in range(4)]
        rd_kw = [("kwT", tb) for tb in range(4)]
        for hh in range(4):
            h = g * 4 + hh
            qb = hh % 2
            proj_feat(C, wbuf, ("wbuf", hh // 2), hh * 128, lambda tb, qb=qb: (qf[qb][:, tb * 512:(tb + 1) * 512], [("qf", qb, tb)]), rope=rope, K=K)
            for tb in range(4):
                P.op("act", lambda e, qb=qb, hh=hh, tb=tb: e.copy(qTb[:, hh, tb * 512:(tb + 1) * 512], qf[qb][:, tb * 512:(tb + 1) * 512]),
                     reads=[("qf", qb, tb)], writes=[("qTb", hh, tb)])
            def cmp_a(tb, qb=qb):
                eb = tb % 2
                P.op("pe", lambda e: e.matmul(C.bank(4)[0:127, :], kcc[:, 0:127], qf[qb][:, tb * 512:(tb + 1) * 512], start=True, stop=True),
                     reads=["kcc", ("qf", qb, tb)], writes=[("psum", 4)])
                P.op("act", lambda e: e.activation(ec[eb][0:127, :], C.bank(4)[0:127, :], AF.Exp, scale=SCALE_DH), reads=[("psum", 4)], writes=[("ec", eb)])
                P.op("pool", lambda e: e.tensor_tensor(ecr[eb][0:127, :], ec[eb][0:127, :], K["cmask"][0:127, tb * 512:(tb + 1) * 512], ALU.mult),
                     reads=[("ec", eb), "cmask"], writes=[("ecr", eb)])

            cmp_a(0)
            for tb in range(4):
                eb = tb % 2
                if tb + 1 < 4:
                    cmp_a(tb + 1)
                for pr in range(2):
                    bk = 5 + pr
                    for q2 in range(2):
                        tt = pr * 2 + q2
                        P.op("pe", lambda e, bk=bk, q2=q2, tt=tt, eb=eb: e.matmul(C.bank(bk)[:, q2 * 162:(q2 + 1) * 162], ecr[eb][0:127, tt * 128:(tt + 1) * 128], vcx[0:127, :],
                                                                                start=True, stop=True, skip_group_check=True),
                             reads=[("ecr", eb), "vcx"], writes=[("psum", bk)])
                    t0 = tb * 4 + pr * 2
                    pv = C.bank(bk)[:, 0:324].rearrange("p (a c) -> p a c", a=2)
                    P.op("dve", lambda e, pv=pv: e.tensor_scalar(rdn[:, 0:2], pv[:, :, 128], 1e-30, None, ALU.max), reads=[("psum", bk)], writes=["rdn"])
                    P.op("dve", lambda e: e.reciprocal(rdn[:, 0:2], rdn[:, 0:2]), reads=["rdn"], writes=["rdn"])
                    P.op("dve", lambda e, t0=t0, h=h: e.tensor_tensor(wgt[:, 0:2], rdn[:, 0:2], ng[:, t0:t0 + 2, h * 3], ALU.mult), reads=["rdn", "ng"], writes=["wgt"])
                    P.op("dve", lambda e, pv=pv, pr=pr: e.tensor_tensor(ycs[pr], pv[:, :, 0:128], wgt[:, 0:2].unsqueeze(2).to_broadcast([128, 2, 128]), ALU.mult),
                         reads=[("psum", bk), "wgt"], writes=[("ycs", pr)])
                    P.dma(A["yb"][t0 * 128:(t0 + 2) * 128, h * 128:(h + 1) * 128].rearrange("(a p) c -> p a c", p=128), ycs[pr],
                          reads=[("ycs", pr)], writes=[("ybd", t0, hh), ("ybd", t0 + 1, hh)])
                    if hh == 0:
                        P.op("dve", lambda e, pv=pv, t0=t0: e.tensor_tensor(impacc[:, t0:t0 + 2, :], pv[:, :, 129:161], rdn[:, 0:2].unsqueeze(2).to_broadcast([128, 2, 32]), ALU.mult),
                             reads=[("psum", bk), "rdn"], writes=[("imp", t0)])
                    else:
                        P.op("dve", lambda e, pv=pv: e.tensor_tensor(itmp, pv[:, :, 129:161], rdn[:, 0:2].unsqueeze(2).to_broadcast([128, 2, 32]), ALU.mult),
                             reads=[("psum", bk), "rdn"], writes=["itmp"])
                        P.op("pool", lambda e, t0=t0: e.tensor_tensor(impacc[:, t0:t0 + 2, :], impacc[:, t0:t0 + 2, :], itmp, ALU.add), reads=["itmp", ("imp", t0)], writes=[("imp", t0)])
        C.dbg("imp%d" % g, impacc, [("imp", t0) for t0 in range(0, NT, 2)])
        P.label = "A2s3"
        rd_imp = [("imp", t0) for t0 in range(0, NT, 2)]
        P.op("dve", lambda e: e.tensor_tensor(impacc, impacc, K["M1"], ALU.mult), reads=rd_imp + ["M1"], writes=rd_imp)
        P.op("dve", lambda e: e.tensor_tensor(impacc, impacc, K["M2"], ALU.add), reads=rd_imp + ["M2"], writes=rd_imp)
        mx8 = C.sb([128, 8], F32)
        sel32 = C.sb([128, 32], F32)
        for t in range(NT):
            P.op("dve", lambda e, t=t: e.max(mx8, impacc[:, t, :]), reads=rd_imp, writes=["mx8"])
            P.op("dve", lambda e, t=t: e.tensor_scalar(sel32, impacc[:, t, :], mx8[:, 7:8], None, ALU.is_ge), reads=rd_imp + ["mx8"], writes=["sel32"])
            P.op("pe", lambda e, t=t: e.transpose(C.bank(4)[0:32, (t % 4) * 128:(t % 4 + 1) * 128], sel32, C.ident), reads=["sel32", "ident"], writes=[("psum", 4)])
            if t % 4 == 3:
                P.op("act", lambda e, t=t: e.copy(selT[:, (t - 3) * 128:(t + 1) * 128], C.bank(4)[0:32, :]), reads=[("psum", 4)], writes=[("selT", t // 4)])
        C.dbg("selT%d" % g, selT, [("selT", i) for i in range(4)])
        P.label = "A2s4"
        msk = [C.sb([128, 4, 128], BF16) for _ in range(3)]
        state = {"u": 0, "q": [], "mi": 0}
        eb16 = [C.sb([128, 4, 128], BF16) for _ in range(3)]

        def flush():
            q = state["q"]
            if q and not q[-1][0]:
                q[-1][1]()
                q[-1][0] = True
            for ent in q:
                for f in ent[2]:
                    f()
            state["q"] = []

        def unit(kts, kT_sb, kreg, v_sb, vreg, hh, tt, mask_ap, mask_regs, out_bank_ap, out_reg, first, last, post_fn):
            u = state["u"]
            state["u"] += 1
            bk = u % 2
            eb = u % 3
            n = len(kts)
            for jj, kt in enumerate(kts):
                P.op("pe", lambda e, jj=jj, kt=kt: e.matmul(C.bank(bk)[:, jj * 128:(jj + 1) * 128], kT_sb[:, kt * 128:(kt + 1) * 128],
                                                          qTb[:, hh, tt * 128:(tt + 1) * 128], start=True, stop=True, skip_group_check=True),
                     reads=[(kreg, kt // 4), ("qTb", hh, tt // 4)], writes=[("psum", bk)])
            q = state["q"]
            if q and not q[-1][0]:
                q[-1][1]()
                q[-1][0] = True
            if len(q) >= 2:
                ent = q.pop(0)
                for f in ent[2]:
                    f()

            def mid():
                P.op("act", lambda e: e.activation(eb16[eb][:, 0:n, :], C.bank(bk)[:, 0:n * 128].rearrange("p (a c) -> p a c", a=n), AF.Exp, scale=SCALE_DH),
                     reads=[("psum", bk)], writes=[("eb16", eb)])
                P.op("dve" if u % 2 == 0 else "pool", lambda e: e.tensor_tensor(eb16[eb][:, 0:n, :], eb16[eb][:, 0:n, :], mask_ap, ALU.mult),
                     reads=[("eb16", eb)] + mask_regs, writes=[("eb16", eb)])

            def pv():
                for jj, kt in enumerate(kts):
                    P.op("pe", lambda e, jj=jj, kt=kt: e.matmul(out_bank_ap, eb16[eb][:, jj, :], v_sb[:, kt, :], start=(first and jj == 0), stop=(last and jj == n - 1),
                                                              skip_group_check=True),
                         reads=[("eb16", eb), (vreg, kt), vreg + "1"], writes=[out_reg])

            state["q"].append([False, mid, [pv] + ([post_fn] if post_fn is not None else [])])

        def make_mask(tt, gi):
            kk = [kt for kt in range(gi * 4, gi * 4 + 4) if kt <= tt]
            n = len(kk)
            mb = state["mi"] % 3
            state["mi"] += 1
            for jj, kt in enumerate(kk):
                P.op("pe", lambda e, jj=jj, kt=kt: e.matmul(C.bank(6)[:, jj * 128:(jj + 1) * 128], K["Eall"][:, kt, :], selT[:, tt * 128:(tt + 1) * 128],
                                                          start=True, stop=True, skip_group_check=True),
                     reads=["Eall", ("selT", tt // 4)], writes=[("psum", 6)])
            P.op("act", lambda e: e.copy(msk[mb][:, 0:n, :], C.bank(6)[:, 0:n * 128].rearrange("p (a c) -> p a c", a=n)), reads=[("psum", 6)], writes=[("msk", mb)])
            if kk[-1] == tt:
                P.op("pool", lambda e: e.tensor_tensor(msk[mb][:, n - 1, :], msk[mb][:, n - 1, :], K["tri"], ALU.mult), reads=[("msk", mb), "tri"], writes=[("msk", mb)])
            return kk, mb

        for tt in range(NT):
            ngrp = tt // 4 + 1
            yb_b = tt % 2
            ybreg = "ybt%d" % yb_b
            ybt_b = ybt[yb_b]
            P.dma(ybt_b, A["yb"][tt * 128:(tt + 1) * 128, g * 512:(g + 1) * 512], reads=[("ybd", tt, hh) for hh in range(4)], writes=[(ybreg, hh) for hh in range(4)])
            P.dma(gbt[yb_b], A["gates"][tt * 128:(tt + 1) * 128, 1024 + g * 512:1024 + (g + 1) * 512], writes=[("gbt", yb_b)])
            nxt = make_mask(tt, 0)
            kts = [kt for kt in (tt - 2, tt - 1, tt) if kt >= 0]
            j0 = 3 - len(kts)
            for hh in range(4):
                ob = 2 + hh % 2
                pf = (lambda hh=hh, ob=ob, ybt_b=ybt_b, ybreg=ybreg, tt=tt: nsa_post(C, P, C.bank(ob), ("psum", ob), ybt_b, ybreg, ng, tt, hh, g * 4 + hh, 2, rdn, wgt))
                unit(kts, kwT, "kwT", vw, "vw", hh, tt, K["wmask"][:, j0:3, :], ["wmask"], C.bank(ob)[:, 0:130], ("psum", ob), True, True, pf)
            for gi in range(ngrp):
                kk, mb = nxt
                if gi + 1 < ngrp:
                    nxt = make_mask(tt, gi + 1)
                for hh in range(4):
                    ob = (2, 3, 5, 7)[hh]
                    lastg = (gi == ngrp - 1)
                    pf = None
                    if lastg:
                        pf = (lambda hh=hh, ob=ob, ybt_b=ybt_b, ybreg=ybreg, tt=tt: nsa_post(C, P, C.bank(ob)[:, 256:512], ("psum", ob), ybt_b, ybreg, ng, tt, hh, g * 4 + hh, 1, rdn, wgt))
                    unit(kk, ksT, "ksT", vs, "vs", hh, tt, msk[mb][:, 0:len(kk), :], [("msk", mb)], C.bank(ob)[:, 256:386], ("psum", ob), gi == 0, lastg, pf)
            def tail(tt=tt, ybt_b=ybt_b, ybreg=ybreg, yb_b=yb_b):
                P.op("pool", lambda e, gbt_b=gbt[yb_b]: e.tensor_tensor(ybt_b, ybt_b, gbt_b, ALU.mult),
                     reads=[(ybreg, hh) for hh in range(4)] + [("gbt", yb_b)], writes=[(ybreg, hh) for hh in range(4)])
                P.dma(A["yb"][tt * 128:(tt + 1) * 128, g * 512:(g + 1) * 512], ybt_b, reads=[(ybreg, hh) for hh in range(4)], writes=[("ybd", tt, hh) for hh in range(4)])

            state["q"][-1][2].append(tail)
        flush()

    for g in range(2):
        nsa_group(g)
    P.fence()
    C.release(m0)


def nsa_post(C, P, ps, psreg, ybt, ybreg, ng, tt, hh, h, br, rdn, wgt):
    c = 2 + br
    P.op("dve", lambda e: e.tensor_scalar(rdn[:, c:c + 1], ps[:, 128:129], 1e-30, None, ALU.max), reads=[psreg], writes=[("rdn", c)])
    P.op("dve", lambda e: e.reciprocal(rdn[:, c:c + 1], rdn[:, c:c + 1]), reads=[("rdn", c)], writes=[("rdn", c)])
    P.op("dve", lambda e: e.tensor_tensor(wgt[:, c:c + 1], rdn[:, c:c + 1], ng[:, tt, h * 3 + br:h * 3 + br + 1], ALU.mult), reads=[("rdn", c), "ng"], writes=[("wgt", c)])
    P.op("dve", lambda e: e.scalar_tensor_tensor(ybt[:, hh * 128:(hh + 1) * 128], ps[:, 0:128], wgt[:, c:c + 1], ybt[:, hh * 128:(hh + 1) * 128], ALU.mult, ALU.add),
         reads=[psreg, ("wgt", c), (ybreg, hh)], writes=[(ybreg, hh)])


LN_QSCALE = float(np.log(128.0 ** -0.5))


def phase_A3(C, A):
    P = C.P
    P.label = "A3set"
    P.fence()
    m0 = C.mark()
    ident = C.ident
    xT = C.xT
    U64 = C.sb([64, 64], F32)
    SU64 = C.sb([64, 64], F32)
    mS = C.sb([64, 64], F32)
    ones64 = C.sb([64, 128], F32)
    one1 = C.sb([128, 1], F32)
    epsr = C.sb([128, 1], F32)
    lnq = C.sb([128, 1], F32)
    zer1 = C.sb([128, 1], F32)
    zeros = C.sb([128, 32], F32)
    P.op("pool", lambda e: e.memset(U64, 1.0), writes=["U64"])
    P.op("pool", lambda e: e.affine_select(out=U64, in_=U64, pattern=[[1, 64]], compare_op=ALU.is_ge, fill=0.0, base=0, channel_multiplier=-1), reads=["U64"], writes=["U64"])
    P.op("pool", lambda e: e.tensor_scalar(SU64, U64, -1.0, 1.0, ALU.mult, ALU.add), reads=["U64"], writes=["SU64"])
    P.op("pool", lambda e: e.memset(mS, 1.0), writes=["mS"])
    P.op("pool", lambda e: e.affine_select(out=mS, in_=mS, pattern=[[1, 64]], compare_op=ALU.is_ge, fill=0.0, base=-1, channel_multiplier=-1), reads=["mS"], writes=["mS"])
    P.op("pool", lambda e: e.memset(ones64, 1.0), writes=["ones64"])
    P.op("pool", lambda e: e.memset(one1, 1.0), writes=["one1"])
    P.op("pool", lambda e: e.memset(epsr, RMS_EPS), writes=["epsr"])
    P.op("pool", lambda e: e.memset(lnq, LN_QSCALE), writes=["lnq"])
    P.op("pool", lambda e: e.memset(zer1, 0.0), writes=["zer1"])
    P.op("pool", lambda e: e.memset(zeros, 0.0), writes=["zeros"])
    wgb = C.sb([128, 8, 16], F32R)
    P.dma(wgb, A["w_gb"], writes=["wgb"], eng="pool")
    for n in range(32):
        for kc in range(8):
            P.op("pe", lambda e, n=n, kc=kc: e.matmul(C.bank(0)[0:64, n * 16:(n + 1) * 16], xT[:, kc, n * 64:(n + 1) * 64], wgb[:, kc, :], start=(kc == 0), stop=(kc == 7)),
                 reads=[("xT", n // 2, kc // 4), "wgb"], writes=[("psum", 0)])
    gb = C.sb([64, 32, 16], F32)
    P.op("dve", lambda e: e.tensor_copy(gb.rearrange("p n c -> p (n c)"), C.bank(0)[0:64, :]), reads=[("psum", 0)], writes=["gb"])
    beta = C.sb([64, 32, 8], F32)
    negb = C.sb([64, 32, 8], F32)
    gg = C.sb([64, 32, 8], F32)
    egc = C.sb([64, 32, 8], F32)
    ekd = C.sb([64, 32, 8], F32)
    eglb = C.sb([128, 32, 8], F32)
    dtb = C.sb([64, 8], F32)
    nea = C.sb([64, 8], F32)
    nw = C.sb([64, 128], F32)
    P.dma(dtb, A["gdn_dt_bias"].partition_broadcast(64), writes=["dtb"])
    P.dma(nea, A["gdn_a_log"].partition_broadcast(64), writes=["nea"])
    P.dma(nw, A["gdn_norm_w"].partition_broadcast(64), writes=["nw"])
    P.op("act", lambda e: e.activation(beta, gb[:, :, 0:8], AF.Sigmoid), reads=["gb"], writes=["beta"])
    P.op("dve", lambda e: e.tensor_scalar(negb, beta, -1.0, None, ALU.mult), reads=["beta"], writes=["negb"])
    P.op("act", lambda e: e.activation(nea, nea, AF.Exp), reads=["nea"], writes=["nea"])
    P.op("dve", lambda e: e.tensor_scalar(nea, nea, -1.0, None, ALU.mult), reads=["nea"], writes=["nea"])
    P.op("dve", lambda e: e.tensor_tensor(gg, gb[:, :, 8:16], dtb.unsqueeze(1).to_broadcast([64, 32, 8]), ALU.add), reads=["gb", "dtb"], writes=["gg"])
    P.op("act", lambda e: e.activation(gg, gg, AF.Exp), reads=["gg"], writes=["gg"])
    P.op("act", lambda e: e.activation(gg, gg, AF.Ln, bias=one1[0:64]), reads=["gg", "one1"], writes=["gg"])
    P.op("dve", lambda e: e.tensor_tensor(gg, gg, nea.unsqueeze(1).to_broadcast([64, 32, 8]), ALU.mult), reads=["gg", "nea"], writes=["gg"])
    ggf = gg.rearrange("p n h -> p (n h)")
    P.op("pe", lambda e: e.matmul(C.bank(1)[0:64, 0:256], U64, ggf, start=True, stop=True), reads=["U64", "gg"], writes=[("psum", 1)])
    P.op("pe", lambda e: e.matmul(C.bank(2)[0:64, 0:256], SU64, ggf, start=True, stop=True), reads=["SU64", "gg"], writes=[("psum", 2)])
    P.op("pe", lambda e: e.matmul(C.bank(3)[:, 0:256], ones64, ggf, start=True, stop=True), reads=["ones64", "gg"], writes=[("psum", 3)])
    P.op("act", lambda e: e.activation(egc.rearrange("p n h -> p (n h)"), C.bank(1)[0:64, 0:256], AF.Exp), reads=[("psum", 1)], writes=["egc"])
    P.op("act", lambda e: e.activation(ekd.rearrange("p n h -> p (n h)"), C.bank(2)[0:64, 0:256], AF.Exp), reads=[("psum", 2)], writes=["ekd"])
    P.op("act", lambda e: e.activation(eglb.rearrange("p n h -> p (n h)"), C.bank(3)[:, 0:256], AF.Exp), reads=[("psum", 3)], writes=["eglb"])
    C.dbg("gg", gg, ["gg"])
    C.dbg("beta", beta, ["beta"])
    C.dbg("egc", egc, ["egc"])
    gw = [C.sb([128, 8, 384], F32R) for _ in range(2)]
    P.dma(gw[0], A["w_gdn"][0], writes=[("gw", 0)], eng="pool")
    m1 = C.mark()

    def gdn_head(h):
        P.label = "A3S1"
        C.release(m1)
        qT = C.sb([128, S], BF16)
        kT = C.sb([128, S], BF16)
        vT = C.sb([128, S], BF16)
        qkT = (qT, kT)
        E = C.sb([64, 32, 64], F32)
        EMI = C.sb([64, 32, 64], F32)
        EMS = C.sb([64, 32, 64], F32)
        GU = EMS
        ms1 = C.mark()
        w = gw[h % 2]
        cw = C.sb([128, 3, 4], F32)
        P.dma(cw, A["gdn_cw"][h], writes=["cw"])
        dg = C.sb([128, 3, 4, 128], F32R)
        for i in range(3):
            for k in range(4):
                P.op("pool" if (i * 4 + k) % 2 else "dve", lambda e, i=i, k=k: e.tensor_scalar(dg[:, i, k, :], ident, cw[:, i, k:k + 1], None, ALU.mult),
                     reads=["ident", "cw"], writes=[("dg", i, k)])
        raw = C.sb([128, 3, 2056], F32R)
        P.op("pool", lambda e: e.tensor_copy(raw[:, :, 0:8], zeros[:, 0:24].rearrange("p (a b) -> p a b", a=3)), reads=["zeros"], writes=[("rawpad")])
        qk32 = [C.sb([128, S], F32) for _ in range(2)]
        sqr = C.sb([128, S], F32R)
        lt = [C.sb([128, 512], F32) for _ in range(2)]
        P.op("dve", lambda e: e.tensor_tensor(GU, U64.unsqueeze(1).to_broadcast([64, 32, 64]), gg[:, :, h:h + 1].to_broadcast([64, 32, 64]), ALU.mult),
             reads=["U64", "gg"], writes=["GU"])
        def emit_E():
            for q4 in range(4):
                eb_ = 6 + q4 % 2
                P.op("pe", lambda e, q4=q4, eb_=eb_: e.matmul(C.bank(eb_)[0:64, :], SU64, GU[:, q4 * 8:(q4 + 1) * 8, :].rearrange("p a b -> p (a b)"), start=True, stop=True),
                     reads=["SU64", "GU"], writes=[("psum", eb_)])
                P.op("act", lambda e, q4=q4, eb_=eb_: e.activation(E[:, q4 * 8:(q4 + 1) * 8, :].rearrange("p a b -> p (a b)"), C.bank(eb_)[0:64, :], AF.Exp), reads=[("psum", eb_)], writes=[("E", q4)])
            rdE = [("E", q4) for q4 in range(4)]
            P.op("pool", lambda e: e.tensor_tensor(EMI, E, U64.unsqueeze(1).to_broadcast([64, 32, 64]), ALU.mult), reads=rdE + ["U64"], writes=["EMI"])
            P.op("pool", lambda e: e.tensor_tensor(EMS, E, mS.unsqueeze(1).to_broadcast([64, 32, 64]), ALU.mult), reads=rdE + ["mS", "GU"], writes=["EMS", "GU"])
            P.op("dve", lambda e: e.tensor_tensor(EMS, EMS, negb[:, :, h:h + 1].to_broadcast([64, 32, 64]), ALU.mult), reads=["EMS", "negb"], writes=["EMS"])

        for i in range(3):
            for tb in range(4):
                if i == 1 and tb == 0:
                    emit_E()
                bk = tb % 2
                for kc in range(8):
                    P.op("pe", lambda e, bk=bk, kc=kc, tb=tb, i=i: e.matmul(C.bank(bk), w[:, kc, i * 128:(i + 1) * 128], xT[:, kc, tb * 512:(tb + 1) * 512],
                                                                          start=(kc == 0), stop=(kc == 7)),
                         reads=[("gw", h % 2)] + C.rd_xT_tb[tb], writes=[("psum", bk)])
                copy_op(P, C.evac_eng(), raw[:, i, 8 + tb * 512:8 + (tb + 1) * 512], C.bank(bk), reads=[("psum", bk)], writes=[("raw", i, tb)])
        if h + 1 < 8:
            P.dma(gw[(h + 1) % 2], A["w_gdn"][h + 1], writes=[("gw", (h + 1) % 2)], eng="pool")
        for i in range(3):
            for tb in range(4):
                bk = 2 + tb % 2
                for k in range(4):
                    P.op("pe", lambda e, bk=bk, k=k, tb=tb, i=i: e.matmul(C.bank(bk), dg[:, i, k, :], raw[:, i, 5 + k + tb * 512:5 + k + (tb + 1) * 512],
                                                                        start=(k == 0), stop=(k == 3)),
                         reads=[("dg", i, k), ("raw", i, tb), "rawpad"] + ([("raw", i, tb - 1)] if tb > 0 else []), writes=[("psum", bk)])
                if i < 2:
                    P.op("act", lambda e, bk=bk, tb=tb, i=i: e.activation(qk32[i][:, tb * 512:(tb + 1) * 512], C.bank(bk), AF.Silu), reads=[("psum", bk)], writes=[("qk32", i, tb)])
                else:
                    P.op("act", lambda e, bk=bk, tb=tb: e.activation(vT[:, tb * 512:(tb + 1) * 512], C.bank(bk), AF.Silu), reads=[("psum", bk)], writes=[("vT", tb)])
        for i in range(2):
            for tb in range(4):
                P.op("act", lambda e, tb=tb, i=i: e.activation(sqr[:, tb * 512:(tb + 1) * 512], qk32[i][:, tb * 512:(tb + 1) * 512], AF.Square),
                     reads=[("qk32", i, tb)], writes=[("sqr", tb)])
            for tb in range(4):
                bk = 4 + tb % 2
                b = tb % 2
                P.op("pe", lambda e, bk=bk, tb=tb: e.matmul(C.bank(bk), C.onesr, sqr[:, tb * 512:(tb + 1) * 512], start=True, stop=True),
                     reads=["onesr", ("sqr", tb)], writes=[("psum", bk)])
                P.op("act", lambda e, bk=bk, b=b: e.activation(lt[b], C.bank(bk), AF.Ln, bias=epsr), reads=[("psum", bk), "epsr"], writes=[("lt", b)])
                P.op("act", lambda e, b=b, i=i: e.activation(lt[b], lt[b], AF.Exp, bias=(lnq if i == 0 else zer1), scale=-0.5), reads=[("lt", b), "lnq", "zer1"], writes=[("lt", b)])
                P.op("dve", lambda e, b=b, tb=tb, i=i: e.tensor_tensor(qkT[i][:, tb * 512:(tb + 1) * 512], qk32[i][:, tb * 512:(tb + 1) * 512], lt[b], ALU.mult),
                     reads=[("lt", b), ("qk32", i, tb)], writes=[("qkT", i, tb)])
        if h == 0:
            C.dbg("qT0", qT, [("qkT", 0, tb) for tb in range(4)])
            C.dbg("kT0", kT, [("qkT", 1, tb) for tb in range(4)])
            C.dbg("vT0", vT, [("vT", tb) for tb in range(4)])
        C.release(ms1)
        P.label = "A3pre"
        YT = C.sb([64, 32, 2, 64], BF16)
        Xp = C.sb([64, 32, 64], BF16)
        qk_o = C.sb([64, 32, 64], F32R)
        rdq = [("qkT", 0, tb) for tb in range(4)]
        rdk = [("qkT", 1, tb) for tb in range(4)]
        rdv = [("vT", tb) for tb in range(4)]
        for n in range(32):
            P.op("pe", lambda e, n=n: e.matmul(C.bank(4 + n // 8)[0:64, (n % 8) * 64:(n % 8 + 1) * 64], kT[:, n * 64:(n + 1) * 64], kT[:, n * 64:(n + 1) * 64], start=True, stop=True),
                 reads=[("qkT", 1, n // 8)], writes=[("psum", 4 + n // 8)])
        for q4 in range(4):
            P.op("dve", lambda e, q4=q4: e.tensor_tensor(YT[:, q4 * 8:(q4 + 1) * 8, 0, :], C.bank(4 + q4)[0:64, :].rearrange("p (a b) -> p a b", a=8), EMS[:, q4 * 8:(q4 + 1) * 8, :], ALU.mult),
                 reads=[("psum", 4 + q4), "EMS"], writes=[("YT", q4)])
            P.op("pool", lambda e, q4=q4: e.tensor_copy(YT[:, q4 * 8:(q4 + 1) * 8, 1, :], ident[0:64, 0:64].unsqueeze(1).to_broadcast([64, 8, 64])),
                 reads=[("YT", q4), "ident"], writes=[("YT", q4)])
        for n in range(32):
            P.op("pe", lambda e, n=n: e.matmul(C.bank(n // 8)[0:64, (n % 8) * 64:(n % 8 + 1) * 64], kT[:, n * 64:(n + 1) * 64], qT[:, n * 64:(n + 1) * 64], start=True, stop=True),
                 reads=[("qkT", 1, n // 8), ("qkT", 0, n // 8)], writes=[("psum", n // 8)])
        for q4 in range(4):
            P.op("dve", lambda e, q4=q4: e.tensor_tensor(qk_o[:, q4 * 8:(q4 + 1) * 8, :], C.bank(q4)[0:64, :].rearrange("p (a b) -> p a b", a=8), EMI[:, q4 * 8:(q4 + 1) * 8, :], ALU.mult),
                 reads=[("psum", q4), "EMI"], writes=[("qk_o", q4)])
        P.dma(A["sc_qk"][:, :, h, :].rearrange("n m c -> m n c"), qk_o, reads=[("qk_o", q4) for q4 in range(4)], eng="pool")
        for n in range(32):
            P.op("pe", lambda e, n=n: e.transpose(C.bankb(4 + n // 8)[0:64, (n % 8) * 64:(n % 8 + 1) * 64], YT[:, n, 0, :], C.identb[0:64, 0:64]),
                 reads=[("YT", n // 8), "identb"], writes=[("psum", 4 + n // 8)])
        for q4 in range(4):
            copy_op(P, C.evac_eng(), Xp[:, q4 * 8:(q4 + 1) * 8, :], C.bankb(4 + q4)[0:64, 0:512].rearrange("p (a b) -> p a b", a=8), reads=[("psum", 4 + q4)], writes=[("Xp", q4)])
        P.label = "A3chain"
        for itn in range(6):
            last = itn == 5
            for q4 in range(4):
                a0 = 3 * (q4 % 2)
                for j in range(8):
                    n = q4 * 8 + j
                    P.op("pe", lambda e, a0=a0, j=j, n=n: e.matmul(C.bank(a0 + j // 4)[0:64, (j % 4) * 128:(j % 4 + 1) * 128], Xp[:, n, :], YT[:, n, :, :].rearrange("p a b -> p (a b)"),
                                                                 start=True, stop=True),
                         reads=[("Xp", q4), ("YT", q4)], writes=[("psum", a0 + j // 4)])
                    if not last:
                        P.op("pe", lambda e, a0=a0, j=j, n=n: e.matmul(C.bank(a0 + 2)[0:64, j * 64:(j + 1) * 64], YT[:, n, 0, :], Xp[:, n, :], start=True, stop=True),
                             reads=[("Xp", q4), ("YT", q4)], writes=[("psum", a0 + 2)])
                Av = C.bank(a0, 2)[0:64, :].rearrange("p (a b) -> p a b", a=8)
                if not last:
                    P.op("act", lambda e, q4=q4, Av=Av: e.copy(YT[:, q4 * 8:(q4 + 1) * 8, 0, :], Av[:, :, 0:64]), reads=[("psum", a0), ("psum", a0 + 1)], writes=[("YT", q4)])
                P.op("dve", lambda e, q4=q4, Av=Av: e.tensor_tensor(YT[:, q4 * 8:(q4 + 1) * 8, 1, :], Av[:, :, 64:128], YT[:, q4 * 8:(q4 + 1) * 8, 1, :], ALU.add),
                     reads=[("psum", a0), ("psum", a0 + 1), ("YT", q4)], writes=[("YT", q4)])
                if not last:
                    P.op("act", lambda e, q4=q4, a0=a0: e.copy(Xp[:, q4 * 8:(q4 + 1) * 8, :], C.bank(a0 + 2)[0:64, :].rearrange("p (a b) -> p a b", a=8)),
                         reads=[("psum", a0 + 2)], writes=[("Xp", q4)])
        P.label = "A3post"
        kg = C.sb([64, 8, 128], BF16)
        kd_o = C.sb([64, 8, 128], F32R)
        vtok = C.sb([64, 8, 128], BF16)
        bu_o = C.sb([64, 8, 128], F32)
        wT_o = C.sb([128, 8, 64], F32R)
        qd_tok = C.sb([64, 8, 128], BF16)
        qd_o = C.sb([128, 8, 64], F32R)
        for q4 in range(4):
            n0 = q4 * 8
            bc = lambda t, n0=n0: t[:, n0:n0 + 8, h:h + 1].to_broadcast([64, 8, 128])
            for j in range(8):
                n = n0 + j
                P.op("pe", lambda e, j=j, n=n: e.transpose(C.bankb(6)[0:64, j * 128:(j + 1) * 128], kT[:, n * 64:(n + 1) * 64], C.identb),
                     reads=[("qkT", 1, n // 8), "identb"], writes=[("psum", 6)])
            kv = C.bankb(6)[0:64, :].rearrange("p (a b) -> p a b", a=8)
            P.op("dve", lambda e, kv=kv, bc=bc: e.tensor_tensor(kg, kv, bc(egc), ALU.mult), reads=[("psum", 6), "egc"], writes=["kg"])
            P.op("dve", lambda e, kv=kv, bc=bc: e.tensor_tensor(kd_o, kv, bc(ekd), ALU.mult), reads=[("psum", 6), "ekd"], writes=["kd_o"])
            P.dma(A["sc_kd"][n0:n0 + 8, :, h, :].rearrange("n c d -> c n d"), kd_o, reads=["kd_o"], eng="pool")
            for j in range(8):
                n = n0 + j
                P.op("pe", lambda e, j=j, n=n: e.transpose(C.bankb(7)[0:64, j * 128:(j + 1) * 128], vT[:, n * 64:(n + 1) * 64], C.identb),
                     reads=[("vT", n // 8), "identb"], writes=[("psum", 7)])
            P.op("act", lambda e: e.copy(vtok, C.bankb(7)[0:64, :].rearrange("p (a b) -> p a b", a=8)), reads=[("psum", 7)], writes=["vtok"])
            for j in range(8):
                n = n0 + j
                P.op("pe", lambda e, j=j, n=n: e.matmul(C.bank(j // 4)[0:64, (j % 4) * 128:(j % 4 + 1) * 128], YT[:, n, 1, :], vtok[:, j, :], start=True, stop=True),
                     reads=[("YT", q4), "vtok"], writes=[("psum", j // 4)])
            uv = C.bank(0, 2)[0:64, :].rearrange("p (a b) -> p a b", a=8)
            P.op("dve", lambda e, uv=uv, bc=bc: e.tensor_tensor(bu_o, uv, bc(beta), ALU.mult), reads=[("psum", 0), ("psum", 1), "beta"], writes=["bu_o"])
            P.dma(A["sc_bu"][n0:n0 + 8, :, h, :].rearrange("n c d -> c n d"), bu_o, reads=["bu_o"])
            for j in range(8):
                n = n0 + j
                P.op("pe", lambda e, j=j, n=n: e.matmul(C.bank(2)[:, j * 64:(j + 1) * 64], kg[:, j, :], YT[:, n, 1, :], start=True, stop=True),
                     reads=[("YT", q4), "kg"], writes=[("psum", 2)])
            P.op("act", lambda e: e.copy(wT_o, C.bank(2).rearrange("p (a b) -> p a b", a=8)), reads=[("psum", 2)], writes=["wT_o"])
            P.dma(A["sc_wT"][n0:n0 + 8, :, h, :].rearrange("n p c -> p n c"), wT_o, reads=["wT_o"], eng="pool")
            for j in range(8):
                n = n0 + j
                P.op("pe", lambda e, j=j, n=n: e.transpose(C.bankb(3)[0:64, j * 128:(j + 1) * 128], qT[:, n * 64:(n + 1) * 64], C.identb),
                     reads=[("qkT", 0, n // 8), "identb"], writes=[("psum", 3)])
            qv = C.bankb(3)[0:64, :].rearrange("p (a b) -> p a b", a=8)
            P.op("dve", lambda e, qv=qv, bc=bc: e.tensor_tensor(qd_tok, qv, bc(egc), ALU.mult), reads=[("psum", 3), "egc"], writes=["qd_tok"])
            for j in range(8):
                P.op("pe", lambda e, j=j: e.transpose(C.bankb(5)[:, j * 64:(j + 1) * 64], qd_tok[:, j, :], C.identb[0:64, 0:64]),
                     reads=["qd_tok", "identb"], writes=[("psum", 5)])
            P.op("act", lambda e: e.copy(qd_o, C.bankb(5)[:, 0:512].rearrange("p (a b) -> p a b", a=8)), reads=[("psum", 5)], writes=["qd_o"])
            P.dma(A["sc_qd"][n0:n0 + 8, :, h, :].rearrange("n p c -> p n c"), qd_o, reads=["qd_o"], eng="pool")

    for h in range(8):
        gdn_head(h)

    P.label = "A3scan"
    C.release(m1)
    Sr2 = [C.sb([128, 8, 128], F32R) for _ in range(2)]
    tmp2 = C.sb([128, 8, 128], F32)
    P.op("pool", lambda e: e.memset(tmp2, 0.0), writes=["tmp2"])
    P.op("pool", lambda e: e.tensor_copy(Sr2[0], tmp2), reads=["tmp2"], writes=[("Sr", 0)])
    NB = 3
    bu = [C.sb([64, 8, 128], F32) for _ in range(NB)]
    wT = [C.sb([128, 8, 64], F32R) for _ in range(NB)]
    qd = [C.sb([128, 8, 64], F32R) for _ in range(NB)]
    qk = [C.sb([64, 8, 64], F32R) for _ in range(NB)]
    kd = [C.sb([64, 8, 128], F32R) for _ in range(NB)]
    zsb = [C.sb([64, 1024], F32) for _ in range(3)]
    ybb = [C.sb([64, 1024], F32) for _ in range(3)]
    on2 = [C.sb([64, 8, 128], F32) for _ in range(2)]
    tmp = C.sb([64, 8, 128], F32)
    vn = C.sb([64, 8, 128], F32R)
    sq = C.sb([64, 8, 128], F32)
    on = C.sb([64, 8, 128], F32)
    ssum = C.sb([64, 8], F32)

    def loads(n):
        b = n % NB
        P.dma(wT[b].bitcast(F32), A["sc_wT"][n], writes=[("wT", b)])
        P.dma(bu[b], A["sc_bu"][n], writes=[("bu", b)])
        P.dma(kd[b].bitcast(F32), A["sc_kd"][n], writes=[("kd", b)])
        P.dma(qd[b].bitcast(F32), A["sc_qd"][n], writes=[("qd", b)])
        P.dma(qk[b].bitcast(F32), A["sc_qk"][n], writes=[("qk", b)])

    def loads_post(n):
        b3 = n % 3
        P.dma(zsb[b3], A["zs"][n * 64:(n + 1) * 64, :], writes=[("zsb", b3)], eng="pool")
        P.dma(ybb[b3], A["yb"][n * 64:(n + 1) * 64, :], writes=[("ybb", b3)], eng="pool")

    def crit(n):
        b = n % NB
        b2 = n % 2
        Sc = Sr2[n % 2]
        Sn = Sr2[(n + 1) % 2]
        rSc = ("Sr", n % 2)
        rSn = ("Sr", (n + 1) % 2)
        for h in range(8):
            P.op("pe", lambda e, h=h: e.matmul(C.bank(h // 4)[0:64, (h % 4) * 128:(h % 4 + 1) * 128], wT[b][:, h, :], Sc[:, h, :], start=True, stop=True),
                 reads=[("wT", b), rSc], writes=[("psum", h // 4)])
        for h in range(8):
            P.op("pe", lambda e, h=h: e.matmul(C.bank(2 + h // 4)[0:64, (h % 4) * 128:(h % 4 + 1) * 128], qd[b][:, h, :], Sc[:, h, :], start=(h % 4 == 0), stop=False, skip_group_check=True),
                 reads=[("qd", b), rSc], writes=[("psum", 2 + h // 4)])
        p1 = C.bank(0, 2)[0:64, :].rearrange("p (a b) -> p a b", a=8)
        P.op("dve", lambda e: e.tensor_tensor(tmp, p1, negb[:, n, :].unsqueeze(2).to_broadcast([64, 8, 128]), ALU.mult), reads=[("psum", 0), ("psum", 1), "negb"], writes=["tmp"])
        P.op("dve", lambda e: e.tensor_tensor(vn, tmp, bu[b], ALU.add), reads=["tmp", ("bu", b)], writes=["vn"])
        for h in range(8):
            P.op("pe", lambda e, h=h: e.matmul(C.bank(4 + h // 4)[:, (h % 4) * 128:(h % 4 + 1) * 128], kd[b][:, h, :], vn[:, h, :], start=True, stop=True),
                 reads=[("kd", b), "vn"], writes=[("psum", 4 + h // 4)])
        for h in range(8):
            P.op("pe", lambda e, h=h: e.matmul(C.bank(2 + h // 4)[0:64, (h % 4) * 128:(h % 4 + 1) * 128], qk[b][:, h, :], vn[:, h, :], start=False, stop=True, skip_group_check=True),
                 reads=[("qk", b), "vn"], writes=[("psum", 2 + h // 4)])
        if n < 31:
            P.op("dve", lambda e: e.tensor_tensor(Sn, C.bank(4, 2).rearrange("p (a b) -> p a b", a=8), tmp2, ALU.add), reads=[("psum", 4), ("psum", 5), "tmp2"], writes=[rSn])
        if n < 30:
            P.op("dve", lambda e: e.tensor_tensor(tmp2, Sn.bitcast(F32), eglb[:, n + 1, :].unsqueeze(2).to_broadcast([128, 8, 128]), ALU.mult), reads=[rSn, "eglb"], writes=["tmp2"])
        ov_ = C.bank(2, 2)[0:64, :].rearrange("p (a b) -> p a b", a=8)
        P.op("act", lambda e: e.copy(on2[b2], ov_), reads=[("psum", 2), ("psum", 3)], writes=[("on2", b2)])

    def post(n):
        b2 = n % 2
        b3 = n % 3
        P.op("act", lambda e: e.activation(sq, on2[b2], AF.Square), reads=[("on2", b2)], writes=["sq"])
        P.op("dve", lambda e: e.tensor_reduce(ssum, sq, AX.X, ALU.add), reads=["sq"], writes=["ssum"])
        P.op("act", lambda e: e.activation(ssum, ssum, AF.Sqrt, bias=epsr[0:64], scale=1.0 / 128.0), reads=["ssum", "epsr"], writes=["ssum"])
        P.op("dve", lambda e: e.reciprocal(ssum, ssum), reads=["ssum"], writes=["ssum"])
        for h in range(8):
            P.op("act", lambda e, h=h: e.activation(on[:, h, :], on2[b2][:, h, :], AF.Identity, scale=ssum[:, h:h + 1]), reads=[("on2", b2), "ssum"], writes=["on"])
        onf = on.rearrange("p a b -> p (a b)")
        P.op("pool", lambda e: e.tensor_tensor(onf, onf, zsb[b3], ALU.mult), reads=["on", ("zsb", b3)], writes=["on"])
        P.op("pool", lambda e: e.tensor_tensor(ybb[b3], ybb[b3], onf, ALU.add), reads=[("ybb", b3), "on"], writes=[("ybb", b3)])
        P.dma(A["merged"][n * 64:(n + 1) * 64, :], ybb[b3], reads=[("ybb", b3)], eng="pool")

    loads(0)
    loads(1)
    loads_post(0)
    for n in range(32):
        crit(n)
        if n >= 1:
            post(n - 1)
        if n + 1 < 32:
            loads_post(n + 1)
        if n + 2 < 32:
            loads(n + 2)
    post(31)
    C.release(m0)


def kmaj(w):
    K, N = w.shape
    return np.ascontiguousarray(w.reshape(K // 128, 128, N).transpose(1, 0, 2))


IN_OFF = dict(g_q=0, g_k=1024, g_v=2048, g_z=3072, g_b=4096, g_a=4104, n_q=4112, c_k=5136, c_v=5392, s_k=5648, s_v=5904,
              w_k=6160, w_v=6416, n_g=6672, m_g=6696)

WEIGHT_SPECS = {
    "w_gdn": ([8, 128, 8, 384], F32), "w_gb": ([128, 8, 16], F32), "gdn_cw": ([8, 128, 3, 4], F32),
    "gdn_a_log": ([8], F32), "gdn_dt_bias": ([8], F32), "gdn_norm_w": ([128], F32),
    "inv_freq": ([128, 1], F32),
    "w_mga": ([128, 8, 1024], F32), "w_mgb": ([128, 8, 1024], F32), "w_z": ([128, 8, 1024], F32), "w_ng": ([128, 8, 24], F32),
    "w_nsa_c": ([2, 128, 8, 256], F32), "w_nsa_k": ([2, 128, 8, 256], F32), "w_nsa_v": ([2, 128, 8, 256], F32), "w_nsa_q": ([2, 128, 8, 512], F32),
    "cmp_pe": ([2, 32, 128], F32), "cmp_w1": ([2, 128, 32, 256], F32), "cmp_b1": ([128, 2, 2], F32), "cmp_w2": ([2, 256, 128], F32),
    "w_out": ([128, 8, 1024], F32),
    "ln1_g": ([1024], F32), "ln1_b": ([1024], F32),
    "xa_wq": ([128, 8, 1024], F32), "xa_wk": ([128, 8, 1024], F32), "xa_wv": ([128, 8, 1024], F32), "xa_wo": ([128, 8, 1024], F32),
    "ln2_g": ([1024], F32), "ln2_b": ([1024], F32),
    "moe_wr": ([128, 8, 36], F32), "moe_br": ([36], F32),
    "moe_wg": ([32, 128, 8, 256], F32), "moe_wu": ([32, 128, 8, 256], F32), "moe_wd": ([32, 128, 2, 1024], F32),
    "ln3_g": ([1024], F32), "ln3_b": ([1024], F32),
}


def prep_weights(inp):
    l = 0
    w = {}
    wi = inp["w_in"][l]
    O = IN_OFF
    w["inv_freq"] = np.tile((10000.0 ** (-np.arange(64, dtype=np.float32) / np.float32(64))).astype(np.float32), 2).reshape(128, 1)
    w["w_gdn"] = np.stack([kmaj(np.concatenate([wi[:, O[a] + h * 128:O[a] + (h + 1) * 128] for a in ("g_q", "g_k", "g_v")], axis=1)) for h in range(8)])
    w["w_gb"] = kmaj(wi[:, O["g_b"]:O["g_b"] + 16])
    w["gdn_cw"] = np.ascontiguousarray(inp["gdn_conv_w"][l].reshape(4, 3, 8, 128).transpose(2, 3, 1, 0))
    w["gdn_a_log"] = np.ascontiguousarray(inp["gdn_a_log"][l])
    w["gdn_dt_bias"] = np.ascontiguousarray(inp["gdn_dt_bias"][l])
    w["gdn_norm_w"] = np.ascontiguousarray(inp["gdn_norm_w"][l])
    w["w_mga"] = kmaj(wi[:, O["m_g"]:O["m_g"] + 1024])
    w["w_mgb"] = kmaj(wi[:, O["m_g"] + 1024:O["m_g"] + 2048])
    w["w_z"] = kmaj(wi[:, O["g_z"]:O["g_z"] + 1024])
    w["w_ng"] = kmaj(wi[:, O["n_g"]:O["n_g"] + 24])
    def grp(a, b):
        return np.stack([kmaj(np.concatenate([wi[:, O[a] + g * 128:O[a] + (g + 1) * 128], wi[:, O[b] + g * 128:O[b] + (g + 1) * 128]], axis=1)) for g in range(2)])
    w["w_nsa_c"] = grp("c_k", "c_v")
    w["w_nsa_k"] = grp("s_k", "w_k")
    w["w_nsa_v"] = grp("s_v", "w_v")
    w["w_nsa_q"] = np.stack([kmaj(wi[:, O["n_q"] + g * 512:O["n_q"] + (g + 1) * 512]) for g in range(2)])
    w["cmp_pe"] = np.ascontiguousarray(inp["cmp_pe"][l])
    w["cmp_w1"] = np.ascontiguousarray(inp["cmp_w1"][l].reshape(2, 32, 128, 256).transpose(0, 2, 1, 3))
    w["cmp_b1"] = np.ascontiguousarray(inp["cmp_b1"][l].reshape(2, 2, 128).transpose(2, 0, 1))
    w["cmp_w2"] = np.ascontiguousarray(inp["cmp_w2"][l])
    w["w_out"] = kmaj(inp["w_out"][l])
    for k in ("ln1_g", "ln1_b", "ln2_g", "ln2_b", "ln3_g", "ln3_b"):
        w[k] = np.ascontiguousarray(inp[k][l])
    w["xa_wq"] = kmaj(inp["xa_wq"][l])
    w["xa_wk"] = kmaj(inp["xa_wkv"][l][:, :1024])
    w["xa_wv"] = kmaj(inp["xa_wkv"][l][:, 1024:])
    w["xa_wo"] = kmaj(inp["xa_wo"][l])
    w["moe_wr"] = kmaj(np.concatenate([inp["moe_w_group"][l], inp["moe_w_expert"][l]], axis=1))
    w["moe_br"] = np.concatenate([inp["moe_b_group"][l], inp["moe_b_expert"][l]], axis=0)
    w["moe_wg"] = np.ascontiguousarray(inp["moe_w_gate"][l].reshape(32, 8, 128, 256).transpose(0, 2, 1, 3))
    w["moe_wu"] = np.ascontiguousarray(inp["moe_w_up"][l].reshape(32, 8, 128, 256).transpose(0, 2, 1, 3))
    w["moe_wd"] = np.ascontiguousarray(inp["moe_w_down"][l].reshape(32, 2, 128, 1024).transpose(0, 2, 1, 3))
    return w


def build(phases="BCD", ext=()):
    nc = bass.Bass("TRN2", target_bir_lowering=False)
    A = {}
    A["x"] = nc.dram_tensor("x", [S, D], F32, kind="ExternalInput").ap()
    A["mem"] = nc.dram_tensor("mem", [256, D], F32, kind="ExternalInput").ap()
    A["positions"] = nc.dram_tensor("positions", [S], I32, kind="ExternalInput").ap()
    for k, (shape, dt) in WEIGHT_SPECS.items():
        A[k] = nc.dram_tensor(k, shape, dt, kind="ExternalInput").ap()
    A["out"] = nc.dram_tensor("out", [S, D], F32, kind="ExternalOutput").ap()
    for k in ("merged", "h1", "h2", "yb", "zs", "gates"):
        kind = "ExternalInput" if k in ext else ("ExternalOutput" if ("dbg" in ext) else "Internal")
        A[k] = nc.dram_tensor(k, [S, 2 * D if k == "gates" else D], F32, kind=kind).ap()
    A["sc_bu"] = nc.dram_tensor("sc_bu", [32, 64, 8, 128], F32, kind="Internal").ap()
    A["sc_kd"] = nc.dram_tensor("sc_kd", [32, 64, 8, 128], F32, kind="Internal").ap()
    A["sc_wT"] = nc.dram_tensor("sc_wT", [32, 128, 8, 64], F32, kind="Internal").ap()
    A["sc_qd"] = nc.dram_tensor("sc_qd", [32, 128, 8, 64], F32, kind="Internal").ap()
    A["sc_qk"] = nc.dram_tensor("sc_qk", [32, 64, 8, 64], F32, kind="Internal").ap()
    C = Ctx(nc)
    C.debug = "dbg" in ext
    build_consts(C)
    if "A" in phases or "N" in phases or "G" in phases:
        phase_A0(C, A)
    if "A" in phases or "1" in phases:
        phase_A1(C, A)
    if "A" in phases or "N" in phases:
        phase_A2(C, A)
    if "A" in phases or "G" in phases:
        phase_A3(C, A)
    if "A" in phases or "N" in phases or "G" in phases:
        C.release(C.mA0)
    if "B" in phases:
        phase_B(C, A)
    if "C" in phases:
        phase_C(C, A)
    if "D" in phases:
        phase_D(C, A)
    global LAST_PROG, LAST_CTX
    LAST_PROG = C.P
    LAST_CTX = C
    C.P.emit()
    return nc


def kernel(**inputs):
    inputs = {k: np.asarray(v) for k, v in inputs.items()}
    w = prep_weights(inputs)
    nc = build(phases="ABCD", ext=())
    in_maps = []
    for b in range(8):
        m = dict(w)
        m["x"] = np.ascontiguousarray(inputs["x"][b], dtype=np.float32)
        m["mem"] = np.ascontiguousarray(inputs["mem"][b], dtype=np.float32)
        m["positions"] = np.ascontiguousarray(inputs["positions"][b]).astype(np.int32)
        in_maps.append(m)
    res = run_bass_kernel_spmd(nc, in_maps, core_ids=list(range(8)))
    return np.stack([np.asarray(r["out"]) for r in res.results]).astype(np.float32)
```

```python
import contextlib
import numpy as np
import concourse.bass as bass
import concourse.mybir as mybir
from concourse.bass_utils import run_bass_kernel_spmd

F32 = mybir.dt.float32
BF16 = mybir.dt.bfloat16
F32R = mybir.dt.float32r
I32 = mybir.dt.int32
AF = mybir.ActivationFunctionType
ALU = mybir.AluOpType
AX = mybir.AxisListType

ENGS = ("pe", "act", "dve", "pool", "sp")
EPOCH = 12000
CHECK_PSUM = False
INST_LABELS = None

S = 2048
D = 1024
NT = 16
DN_ALPHA = 2.0 ** 0.25
LN_EPS = 1e-5
RMS_EPS = 1e-6


class Reg:
    __slots__ = ("key", "psum", "last_w", "readers")

    def __init__(self, key, psum):
        self.key = key
        self.psum = psum
        self.last_w = None
        self.readers = []


class Op:
    __slots__ = ("eng", "fn", "deps", "sem", "target", "dma", "needs_inc", "idx", "pbanks", "label")

    def __init__(self, eng, fn, dma):
        self.eng = eng
        self.fn = fn
        self.deps = []
        self.sem = None
        self.target = 0
        self.dma = dma
        self.needs_inc = dma
        self.idx = 0


class Prog:
    def __init__(self, nc, n_dma_sems=40):
        self.nc = nc
        self.ops = {e: [] for e in ENGS}
        self.regs = {}
        self.n_dma_sems = n_dma_sems
        self.dma_last = [None] * n_dma_sems
        self.dma_count = [0] * n_dma_sems
        self.dma_rr = 0
        self.dma_rr_pool = 0
        self.fence_ops = []

    def R(self, *key):
        r = self.regs.get(key)
        if r is None:
            r = Reg(key, len(key) > 0 and key[0] == "psum")
            self.regs[key] = r
        return r

    def _regs(self, lst):
        out = []
        for x in lst:
            if isinstance(x, Reg):
                out.append(x)
            elif isinstance(x, tuple):
                out.append(self.R(*x))
            else:
                out.append(self.R(x))
        return out

    def fence(self):
        f = []
        for e in ENGS:
            for o in reversed(self.ops[e]):
                if not o.dma:
                    f.append(o)
                    break
        for o in self.dma_last:
            if o is not None:
                f.append(o)
        for o in f:
            o.needs_inc = True
        self.fence_ops = f
        self.regs = {}

    def op(self, eng, fn, reads=(), writes=(), dma=False):
        o = Op(eng, fn, dma)
        deps = {}
        reads = self._regs(reads)
        writes = self._regs(writes)
        o.pbanks = set(r.key[1] for r in reads + writes if r.psum)
        o.label = getattr(self, "label", "")
        for r in reads:
            if r.psum:
                writes.append(r)
                continue
            if r.last_w is not None:
                deps[id(r.last_w)] = r.last_w
            r.readers.append(o)
        for r in writes:
            if r.last_w is not None:
                deps[id(r.last_w)] = r.last_w
            for q in r.readers:
                if q is not o:
                    deps[id(q)] = q
            r.readers = []
            r.last_w = o
        for q in self.fence_ops:
            deps[id(q)] = q
        if dma:
            half = self.n_dma_sems // 2
            if eng == "pool":
                s = half + self.dma_rr_pool
                self.dma_rr_pool = (self.dma_rr_pool + 1) % (self.n_dma_sems - half)
            else:
                s = self.dma_rr
                self.dma_rr = (self.dma_rr + 1) % half
            prev = self.dma_last[s]
            if prev is not None:
                deps[id(prev)] = prev
            self.dma_last[s] = o
            self.dma_count[s] += 1
            o.sem = ("dma", s)
            o.target = 16 * self.dma_count[s]
        o.deps = list(deps.values())
        for d in o.deps:
            d.needs_inc = True
        o.idx = len(self.ops[eng])
        self.ops[eng].append(o)
        return o

    def dma(self, out, in_, reads=(), writes=(), eng="sp", **kw):
        return self.op(eng, lambda e: e.dma_start(out=out, in_=in_, **kw), reads, writes, dma=True)

    def emit(self):
        nc = self.nc
        n_eng_sems = {}
        for e in ENGS:
            cnt = 0
            ep = 0
            for o in self.ops[e]:
                if o.dma:
                    continue
                if o.needs_inc:
                    cnt += 1
                    if cnt > EPOCH:
                        ep += 1
                        cnt = 1
                o.sem = (e, ep)
                o.target = cnt
            n_eng_sems[e] = ep + 1
        with contextlib.ExitStack() as st:
            sems = {}
            for e in ENGS:
                for ep in range(n_eng_sems[e]):
                    sems[(e, ep)] = st.enter_context(nc.semaphore(f"s_{e}_{ep}"))
            for s in range(self.n_dma_sems):
                if self.dma_count[s] > 0:
                    sems[("dma", s)] = st.enter_context(nc.semaphore(f"s_dma_{s}"))
            block = st.enter_context(nc.Block())

            def make(e):
                ops = self.ops[e]

                def body(eng):
                    waited = {}
                    for o in ops:
                        for d in o.deps:
                            if d.eng == e and not d.dma and not o.dma:
                                if e == "pe":
                                    continue
                                if e != "pool" and o.idx - d.idx > 3:
                                    continue
                            if waited.get(d.sem, 0) < d.target:
                                eng.wait_ge(sems[d.sem], d.target)
                                waited[d.sem] = d.target
                        ins = o.fn(eng)
                        if INST_LABELS is not None:
                            INST_LABELS[ins.ins.name] = o.label
                        if CHECK_PSUM:
                            touched = set()
                            for pap in tuple(ins.ins.ins) + tuple(ins.ins.outs):
                                if getattr(pap, "memref", None) == "psum_all":
                                    epb = 1024 if pap.dtype == BF16 else 512
                                    off = int(pap.offset) % (8 * epb)
                                    ext = 0
                                    for st, nn in list(pap.ap)[1:]:
                                        ext += abs(int(st)) * (int(nn) - 1)
                                    touched.update(range(off // epb, (off + ext) // epb + 1))
                            if not touched <= o.pbanks:
                                raise AssertionError(f"PSUM banks touched {touched} not declared {o.pbanks} in {ins.ins.concise()}")
                        if o.needs_inc:
                            ins.then_inc(sems[o.sem], 16 if o.dma else 1)
                    for o in ops:
                        if o.dma and self.dma_last[o.sem[1]] is o:
                            if waited.get(o.sem, 0) < o.target:
                                eng.wait_ge(sems[o.sem], o.target)
                                waited[o.sem] = o.target

                return body

            block.tensor(make("pe"))
            block.scalar(make("act"))
            block.vector(make("dve"))
            block.gpsimd(make("pool"))
            block.sync(make("sp"))


class Ctx:
    SB_BASE = 16640
    SB_LIMIT = 228800

    def __init__(self, nc):
        self.nc = nc
        self.P = Prog(nc)
        self.ptr = self.SB_BASE
        self.nalloc = 0
        self.rr = 0
        ps = nc.alloc_psum_tensor("psum_all", [128, 4096], F32).ap()
        self.ps = ps
        self.psb = ps.bitcast(BF16)

    def bank(self, b, n=1):
        return self.ps[:, b * 512:(b + n) * 512]

    def bankb(self, b, n=1):
        return self.psb[:, b * 1024:(b + n) * 1024]

    def sb(self, shape, dtype=F32):
        nbytes = int(np.prod(shape[1:])) * (2 if dtype == BF16 else 4)
        nbytes = (nbytes + 31) // 32 * 32
        off = self.ptr
        self.ptr += nbytes
        assert self.ptr <= self.SB_LIMIT, f"SBUF overflow {self.ptr}"
        self.nalloc += 1
        import sys as _sys
        if not hasattr(self, "names"):
            self.names = {}
        self.names[self.nalloc] = (list(shape), str(dtype), _sys._getframe(1).f_lineno, off)
        return self.nc.alloc_sbuf_tensor_at(f"t{self.nalloc}", list(shape), dtype, offset=off).ap()

    def mark(self):
        return self.ptr

    def release(self, m):
        self.P.fence()
        self.ptr = m

    def dbg(self, name, ap, reads):
        if not getattr(self, "debug", False):
            return
        shape = list(ap.shape)
        dt_ = F32 if ap.dtype == F32R else ap.dtype
        d = self.nc.dram_tensor("dbg_" + name, shape, dt_, kind="ExternalOutput").ap()
        self.P.dma(d, ap.bitcast(F32) if ap.dtype == F32R else ap, reads=reads)

    def evac_eng(self):
        self.rr += 1
        return "act" if self.rr % 2 else "dve"


def copy_op(P, eng, out, in_, reads, writes):
    if eng == "act":
        return P.op("act", lambda e: e.copy(out, in_), reads, writes)
    return P.op(eng, lambda e: e.tensor_copy(out, in_), reads, writes)


def build_consts(C):
    P = C.P
    ident = C.sb([128, 128], F32)
    P.op("pool", lambda e: e.memset(ident, 0.0), writes=["ident"])
    P.op("pool", lambda e: e.affine_select(out=ident, in_=ident, pattern=[[-1, 128]], compare_op=ALU.not_equal,
                                            fill=1.0, base=0, channel_multiplier=1), reads=["ident"], writes=["ident"])
    onesr = C.sb([128, 128], F32R)
    onesf = C.sb([128, 128], F32)
    P.op("pool", lambda e: e.memset(onesf, 1.0), writes=["onesf"])
    P.op("pool", lambda e: e.tensor_copy(onesr, onesf), reads=["onesf"], writes=["onesr"])
    eps_ln = C.sb([128, 1], F32)
    P.op("pool", lambda e: e.memset(eps_ln, LN_EPS), writes=["eps_ln"])
    identb = C.sb([128, 128], BF16)
    P.op("pool", lambda e: e.tensor_copy(identb, ident), reads=["ident"], writes=["identb"])
    C.identb = identb
    C.ident = ident
    C.onesr = onesr
    C.eps_ln = eps_ln


def to_featmajor(C, src, hT, name, ntiles=NT, xt_bufs=None):
    P = C.P
    m = C.mark()
    xt = xt_bufs if xt_bufs is not None else [C.sb([128, 1024], F32) for _ in range(2)]
    for t in range(ntiles):
        b = t % 2
        P.dma(xt[b], src[t * 128:(t + 1) * 128, :], writes=[(name + "_xt", b)])
        for half in range(2):
            bk = 2 * b + half
            for j in range(4):
                kc = half * 4 + j
                P.op("pe", lambda e, bk=bk, j=j, kc=kc, b=b: e.transpose(C.bank(bk)[:, j * 128:(j + 1) * 128],
                                                                        xt[b][:, kc * 128:(kc + 1) * 128], C.ident),
                     reads=[(name + "_xt", b), "ident"], writes=[("psum", bk)])
            copy_op(P, C.evac_eng(), hT[:, half * 4:(half + 1) * 4, t * 128:(t + 1) * 128],
                    C.bank(bk).rearrange("p (k n) -> p k n", k=4), reads=[("psum", bk)], writes=[(name, t, half)])
    if xt_bufs is None:
        C.release(m)
    return [(name, t, half) for t in range(ntiles) for half in range(2)]


class LNPipe:
    def __init__(self, C, g_bc, b_bc, tag, scratch):
        self.C, self.g_bc, self.b_bc, self.tag, self.scratch = C, g_bc, b_bc, tag, scratch
        self.pending = None

    def push(self, pre, out, rd_pre, wr_out, par, after_fn=None):
        C, P, tag = self.C, self.C.P, self.tag
        stats, mv, rstd, nmr = self.scratch[par]
        tg = (tag, par)
        for c in range(2):
            P.op("dve", lambda e, c=c: e.bn_stats(stats[:, c, :], pre[:, c * 512:(c + 1) * 512]), reads=rd_pre, writes=[(tg, "st", c)])
        P.op("dve", lambda e: e.bn_aggr(mv, stats.rearrange("p a b -> p (a b)")), reads=[(tg, "st", 0), (tg, "st", 1)], writes=[(tg, "mv")])
        P.op("act", lambda e: e.activation(rstd, mv[:, 1:2], AF.Sqrt, bias=C.eps_ln), reads=[(tg, "mv"), "eps_ln"], writes=[(tg, "sd")])
        g_bc, b_bc = self.g_bc, self.b_bc

        def stage2():
            P.op("dve", lambda e: e.reciprocal(rstd, rstd), reads=[(tg, "sd")], writes=[(tg, "rstd")])
            P.op("dve", lambda e: e.tensor_scalar(nmr, mv[:, 0:1], rstd, -1.0, ALU.mult, ALU.mult), reads=[(tg, "mv"), (tg, "rstd")], writes=[(tg, "nmr")])
            P.op("act", lambda e: e.activation(out, pre, AF.Identity, bias=nmr, scale=rstd), reads=list(rd_pre) + [(tg, "rstd"), (tg, "nmr")], writes=wr_out)
            P.op("dve", lambda e: e.tensor_tensor(out, out, g_bc, ALU.mult), reads=list(wr_out) + [(tag, "g")], writes=wr_out)
            P.op("pool", lambda e: e.tensor_tensor(out, out, b_bc, ALU.add), reads=list(wr_out) + [(tag, "b")], writes=wr_out)
            if after_fn is not None:
                after_fn()

        prev = self.pending
        self.pending = stage2
        if prev is not None:
            prev()

    def flush(self):
        if self.pending is not None:
            self.pending()
        self.pending = None


def ln_setup(C, g_dram, b_dram, tag, nbuf=2):
    P = C.P
    g_bc = C.sb([128, 1024], F32)
    b_bc = C.sb([128, 1024], F32)
    P.dma(g_bc, g_dram.partition_broadcast(128), writes=[(tag, "g")])
    P.dma(b_bc, b_dram.partition_broadcast(128), writes=[(tag, "b")])
    scratch = [(C.sb([128, 2, 6], F32), C.sb([128, 2], F32), C.sb([128, 1], F32), C.sb([128, 1], F32)) for _ in range(nbuf)]
    return g_bc, b_bc, scratch


def phase_B(C, A):
    P = C.P
    P.label = "B"
    P.fence()
    m0 = C.mark()
    mT = C.sb([128, 8, S], F32R)
    wo = C.sb([128, 8, 1024], F32R)
    P.dma(wo, A["w_out"], writes=["wo"], eng="pool")
    g_bc, b_bc, scratch = ln_setup(C, A["ln1_g"], A["ln1_b"], "ln1", nbuf=4)
    xt = [C.sb([128, 1024], F32) for _ in range(4)]
    pre = [C.sb([128, 1024], F32) for _ in range(4)]
    to_featmajor(C, A["merged"], mT, "mT")
    lnp = LNPipe(C, g_bc, b_bc, "ln1", scratch)
    for t in range(NT):
        b = t % 4
        pb = t % 2
        P.dma(xt[b], A["x"][t * 128:(t + 1) * 128, :], writes=[("Bx", b)])
        for half in range(2):
            bk = 4 + 2 * pb + half
            for kc in range(8):
                P.op("pe", lambda e, bk=bk, kc=kc, t=t, half=half: e.matmul(C.bank(bk), mT[:, kc, t * 128:(t + 1) * 128],
                                                                          wo[:, kc, half * 512:(half + 1) * 512], start=(kc == 0), stop=(kc == 7)),
                     reads=[("mT", t, kc // 4), "wo"], writes=[("psum", bk)])
        P.op("dve", lambda e, b=b, pb=pb: e.scalar_tensor_tensor(pre[b], xt[b], DN_ALPHA, C.bank(4 + 2 * pb, 2), ALU.mult, ALU.add),
             reads=[("Bx", b), ("psum", 4 + 2 * pb), ("psum", 5 + 2 * pb)], writes=[("Bpre", b)])
        lnp.push(pre[b], xt[b], [("Bpre", b)], [("Bx", b)], b,
                 after_fn=(lambda t=t, b=b: P.dma(A["h1"][t * 128:(t + 1) * 128, :], xt[b], reads=[("Bx", b)], eng="pool")))
    lnp.flush()
    C.release(m0)


def phase_C(C, A):
    P = C.P
    P.label = "C"
    P.fence()
    m0 = C.mark()
    h1Tb = [C.sb([128, 8, 512], F32R) for _ in range(2)]
    memT = C.sb([128, 8, 256], F32R)
    kT = C.sb([128, 8, 256], F32R)
    V = C.sb([128, 2, 1024], F32R)
    wq = C.sb([128, 8, 1024], F32R)
    wkv = C.sb([128, 8, 1024], F32R)
    wo = wkv
    P.dma(wkv, A["xa_wk"], writes=["wkv"], eng="pool")
    P.dma(wq, A["xa_wq"], writes=["wq"], eng="pool")
    to_featmajor(C, A["mem"], memT, "memT", ntiles=2)
    rd_memT = [("memT", t, hf) for t in range(2) for hf in range(2)]
    for j in range(8):
        bk = j % 2
        for kc in range(8):
            P.op("pe", lambda e, bk=bk, kc=kc, j=j: e.matmul(C.bank(bk)[:, 0:256], wkv[:, kc, j * 128:(j + 1) * 128], memT[:, kc, :],
                                                          start=(kc == 0), stop=(kc == 7)),
                 reads=["wkv"] + rd_memT, writes=[("psum", bk)])
        copy_op(P, C.evac_eng(), kT[:, j, :], C.bank(bk)[:, 0:256], reads=[("psum", bk)], writes=[("kT", j)])
    P.dma(wkv, A["xa_wv"], writes=["wkv"], eng="pool")
    for mt in range(2):
        for half in range(2):
            bk = 2 + half
            for kc in range(8):
                P.op("pe", lambda e, bk=bk, kc=kc, mt=mt, half=half: e.matmul(C.bank(bk), memT[:, kc, mt * 128:(mt + 1) * 128],
                                                                            wkv[:, kc, half * 512:(half + 1) * 512], start=(kc == 0), stop=(kc == 7)),
                     reads=["wkv"] + rd_memT, writes=[("psum", bk)])
            copy_op(P, C.evac_eng(), V[:, mt, half * 512:(half + 1) * 512], C.bank(bk), reads=[("psum", bk)], writes=[("V", mt, half)])
    C.dbg("kT", kT, [("kT", j) for j in range(8)])
    C.dbg("V", V, [("V", a, b_) for a in range(2) for b_ in range(2)])
    C.dbg("memT", memT, rd_memT)
    P.dma(wo, A["xa_wo"], writes=["wkv"], eng="pool")
    g_bc, b_bc, scratch = ln_setup(C, A["ln2_g"], A["ln2_b"], "ln2", nbuf=4)
    xtf = [C.sb([128, 1024], F32) for _ in range(2)]
    qT = [C.sb([128, 2, 512], F32R) for _ in range(2)]
    ex = [C.sb([128, 2, 512], F32R) for _ in range(2)]
    rden = [C.sb([128, 512], F32) for _ in range(2)]
    oT = C.sb([128, 8, 512], F32R)
    xt = [C.sb([128, 1024], F32) for _ in range(4)]
    pre = [C.sb([128, 1024], F32) for _ in range(4)]
    it = 0
    lnp = LNPipe(C, g_bc, b_bc, "ln2", scratch)
    for tb in range(4):
        h1T = h1Tb[tb % 2]
        hname = "h1T%d" % (tb % 2)
        rd_h = to_featmajor(C, A["h1"][tb * 512:(tb + 1) * 512, :], h1T, hname, ntiles=4, xt_bufs=xtf)
        for h in range(4):
            b = it % 2
            it += 1
            for c in range(2):
                bk = c
                for kc in range(8):
                    P.op("pe", lambda e, bk=bk, kc=kc, h=h, c=c, h1T=h1T: e.matmul(C.bank(bk), wq[:, kc, h * 256 + c * 128:h * 256 + (c + 1) * 128],
                                                                               h1T[:, kc, :], start=(kc == 0), stop=(kc == 7)),
                         reads=["wq"] + rd_h, writes=[("psum", bk)])
                P.op("act", lambda e, bk=bk, b=b, c=c: e.activation(qT[b][:, c, :], C.bank(bk), AF.Identity, scale=1.0 / 16.0),
                     reads=[("psum", bk)], writes=[("qT", b, c)])
            for mt in range(2):
                bk = 2 + mt
                for c in range(2):
                    P.op("pe", lambda e, bk=bk, c=c, mt=mt, h=h, b=b: e.matmul(C.bank(bk), kT[:, h * 2 + c, mt * 128:(mt + 1) * 128], qT[b][:, c, :],
                                                                             start=(c == 0), stop=(c == 1)),
                         reads=[("kT", h * 2 + c), ("qT", b, c)], writes=[("psum", bk)])
                P.op("act", lambda e, bk=bk, b=b, mt=mt: e.activation(ex[b][:, mt, :], C.bank(bk), AF.Exp), reads=[("psum", bk)], writes=[("ex", b, mt)])
            for mt in range(2):
                P.op("pe", lambda e, mt=mt, b=b: e.matmul(C.bank(4), C.onesr, ex[b][:, mt, :], start=(mt == 0), stop=(mt == 1)),
                     reads=["onesr", ("ex", b, mt)], writes=[("psum", 4)])
            P.op("dve", lambda e, b=b: e.reciprocal(rden[b], C.bank(4)), reads=[("psum", 4)], writes=[("rden", b)])
            for c in range(2):
                bk = 5 + c
                for mt in range(2):
                    P.op("pe", lambda e, bk=bk, mt=mt, c=c, h=h, b=b: e.matmul(C.bank(bk), V[:, mt, h * 256 + c * 128:h * 256 + (c + 1) * 128], ex[b][:, mt, :],
                                                                             start=(mt == 0), stop=(mt == 1)),
                         reads=[("V", mt, (h * 256 + c * 128) // 512), ("ex", b, mt)], writes=[("psum", bk)])
                P.op("dve", lambda e, bk=bk, b=b, h=h, c=c: e.tensor_tensor(oT[:, h * 2 + c, :], C.bank(bk), rden[b], ALU.mult),
                     reads=[("psum", bk), ("rden", b)], writes=[("oT", h * 2 + c)])
        if tb == 0:
            C.dbg("oT", oT, [("oT", j) for j in range(8)])
            C.dbg("qT", qT[1], [("qT", 1, 0), ("qT", 1, 1)])
            C.dbg("ex", ex[1], [("ex", 1, 0), ("ex", 1, 1)])
            C.dbg("rden", rden[1], [("rden", 1)])
            C.dbg("h1T", h1T, rd_h)
        for tt in range(4):
            t = tb * 4 + tt
            b = t % 4
            pb = t % 2
            P.dma(xt[b], A["h1"][t * 128:(t + 1) * 128, :], writes=[("Cx", b)])
            for half in range(2):
                bk = 0 + half if pb == 0 else 2 + half
                for j in range(8):
                    P.op("pe", lambda e, bk=bk, j=j, tt=tt, half=half: e.matmul(C.bank(bk), oT[:, j, tt * 128:(tt + 1) * 128],
                                                                              wo[:, j, half * 512:(half + 1) * 512], start=(j == 0), stop=(j == 7)),
                         reads=[("oT", j), "wkv"], writes=[("psum", bk)])
            b0 = 0 if pb == 0 else 2
            P.op("dve", lambda e, b=b, b0=b0: e.scalar_tensor_tensor(pre[b], xt[b], DN_ALPHA, C.bank(b0, 2), ALU.mult, ALU.add),
                 reads=[("Cx", b), ("psum", b0), ("psum", b0 + 1)], writes=[("Cpre", b)])
            lnp.push(pre[b], xt[b], [("Cpre", b)], [("Cx", b)], b,
                     after_fn=(lambda t=t, b=b: P.dma(A["h2"][t * 128:(t + 1) * 128, :], xt[b], reads=[("Cx", b)], eng="pool")))
    lnp.flush()
    C.release(m0)


def phase_D(C, A):
    P = C.P
    P.label = "Drouter"
    P.fence()
    m0 = C.mark()
    h2T = C.sb([128, 8, S], F32R)
    acc = C.sb([128, NT, 1024], F32)
    comb = C.sb([128, NT, 32], F32)
    wg = [C.sb([128, 8, 256], F32R) for _ in range(2)]
    wu = [C.sb([128, 8, 256], F32R) for _ in range(2)]
    wd = [C.sb([128, 2, 1024], F32R) for _ in range(2)]
    for ex_ in range(2):
        P.dma(wg[ex_], A["moe_wg"][ex_], writes=[("wg", ex_)], eng="pool")
        P.dma(wu[ex_], A["moe_wu"][ex_], writes=[("wu", ex_)], eng="pool")
        P.dma(wd[ex_], A["moe_wd"][ex_], writes=[("wd", ex_)], eng="pool")
    m1 = C.mark()
    wr = C.sb([128, 8, 36], F32R)
    P.dma(wr, A["moe_wr"], writes=["wr"], eng="pool")
    br = C.sb([128, 36], F32)
    P.dma(br, A["moe_br"].partition_broadcast(128), writes=["br"])
    rd_all = to_featmajor(C, A["h2"], h2T, "h2T")
    lg = C.sb([128, NT, 36], F32)
    for t in range(NT):
        bk = t // 8
        for kc in range(8):
            P.op("pe", lambda e, bk=bk, kc=kc, t=t: e.matmul(C.bank(bk)[:, (t % 8) * 64:(t % 8) * 64 + 36], h2T[:, kc, t * 128:(t + 1) * 128], wr[:, kc, :],
                                                          start=(kc == 0), stop=(kc == 7)),
                 reads=[("h2T", t, kc // 4), "wr"], writes=[("psum", bk)])
    for bk in range(2):
        P.op("dve", lambda e, bk=bk: e.tensor_tensor(lg[:, bk * 8:(bk + 1) * 8, :], C.bank(bk).rearrange("p (t c) -> p t c", c=64)[:, :, 0:36],
                                                     br.unsqueeze(1).to_broadcast([128, 8, 36]), ALU.add),
             reads=[("psum", bk), "br"], writes=[("lg", bk)])
    rd_lg = [("lg", 0), ("lg", 1)]
    gmx = C.sb([128, NT], F32)
    gex = C.sb([128, NT, 4], F32)
    gsum = C.sb([128, NT], F32)
    ptop = C.sb([128, NT], F32)
    ohg = C.sb([128, NT, 4], F32)
    sel = C.sb([128, NT, 4, 8], F32)
    el = C.sb([128, NT, 8], F32)
    e1 = C.sb([128, NT], F32)
    e2 = C.sb([128, NT], F32)
    oh1 = C.sb([128, NT, 8], F32)
    oh2 = C.sb([128, NT, 8], F32)
    el2 = C.sb([128, NT, 8], F32)
    dd = C.sb([128, NT], F32)
    w1 = C.sb([128, NT], F32)
    w2 = C.sb([128, NT], F32)
    c8 = C.sb([128, NT, 8], F32)
    lgg = lg[:, :, 0:4]
    lge = lg[:, :, 4:36].rearrange("p t (g e) -> p t g e", g=4)
    V = "dve"
    P.op(V, lambda e: e.tensor_reduce(gmx, lgg, AX.X, ALU.max), reads=rd_lg, writes=["gmx"])
    P.op(V, lambda e: e.tensor_tensor(gex, lgg, gmx.unsqueeze(2).to_broadcast([128, NT, 4]), ALU.subtract), reads=rd_lg + ["gmx"], writes=["gex"])
    P.op(V, lambda e: e.tensor_tensor(ohg, lgg, gmx.unsqueeze(2).to_broadcast([128, NT, 4]), ALU.is_equal), reads=rd_lg + ["gmx"], writes=["ohg"])
    P.op("act", lambda e: e.activation(gex, gex, AF.Exp), reads=["gex"], writes=["gex"])
    P.op(V, lambda e: e.tensor_reduce(gsum, gex, AX.X, ALU.add), reads=["gex"], writes=["gsum"])
    P.op(V, lambda e: e.reciprocal(ptop, gsum), reads=["gsum"], writes=["ptop"])
    P.op(V, lambda e: e.tensor_tensor(sel, lge, ohg.unsqueeze(3).to_broadcast([128, NT, 4, 8]), ALU.mult), reads=rd_lg + ["ohg"], writes=["sel"])
    P.op(V, lambda e: e.tensor_reduce(el, sel.rearrange("p t g e -> p t e g"), AX.X, ALU.add), reads=["sel"], writes=["el"])
    P.op(V, lambda e: e.tensor_reduce(e1, el, AX.X, ALU.max), reads=["el"], writes=["e1"])
    P.op(V, lambda e: e.tensor_tensor(oh1, el, e1.unsqueeze(2).to_broadcast([128, NT, 8]), ALU.is_equal), reads=["el", "e1"], writes=["oh1"])
    P.op(V, lambda e: e.scalar_tensor_tensor(el2, oh1, -1e30, el, ALU.mult, ALU.add), reads=["oh1", "el"], writes=["el2"])
    P.op(V, lambda e: e.tensor_reduce(e2, el2, AX.X, ALU.max), reads=["el2"], writes=["e2"])
    P.op(V, lambda e: e.tensor_tensor(oh2, el2, e2.unsqueeze(2).to_broadcast([128, NT, 8]), ALU.is_equal), reads=["el2", "e2"], writes=["oh2"])
    P.op(V, lambda e: e.tensor_tensor(dd, e2, e1, ALU.subtract), reads=["e1", "e2"], writes=["dd"])
    P.op("act", lambda e: e.activation(dd, dd, AF.Exp), reads=["dd"], writes=["dd"])
    P.op(V, lambda e: e.tensor_scalar(w1, dd, 1.0, None, ALU.add), reads=["dd"], writes=["w1"])
    P.op(V, lambda e: e.reciprocal(w1, w1), reads=["w1"], writes=["w1"])
    P.op(V, lambda e: e.tensor_tensor(w1, w1, ptop, ALU.mult), reads=["w1", "ptop"], writes=["w1"])
    P.op(V, lambda e: e.tensor_tensor(w2, w1, dd, ALU.mult), reads=["w1", "dd"], writes=["w2"])
    P.op(V, lambda e: e.tensor_tensor(oh1, oh1, w1.unsqueeze(2).to_broadcast([128, NT, 8]), ALU.mult), reads=["oh1", "w1"], writes=["oh1"])
    P.op(V, lambda e: e.tensor_tensor(oh2, oh2, w2.unsqueeze(2).to_broadcast([128, NT, 8]), ALU.mult), reads=["oh2", "w2"], writes=["oh2"])
    P.op(V, lambda e: e.tensor_tensor(c8, oh1, oh2, ALU.add), reads=["oh1", "oh2"], writes=["c8"])
    P.op(V, lambda e: e.tensor_tensor(comb.rearrange("p t (g e) -> p t g e", g=4), ohg.unsqueeze(3).to_broadcast([128, NT, 4, 8]),
                                      c8.unsqueeze(2).to_broadcast([128, NT, 4, 8]), ALU.mult), reads=["ohg", "c8"], writes=["comb"])
    P.op("pool", lambda e: e.memset(acc, 0.0), writes=[("acc", t) for t in range(NT)])
    P.fence()
    C.release(m1)
    P.label = "Dexperts"
    sg = [C.sb([128, 512], F32) for _ in range(2)]
    hg = [C.sb([128, 2, 512], F32R) for _ in range(2)]
    it = 0
    dn = 0
    for ex in range(32):
        wb = ex % 2
        if ex >= 2:
            P.dma(wg[wb], A["moe_wg"][ex], writes=[("wg", wb)], eng="pool")
            P.dma(wu[wb], A["moe_wu"][ex], writes=[("wu", wb)], eng="pool")
            P.dma(wd[wb], A["moe_wd"][ex], writes=[("wd", wb)], eng="pool")
        for tb in range(4):
            hb = it % 2
            it += 1
            rd_h = [("h2T", t, hf) for t in range(tb * 4, tb * 4 + 4) for hf in range(2)]
            for fc in range(2):
                for kc in range(8):
                    P.op("pe", lambda e, fc=fc, kc=kc, wb=wb, tb=tb: e.matmul(C.bank(fc), wg[wb][:, kc, fc * 128:(fc + 1) * 128], h2T[:, kc, tb * 512:(tb + 1) * 512],
                                                                            start=(kc == 0), stop=(kc == 7)),
                         reads=[("wg", wb)] + rd_h, writes=[("psum", fc)])
                for kc in range(8):
                    P.op("pe", lambda e, fc=fc, kc=kc, wb=wb, tb=tb: e.matmul(C.bank(2 + fc), wu[wb][:, kc, fc * 128:(fc + 1) * 128], h2T[:, kc, tb * 512:(tb + 1) * 512],
                                                                            start=(kc == 0), stop=(kc == 7)),
                         reads=[("wu", wb)] + rd_h, writes=[("psum", 2 + fc)])
                P.op("act", lambda e, fc=fc: e.activation(sg[fc], C.bank(fc), AF.Silu), reads=[("psum", fc)], writes=[("sg", fc)])
                P.op("dve", lambda e, fc=fc, hb=hb: e.tensor_tensor(hg[hb][:, fc, :], C.bank(2 + fc), sg[fc], ALU.mult),
                     reads=[("psum", 2 + fc), ("sg", fc)], writes=[("hg", hb, fc)])
            for tt in range(4):
                t = tb * 4 + tt
                b0 = 4 + 2 * (dn % 2)
                dn += 1
                for half in range(2):
                    for fc in range(2):
                        P.op("pe", lambda e, b0=b0, half=half, fc=fc, hb=hb, tt=tt, wb=wb: e.matmul(C.bank(b0 + half), hg[hb][:, fc, tt * 128:(tt + 1) * 128],
                                                                                                 wd[wb][:, fc, half * 512:(half + 1) * 512], start=(fc == 0), stop=(fc == 1)),
                             reads=[("hg", hb, fc), ("wd", wb)], writes=[("psum", b0 + half)])
                P.op("dve", lambda e, b0=b0, t=t, ex=ex: e.scalar_tensor_tensor(acc[:, t, :], C.bank(b0, 2), comb[:, t, ex:ex + 1], acc[:, t, :], ALU.mult, ALU.add),
                     reads=[("psum", b0), ("psum", b0 + 1), "comb", ("acc", t)], writes=[("acc", t)])
    P.fence()
    C.release(m1)
    P.label = "Dln3"
    g_bc, b_bc, scratch = ln_setup(C, A["ln3_g"], A["ln3_b"], "ln3", nbuf=4)
    xt = [C.sb([128, 1024], F32) for _ in range(4)]
    lnp = LNPipe(C, g_bc, b_bc, "ln3", scratch)
    for t in range(NT):
        b = t % 4
        P.dma(xt[b], A["h2"][t * 128:(t + 1) * 128, :], writes=[("Dx", b)])
        P.op("dve", lambda e, b=b, t=t: e.scalar_tensor_tensor(acc[:, t, :], xt[b], DN_ALPHA, acc[:, t, :], ALU.mult, ALU.add),
             reads=[("Dx", b), ("acc", t)], writes=[("acc", t)])
        lnp.push(acc[:, t, :], xt[b], [("acc", t)], [("Dx", b)], b,
                 after_fn=(lambda t=t, b=b: P.dma(A["out"][t * 128:(t + 1) * 128, :], xt[b], reads=[("Dx", b)], eng="pool")))
    lnp.flush()
    C.release(m0)


SCALE_DH = 128.0 ** -0.5
BIGM = 1.0e4
TWO_PI = 6.283185307179586
RC1 = 6.28125
RC2 = TWO_PI - RC1


def cast_copy(P, eng, out, in_, reads, writes):
    return P.op(eng, lambda e: e.tensor_copy(out, in_), reads, writes)


def phase_A0(C, A):
    P = C.P
    P.label = "A0"
    P.fence()
    C.mA0 = C.mark()
    C.xT = C.sb([128, 8, S], F32R)
    C.rd_xT = to_featmajor(C, A["x"], C.xT, "xT")
    C.rd_xT_tb = [[("xT", t, hf) for t in range(tb * 4, tb * 4 + 4) for hf in range(2)] for tb in range(4)]


def proj_tok_to_dram(C, W_dram, ncols, func, dst, col0):
    P = C.P
    P.label = "A1"
    m = C.mark()
    w = C.sb([128, 8, ncols], F32R)
    P.dma(w, W_dram, writes=["ptw"], eng="pool")
    ot = [C.sb([128, ncols], F32) for _ in range(2)]
    nb = ncols // 512
    for t in range(NT):
        b = t % 2
        for j in range(nb):
            bk = (t % 2) * 2 + j
            for kc in range(8):
                P.op("pe", lambda e, bk=bk, kc=kc, t=t, j=j: e.matmul(C.bank(bk), C.xT[:, kc, t * 128:(t + 1) * 128], w[:, kc, j * 512:(j + 1) * 512],
                                                                    start=(kc == 0), stop=(kc == 7)),
                     reads=[("xT", t, kc // 4), "ptw"], writes=[("psum", bk)])
            P.op("act", lambda e, bk=bk, b=b, j=j: e.activation(ot[b][:, j * 512:(j + 1) * 512], C.bank(bk), func), reads=[("psum", bk)], writes=[("pto", b, j)])
        P.dma(dst[t * 128:(t + 1) * 128, col0:col0 + ncols], ot[b], reads=[("pto", b, j) for j in range(nb)])
    P.fence()
    C.release(m)


def phase_A1(C, A):
    P = C.P
    proj_tok_to_dram(C, A["w_mgb"], 1024, AF.Sigmoid, A["gates"], 1024)
    P.label = "A1"
    m = C.mark()
    wa = C.sb([128, 8, 1024], F32R)
    wz = C.sb([128, 8, 1024], F32R)
    P.dma(wa, A["w_mga"], writes=["wa"], eng="pool")
    P.dma(wz, A["w_z"], writes=["wz"], eng="pool")
    nwb = C.sb([128, 128], F32)
    P.dma(nwb, A["gdn_norm_w"].partition_broadcast(128), writes=["nwb"])
    ga_t = [C.sb([128, 1024], F32) for _ in range(2)]
    z_t = [C.sb([128, 1024], F32) for _ in range(2)]
    for t in range(NT):
        b = t % 2
        for wi_, (w_, wreg, func, dst, dreg) in enumerate(((wa, "wa", AF.Sigmoid, ga_t, "ga_t"), (wz, "wz", AF.Silu, z_t, "z_t"))):
            for j in range(2):
                bk = b * 4 + wi_ * 2 + j
                for kc in range(8):
                    P.op("pe", lambda e, bk=bk, kc=kc, t=t, j=j, w_=w_: e.matmul(C.bank(bk), C.xT[:, kc, t * 128:(t + 1) * 128], w_[:, kc, j * 512:(j + 1) * 512],
                                                                               start=(kc == 0), stop=(kc == 7)),
                         reads=[("xT", t, kc // 4), wreg], writes=[("psum", bk)])
                P.op("act", lambda e, bk=bk, b=b, j=j, func=func, dst=dst: e.activation(dst[b][:, j * 512:(j + 1) * 512], C.bank(bk), func),
                     reads=[("psum", bk)], writes=[(dreg, b, j)])
        P.op("pool", lambda e, b=b: e.tensor_tensor(z_t[b], z_t[b], ga_t[b], ALU.mult), reads=[("ga_t", b, 0), ("ga_t", b, 1), ("z_t", b, 0), ("z_t", b, 1)],
             writes=[("z_t", b, 0), ("z_t", b, 1)])
        P.op("pool", lambda e, b=b: e.tensor_tensor(z_t[b].rearrange("p (h d) -> p h d", h=8), z_t[b].rearrange("p (h d) -> p h d", h=8),
                                                   nwb.unsqueeze(1).to_broadcast([128, 8, 128]), ALU.mult), reads=[("z_t", b, 0), ("z_t", b, 1), "nwb"],
             writes=[("z_t", b, 0), ("z_t", b, 1)])
        P.dma(A["zs"][t * 128:(t + 1) * 128, :], z_t[b], reads=[("z_t", b, 0), ("z_t", b, 1)])
    C.release(m)


def nsa_consts(C, A):
    P = C.P
    K = {}
    ident = C.ident
    K["R"] = C.sb([128, 128], F32R)
    K["cos"] = C.sb([128, S], F32)
    K["sin"] = C.sb([128, S], F32)
    K["cmask"] = C.sb([128, S], BF16)
    K["tri"] = C.sb([128, 128], BF16)
    K["wmask"] = C.sb([128, 3, 128], BF16)
    K["Eall"] = C.sb([32, 16, 128], BF16)
    K["M1"] = C.sb([128, 16, 32], F32)
    K["M2"] = C.sb([128, 16, 32], F32)
    K["ov"] = C.sb([128, 32], F32)
    K["zeros"] = C.sb([128, 32], F32)
    P.op("pool", lambda e: e.memset(K["zeros"], 0.0), writes=["zeros"])
    mtmp = C.mark()
    r32 = C.sb([128, 128], F32)
    P.op("pool", lambda e: e.memset(r32, 0.0), writes=["r32"])
    P.op("pool", lambda e: e.tensor_scalar(r32[64:128, 0:64], ident[64:128, 64:128], -1.0, None, ALU.mult), reads=["r32"], writes=["r32"])
    P.op("pool", lambda e: e.tensor_copy(r32[0:64, 64:128], ident[0:64, 0:64]), reads=["r32"], writes=["r32"])
    P.op("pool", lambda e: e.tensor_copy(K["R"], r32), reads=["r32"], writes=["R"])
    posi = C.sb([128, S], I32)
    P.dma(posi, A["positions"].partition_broadcast(128), writes=["posi"])
    invf = C.sb([128, 1], F32)
    P.dma(invf, A["inv_freq"], writes=["invf"])
    ang = C.sb([128, S], F32)
    u = C.sb([128, S], F32)
    ni = C.sb([128, S], I32)
    P.op("dve", lambda e: e.tensor_copy(ang, posi), reads=["posi"], writes=["ang"])
    P.op("dve", lambda e: e.tensor_scalar(ang, ang, invf, None, ALU.mult), reads=["ang", "invf"], writes=["ang"])
    for nm, shift in (("sin", 0.0), ("cos", np.pi / 2)):
        dst = K[nm]
        P.op("dve", lambda e, shift=shift: e.tensor_scalar(u, ang, shift, 1.0 / TWO_PI, ALU.add, ALU.mult), reads=["ang"], writes=["u"])
        P.op("dve", lambda e: e.tensor_copy(ni, u), reads=["u"], writes=["ni"])
        P.op("dve", lambda e: e.tensor_copy(u, ni), reads=["ni"], writes=["u"])
        P.op("dve", lambda e, dst=dst, shift=shift: e.tensor_scalar(dst, ang, shift, None, ALU.add), reads=["ang"], writes=[nm])
        P.op("dve", lambda e, dst=dst: e.scalar_tensor_tensor(dst, u, -RC1, dst, ALU.mult, ALU.add), reads=["u", nm], writes=[nm])
        P.op("dve", lambda e, dst=dst: e.scalar_tensor_tensor(dst, u, -RC2, dst, ALU.mult, ALU.add), reads=["u", nm], writes=[nm])
        P.op("dve", lambda e, dst=dst: e.tensor_scalar(dst, dst, -3.1415925, 3.1415925, ALU.max, ALU.min), reads=[nm], writes=[nm])
        P.op("act", lambda e, dst=dst: e.activation(dst, dst, AF.Sin), reads=[nm], writes=[nm])
    cm32 = C.sb([128, S], F32)
    P.op("pool", lambda e: e.memset(cm32, 1.0), writes=["cm32"])
    P.op("pool", lambda e: e.affine_select(out=cm32, in_=cm32, pattern=[[1, S]], compare_op=ALU.is_ge, fill=0.0, base=-31, channel_multiplier=-16),
         reads=["cm32"], writes=["cm32"])
    P.op("pool", lambda e: e.tensor_copy(K["cmask"], cm32), reads=["cm32"], writes=["cmask"])
    tri32 = C.sb([128, 128], F32)
    P.op("pool", lambda e: e.memset(tri32, 1.0), writes=["tri32"])
    P.op("pool", lambda e: e.affine_select(out=tri32, in_=tri32, pattern=[[1, 128]], compare_op=ALU.is_ge, fill=0.0, base=0, channel_multiplier=-1),
         reads=["tri32"], writes=["tri32"])
    P.op("pool", lambda e: e.tensor_copy(K["tri"], tri32), reads=["tri32"], writes=["tri"])
    P.op("pool", lambda e: e.tensor_scalar(K["wmask"][:, 0, :], tri32, -1.0, 1.0, ALU.mult, ALU.add), reads=["tri32"], writes=["wmask"])
    P.op("pool", lambda e: e.memset(K["wmask"][:, 1, :], 1.0), reads=["wmask"], writes=["wmask"])
    P.op("pool", lambda e: e.tensor_copy(K["wmask"][:, 2, :], tri32), reads=["wmask", "tri32"], writes=["wmask"])
    e32 = C.sb([32, 16, 2, 64], F32)
    P.op("pool", lambda e: e.memset(e32, 0.0), writes=["e32"])
    P.op("pool", lambda e: e.affine_select(out=e32, in_=e32, pattern=[[-2, 16], [-1, 2], [0, 64]], compare_op=ALU.not_equal, fill=1.0, base=0, channel_multiplier=1),
         reads=["e32"], writes=["e32"])
    P.op("pool", lambda e: e.tensor_copy(K["Eall"], e32.rearrange("p a b c -> p a (b c)")), reads=["e32"], writes=["Eall"])
    dI = C.sb([128, 16, 32], F32)
    P.op("pool", lambda e: e.iota(dI[0:64], pattern=[[-2, 16], [1, 32]], base=0, channel_multiplier=0, allow_small_or_imprecise_dtypes=True), writes=["dI"])
    P.op("pool", lambda e: e.iota(dI[64:128], pattern=[[-2, 16], [1, 32]], base=-1, channel_multiplier=0, allow_small_or_imprecise_dtypes=True), reads=["dI"], writes=["dI"])
    f1 = C.sb([128, 16, 32], F32)
    f2 = C.sb([128, 16, 32], F32)
    P.op("dve", lambda e: e.tensor_scalar(f1, dI, 0.0, None, ALU.is_equal), reads=["dI"], writes=["f1"])
    P.op("dve", lambda e: e.tensor_scalar(f2, dI, -1.0, None, ALU.is_equal), reads=["dI"], writes=["f2"])
    P.op("dve", lambda e: e.tensor_tensor(f1, f1, f2, ALU.max), reads=["f1", "f2"], writes=["f1"])
    P.op("dve", lambda e: e.memset(f1[:, :, 0:1], 1.0), reads=["f1"], writes=["f1"])
    P.op("dve", lambda e: e.tensor_scalar(f2, dI, 0.0, None, ALU.is_gt), reads=["dI", "f1"], writes=["f2"])
    P.op("dve", lambda e: e.tensor_tensor(K["M2"], f1, f2, ALU.subtract), reads=["f1", "f2"], writes=["M2"])
    P.op("dve", lambda e: e.tensor_tensor(K["M1"], f1, f2, ALU.add), reads=["f1", "f2"], writes=["M1"])
    P.op("dve", lambda e: e.tensor_scalar(K["M1"], K["M1"], -1.0, 1.0, ALU.mult, ALU.add), reads=["M1"], writes=["M1"])
    P.op("dve", lambda e: e.tensor_scalar(K["M2"], K["M2"], BIGM, None, ALU.mult), reads=["M2"], writes=["M2"])
    a1 = C.sb([128, 32], F32)
    a2 = C.sb([128, 32], F32)
    ov = K["ov"]
    P.op("pool", lambda e: e.iota(a1, pattern=[[0, 32]], base=32, channel_multiplier=16, allow_small_or_imprecise_dtypes=True), writes=["a1"])
    P.op("pool", lambda e: e.iota(a2, pattern=[[64, 32]], base=64, channel_multiplier=0, allow_small_or_imprecise_dtypes=True), writes=["a2"])
    P.op("dve", lambda e: e.tensor_tensor(ov, a1, a2, ALU.min), reads=["a1", "a2"], writes=["ov"])
    P.op("dve", lambda e: e.tensor_scalar(a1, a1, -32.0, None, ALU.add), reads=["a1", "ov"], writes=["a1"])
    P.op("dve", lambda e: e.tensor_scalar(a2, a2, -64.0, None, ALU.add), reads=["a2", "ov"], writes=["a2"])
    P.op("dve", lambda e: e.tensor_tensor(a1, a1, a2, ALU.max), reads=["a1", "a2"], writes=["a1"])
    P.op("dve", lambda e: e.tensor_tensor(ov, ov, a1, ALU.subtract), reads=["ov", "a1"], writes=["ov"])
    P.op("dve", lambda e: e.tensor_scalar(ov, ov, 0.0, 1.0 / 32.0, ALU.max, ALU.mult), reads=["ov"], writes=["ov"])
    C.dbg("cos", K["cos"], ["cos"])
    C.dbg("sin", K["sin"], ["sin"])
    C.dbg("M1", K["M1"], ["M1"])
    C.dbg("M2", K["M2"], ["M2"])
    C.dbg("ov", ov, ["ov"])
    P.fence()
    C.release(mtmp)
    return K


def proj_feat(C, w, wreg, col0, dst_fn, rope=None, K=None, tag="pf"):
    P = C.P
    pend = None
    for tb in range(4):
        bk = tb % 2
        for kc in range(8):
            P.op("pe", lambda e, bk=bk, kc=kc, tb=tb: e.matmul(C.bank(bk), w[:, kc, col0:col0 + 128], C.xT[:, kc, tb * 512:(tb + 1) * 512],
                                                             start=(kc == 0), stop=(kc == 7)),
                 reads=[wreg] + C.rd_xT_tb[tb], writes=[("psum", bk)])
        out, oreg = dst_fn(tb)
        if rope is None:
            copy_op(P, C.evac_eng(), out, C.bank(bk), reads=[("psum", bk)], writes=oreg)
        else:
            raw, t1, t2 = rope
            b = tb % 2
            P.op("act", lambda e, bk=bk, b=b: e.copy(raw[b], C.bank(bk)), reads=[("psum", bk)], writes=[("rraw", b)])

            def epi(bk=bk, b=b, tb=tb, out=out, oreg=oreg):
                P.op("pe", lambda e: e.matmul(C.bank(2 + bk), K["R"], raw[b], start=True, stop=True), reads=["R", ("rraw", b)], writes=[("psum", 2 + bk)])
                P.op("pool", lambda e: e.tensor_tensor(t1[b], raw[b].bitcast(F32), K["cos"][:, tb * 512:(tb + 1) * 512], ALU.mult),
                     reads=[("rraw", b), "cos"], writes=[("rt1", b)])
                P.op("dve", lambda e: e.tensor_tensor(t2[b], C.bank(2 + bk), K["sin"][:, tb * 512:(tb + 1) * 512], ALU.mult),
                     reads=[("psum", 2 + bk), "sin"], writes=[("rt2", b)])
                P.op("dve", lambda e: e.tensor_tensor(out, t1[b], t2[b], ALU.add), reads=[("rt1", b), ("rt2", b)], writes=oreg)

            if pend is not None:
                pend()
            pend = epi
    if pend is not None:
        pend()


def gelu_tanh(C, out, in_psum, bias, tmp, rd, wr, tag):
    P = C.P
    x, x2 = tmp
    c2 = 2.0 * 0.7978845608028654
    P.op("act", lambda e: e.activation(x, in_psum, AF.Identity, bias=bias), reads=rd, writes=[(tag, "x")])
    P.op("dve", lambda e: e.tensor_tensor(x2, x, x, ALU.mult), reads=[(tag, "x")], writes=[(tag, "x2")])
    P.op("dve", lambda e: e.tensor_scalar(x2, x2, 0.044715 * c2, c2, ALU.mult, ALU.add), reads=[(tag, "x2")], writes=[(tag, "x2")])
    P.op("dve", lambda e: e.tensor_tensor(x2, x2, x, ALU.mult), reads=[(tag, "x2"), (tag, "x")], writes=[(tag, "x2")])
    P.op("act", lambda e: e.activation(x2, x2, AF.Sigmoid), reads=[(tag, "x2")], writes=[(tag, "x2")])
    P.op("dve", lambda e: e.tensor_tensor(out, x2, x, ALU.mult), reads=[(tag, "x2"), (tag, "x")], writes=wr)


def phase_A2(C, A):
    P = C.P
    P.label = "A2c"
    P.fence()
    m0 = C.mark()
    K = nsa_consts(C, A)
    ng = C.sb([128, NT, 24], F32)
    wng = C.sb([128, 8, 24], F32R)
    P.dma(wng, A["w_ng"], writes=["wng"], eng="pool")
    for t in range(NT):
        for kc in range(8):
            P.op("pe", lambda e, kc=kc, t=t: e.matmul(C.bank(7)[:, t * 24:(t + 1) * 24], C.xT[:, kc, t * 128:(t + 1) * 128], wng[:, kc, :],
                                                    start=(kc == 0), stop=(kc == 7)),
                 reads=[("xT", t, kc // 4), "wng"], writes=[("psum", 7)])
    P.op("act", lambda e: e.activation(ng.rearrange("p t c -> p (t c)"), C.bank(7)[:, 0:NT * 24], AF.Sigmoid), reads=[("psum", 7)], writes=["ng"])
    wbuf = C.sb([128, 8, 512], F32R)
    vcx = C.sb([128, 162], F32R)
    vcx32 = C.sb([128, 162], F32)
    kcc = C.sb([128, 128], F32R)
    raw = [C.sb([128, 512], F32R) for _ in range(2)]
    t1 = [C.sb([128, 512], F32) for _ in range(2)]
    t2 = [C.sb([128, 512], F32) for _ in range(2)]
    rope = (raw, t1, t2)
    m1 = C.mark()
    def nsa_group(g):
        P.label = "A2s1"
        P.fence()
        C.release(m1)
        ms = C.mark()
        xk = C.sb([128, S], F32)
        xv = C.sb([128, S], F32)
        P.dma(wbuf[:, :, 0:256], A["w_nsa_c"][g], writes=[("wbuf", 0)], eng="pool")
        P.dma(wbuf[:, :, 256:512], A["w_nsa_k"][g], writes=[("wbuf", 1)], eng="pool")
        proj_feat(C, wbuf, ("wbuf", 0), 0, lambda tb: (xk[:, tb * 512:(tb + 1) * 512], [("xk", tb)]), rope=rope, K=K)
        proj_feat(C, wbuf, ("wbuf", 0), 128, lambda tb: (xv[:, tb * 512:(tb + 1) * 512], [("xv", tb)]))
        pe_sb = C.sb([32, 2, 128], F32)
        P.dma(pe_sb, A["cmp_pe"].rearrange("j l d -> l j d"), writes=["pe_sb"])
        peT = C.sb([128, 2, 32], F32)
        for j in range(2):
            P.op("pe", lambda e, j=j: e.transpose(C.bank(6)[:, j * 32:(j + 1) * 32], pe_sb[:, j, :], C.ident[0:32, 0:32]), reads=["pe_sb", "ident"], writes=[("psum", 6)])
        P.op("dve", lambda e: e.tensor_copy(peT.rearrange("p j l -> p (j l)"), C.bank(6)[:, 0:64]), reads=[("psum", 6)], writes=["peT"])
        b1 = C.sb([128, 2, 2], F32)
        P.dma(b1, A["cmp_b1"], writes=["b1"])
        w2 = C.sb([128, 2, 2, 128], F32R)
        P.dma(w2, A["cmp_w2"].rearrange("j (c p) d -> p j c d", p=128), writes=["w2"], eng="pool")
        xim = C.sb([128, 32, 128], F32R)
        P.op("pool", lambda e: e.tensor_copy(xim[:, :, 127:128], K["zeros"].unsqueeze(2)), reads=["zeros"], writes=[("xim", l) for l in range(32)])
        w1c = [C.sb([128, 8, 256], F32R) for _ in range(2)]
        hid = C.sb([128, 2, 128], F32R)
        gx = (C.sb([128, 128], F32), C.sb([128, 128], F32))
        for j, src in ((0, xk), (1, xv)):
            for l in range(32):
                P.op("pool" if l % 2 else "dve", lambda e, l=l, j=j, src=src: e.tensor_scalar(xim[:, l, 0:127], src[:, l:l + 16 * 126 + 1:16], peT[:, j, l:l + 1], None, ALU.add),
                     reads=[("xk" if j == 0 else "xv", tb) for tb in range(4)] + ["peT", ("xim", l)], writes=[("xim", l)])
            for ch in range(4):
                wb = ch % 2
                P.dma(w1c[wb], A["cmp_w1"][j, :, ch * 8:(ch + 1) * 8, :], writes=[("w1c", wb)], eng="pool")
                for ll in range(8):
                    l = ch * 8 + ll
                    for fc in range(2):
                        P.op("pe", lambda e, l=l, ll=ll, fc=fc, wb=wb: e.matmul(C.bank(4 + fc)[:, 0:128], w1c[wb][:, ll, fc * 128:(fc + 1) * 128], xim[:, l, :],
                                                                              start=(l == 0), stop=(l == 31), skip_group_check=True),
                             reads=[("w1c", wb), ("xim", l)], writes=[("psum", 4 + fc)])
            for fc in range(2):
                gelu_tanh(C, hid[:, fc, :], C.bank(4 + fc)[:, 0:128], b1[:, j, fc:fc + 1], gx, [("psum", 4 + fc), "b1"], [("hid", fc)], "gl")
            if j == 0:
                for fc in range(2):
                    P.op("pe", lambda e, fc=fc: e.matmul(C.bank(6)[:, 0:128], w2[:, 0, fc, :], hid[:, fc, :], start=(fc == 0), stop=(fc == 1)),
                         reads=["w2", ("hid", fc)], writes=[("psum", 6)])
                P.op("act", lambda e: e.copy(kcc, C.bank(6)[:, 0:128]), reads=[("psum", 6)], writes=["kcc"])
            else:
                for fc in range(2):
                    P.op("pe", lambda e, fc=fc: e.matmul(C.bank(6)[:, 0:128], hid[:, fc, :], w2[:, 1, fc, :], start=(fc == 0), stop=(fc == 1)),
                         reads=["w2", ("hid", fc)], writes=[("psum", 6)])
                P.op("pool", lambda e: e.memset(vcx32, 0.0), writes=["vcx32"])
                P.op("act", lambda e: e.copy(vcx32[:, 0:128], C.bank(6)[:, 0:128]), reads=[("psum", 6), "vcx32"], writes=["vcx32"])
                P.op("pool", lambda e: e.memset(vcx32[:, 128:129], 1.0), reads=["vcx32"], writes=["vcx32"])
                P.op("pool", lambda e: e.tensor_copy(vcx32[:, 129:161], K["ov"]), reads=["vcx32", "ov"], writes=["vcx32"])
                P.op("pool", lambda e: e.tensor_copy(vcx, vcx32), reads=["vcx32"], writes=["vcx"])
        C.dbg("kcc%d" % g, kcc, ["kcc"])
        C.dbg("vcx%d" % g, vcx, ["vcx"])
        P.label = "A2s2"
        P.fence()
        C.release(ms)
        qTb = C.sb([128, 4, S], BF16)
        ksT = C.sb([128, S], BF16)
        kwT = C.sb([128, S], BF16)
        vs = C.sb([128, NT, 130], BF16)
        vw = C.sb([128, NT, 130], BF16)
        impacc = C.sb([128, NT, 32], F32)
        selT = C.sb([32, S], BF16)
        ycs = [C.sb([128, 2, 128], F32) for _ in range(2)]
        ybt = [C.sb([128, 512], F32) for _ in range(2)]
        gbt = [C.sb([128, 512], F32) for _ in range(2)]
        qf = [C.sb([128, S], F32R) for _ in range(2)]
        ec = [C.sb([128, 512], F32) for _ in range(2)]
        ecr = [C.sb([128, 512], F32R) for _ in range(2)]
        rdn = C.sb([128, 8], F32)
        wgt = C.sb([128, 8], F32)
        itmp = C.sb([128, 2, 32], F32)
        P.dma(wbuf[:, :, 0:256], A["w_nsa_v"][g], writes=[("wbuf", 0)], eng="pool")
        proj_feat(C, wbuf, ("wbuf", 1), 256, lambda tb: (ksT[:, tb * 512:(tb + 1) * 512], [("ksT", tb)]), rope=rope, K=K)
        proj_feat(C, wbuf, ("wbuf", 1), 384, lambda tb: (kwT[:, tb * 512:(tb + 1) * 512], [("kwT", tb)]), rope=rope, K=K)
        P.dma(wbuf[:, :, 256:512], A["w_nsa_q"][g][:, :, 256:512], writes=[("wbuf", 1)], eng="pool")
        P.op("pool", lambda e: e.memset(vs[:, :, 128:130], 1.0), writes=[("vs1")])
        P.op("pool", lambda e: e.memset(vw[:, :, 128:130], 1.0), writes=[("vw1")])
        for t in range(NT):
            bk = t % 2
            for kc in range(8):
                P.op("pe", lambda e, bk=bk, kc=kc, t=t: e.matmul(C.bank(bk)[:, 0:256], C.xT[:, kc, t * 128:(t + 1) * 128], wbuf[:, kc, 0:256], start=(kc == 0), stop=(kc == 7)),
                     reads=[("xT", t, kc // 4), ("wbuf", 0)], writes=[("psum", bk)])
            P.op("act", lambda e, bk=bk, t=t: e.copy(vs[:, t, 0:128], C.bank(bk)[:, 0:128]), reads=[("psum", bk)], writes=[("vs", t)])
            P.op("dve", lambda e, bk=bk, t=t: e.tensor_copy(vw[:, t, 0:128], C.bank(bk)[:, 128:256]), reads=[("psum", bk)], writes=[("vw", t)])
        P.dma(wbuf[:, :, 0:256], A["w_nsa_q"][g][:, :, 0:256], writes=[("wbuf", 0)], eng="pool")
        rd_ks = [("ksT", tb) for tb in range(4)]
        rd_kw = [("kwT", tb) for tb in range(4)]
        for hh in (2, 3, 0, 1):
            h = g * 4 + hh
            qb = hh % 2
            proj_feat(C, wbuf, ("wbuf", hh // 2), hh * 128, lambda tb, qb=qb: (qf[qb][:, tb * 512:(tb + 1) * 512], [("qf", qb, tb)]), rope=rope, K=K)
            for tb in range(4):
                P.op("act", lambda e, qb=qb, hh=hh, tb=tb: e.copy(qTb[:, hh, tb * 512:(tb + 1) * 512], qf[qb][:, tb * 512:(tb + 1) * 512]),
                     reads=[("qf", qb, tb)], writes=[("qTb", hh, tb)])
            def cmp_a(tb, qb=qb):
                eb = tb % 2
                P.op("pe", lambda e: e.matmul(C.bank(4)[0:127, :], kcc[:, 0:127], qf[qb][:, tb * 512:(tb + 1) * 512], start=True, stop=True),
                     reads=["kcc", ("qf", qb, tb)], writes=[("psum", 4)])
                P.op("act", lambda e: e.activation(ec[eb][0:127, :], C.bank(4)[0:127, :], AF.Exp, scale=SCALE_DH), reads=[("psum", 4)], writes=[("ec", eb)])
                P.op("pool", lambda e: e.tensor_tensor(ecr[eb][0:127, :], ec[eb][0:127, :], K["cmask"][0:127, tb * 512:(tb + 1) * 512], ALU.mult),
                     reads=[("ec", eb), "cmask"], writes=[("ecr", eb)])

            cmp_a(0)
            for tb in range(4):
                eb = tb % 2
                if tb + 1 < 4:
                    cmp_a(tb + 1)
                for pr in range(2):
                    bk = 5 + pr
                    for q2 in range(2):
                        tt = pr * 2 + q2
                        P.op("pe", lambda e, bk=bk, q2=q2, tt=tt, eb=eb: e.matmul(C.bank(bk)[:, q2 * 162:(q2 + 1) * 162], ecr[eb][0:127, tt * 128:(tt + 1) * 128], vcx[0:127, :],
                                                                                start=True, stop=True, skip_group_check=True),
                             reads=[("ecr", eb), "vcx"], writes=[("psum", bk)])
                    t0 = tb * 4 + pr * 2
                    pv = C.bank(bk)[:, 0:324].rearrange("p (a c) -> p a c", a=2)
                    P.op("dve", lambda e, pv=pv: e.tensor_scalar(rdn[:, 0:2], pv[:, :, 128], 1e-30, None, ALU.max), reads=[("psum", bk)], writes=["rdn"])
                    P.op("dve", lambda e: e.reciprocal(rdn[:, 0:2], rdn[:, 0:2]), reads=["rdn"], writes=["rdn"])
                    P.op("dve", lambda e, t0=t0, h=h: e.tensor_tensor(wgt[:, 0:2], rdn[:, 0:2], ng[:, t0:t0 + 2, h * 3], ALU.mult), reads=["rdn", "ng"], writes=["wgt"])
                    P.op("dve", lambda e, pv=pv, pr=pr: e.tensor_tensor(ycs[pr], pv[:, :, 0:128], wgt[:, 0:2].unsqueeze(2).to_broadcast([128, 2, 128]), ALU.mult),
                         reads=[("psum", bk), "wgt"], writes=[("ycs", pr)])
                    P.dma(A["yb"][t0 * 128:(t0 + 2) * 128, h * 128:(h + 1) * 128].rearrange("(a p) c -> p a c", p=128), ycs[pr],
                          reads=[("ycs", pr)], writes=[("ybd", t0, hh), ("ybd", t0 + 1, hh)])
                    if hh == 2:
                        P.op("dve", lambda e, pv=pv, t0=t0: e.tensor_tensor(impacc[:, t0:t0 + 2, :], pv[:, :, 129:161], rdn[:, 0:2].unsqueeze(2).to_broadcast([128, 2, 32]), ALU.mult),
                             reads=[("psum", bk), "rdn"], writes=[("imp", t0)])
                    else:
                        P.op("dve", lambda e, pv=pv: e.tensor_tensor(itmp, pv[:, :, 129:161], rdn[:, 0:2].unsqueeze(2).to_broadcast([128, 2, 32]), ALU.mult),
                             reads=[("psum", bk), "rdn"], writes=["itmp"])
                        P.op("pool", lambda e, t0=t0: e.tensor_tensor(impacc[:, t0:t0 + 2, :], impacc[:, t0:t0 + 2, :], itmp, ALU.add), reads=["itmp", ("imp", t0)], writes=[("imp", t0)])
        C.dbg("imp%d" % g, impacc, [("imp", t0) for t0 in range(0, NT, 2)])
        P.label = "A2s3"
        rd_imp = [("imp", t0) for t0 in range(0, NT, 2)]
        P.op("dve", lambda e: e.tensor_tensor(impacc, impacc, K["M1"], ALU.mult), reads=rd_imp + ["M1"], writes=rd_imp)
        P.op("dve", lambda e: e.tensor_tensor(impacc, impacc, K["M2"], ALU.add), reads=rd_imp + ["M2"], writes=rd_imp)
        mx8 = C.sb([128, 8], F32)
        sel32 = C.sb([128, 32], F32)
        for t in range(NT):
            P.op("dve", lambda e, t=t: e.max(mx8, impacc[:, t, :]), reads=rd_imp, writes=["mx8"])
            P.op("dve", lambda e, t=t: e.tensor_scalar(sel32, impacc[:, t, :], mx8[:, 7:8], None, ALU.is_ge), reads=rd_imp + ["mx8"], writes=["sel32"])
            P.op("pe", lambda e, t=t: e.transpose(C.bank(4)[0:32, (t % 4) * 128:(t % 4 + 1) * 128], sel32, C.ident), reads=["sel32", "ident"], writes=[("psum", 4)])
            if t % 4 == 3:
                P.op("act", lambda e, t=t: e.copy(selT[:, (t - 3) * 128:(t + 1) * 128], C.bank(4)[0:32, :]), reads=[("psum", 4)], writes=[("selT", t // 4)])
        C.dbg("selT%d" % g, selT, [("selT", i) for i in range(4)])
        P.label = "A2s4"
        msk = [C.sb([128, 4, 128], BF16) for _ in range(3)]
        state = {"u": 0, "q": [], "mi": 0}
        eb16 = [C.sb([128, 4, 128], BF16) for _ in range(3)]

        def flush():
            q = state["q"]
            if q and not q[-1][0]:
                q[-1][1]()
                q[-1][0] = True
            for ent in q:
                for f in ent[2]:
                    f()
            state["q"] = []

        def unit(kts, kT_sb, kreg, v_sb, vreg, hh, tt, mask_ap, mask_regs, out_bank_ap, out_reg, first, last, post_fn):
            u = state["u"]
            state["u"] += 1
            bk = u % 2
            eb = u % 3
            n = len(kts)
            for jj, kt in enumerate(kts):
                P.op("pe", lambda e, jj=jj, kt=kt: e.matmul(C.bank(bk)[:, jj * 128:(jj + 1) * 128], kT_sb[:, kt * 128:(kt + 1) * 128],
                                                          qTb[:, hh, tt * 128:(tt + 1) * 128], start=True, stop=True, skip_group_check=True),
                     reads=[(kreg, kt // 4), ("qTb", hh, tt // 4)], writes=[("psum", bk)])
            q = state["q"]
            if q and not q[-1][0]:
                q[-1][1]()
                q[-1][0] = True
            if len(q) >= 2:
                ent = q.pop(0)
                for f in ent[2]:
                    f()

            def mid():
                P.op("act", lambda e: e.activation(eb16[eb][:, 0:n, :], C.bank(bk)[:, 0:n * 128].rearrange("p (a c) -> p a c", a=n), AF.Exp, scale=SCALE_DH),
                     reads=[("psum", bk)], writes=[("eb16", eb)])
                P.op("dve" if u % 2 == 0 else "pool", lambda e: e.tensor_tensor(eb16[eb][:, 0:n, :], eb16[eb][:, 0:n, :], mask_ap, ALU.mult),
                     reads=[("eb16", eb)] + mask_regs, writes=[("eb16", eb)])

            def pv():
                for jj, kt in enumerate(kts):
                    P.op("pe", lambda e, jj=jj, kt=kt: e.matmul(out_bank_ap, eb16[eb][:, jj, :], v_sb[:, kt, :], start=(first and jj == 0), stop=(last and jj == n - 1),
                                                              skip_group_check=True),
                         reads=[("eb16", eb), (vreg, kt), vreg + "1"], writes=[out_reg])

            state["q"].append([False, mid, [pv] + ([post_fn] if post_fn is not None else [])])

        def make_mask(tt, gi):
            kk = [kt for kt in range(gi * 4, gi * 4 + 4) if kt <= tt]
            n = len(kk)
            mb = state["mi"] % 3
            state["mi"] += 1
            for jj, kt in enumerate(kk):
                P.op("pe", lambda e, jj=jj, kt=kt: e.matmul(C.bank(6)[:, jj * 128:(jj + 1) * 128], K["Eall"][:, kt, :], selT[:, tt * 128:(tt + 1) * 128],
                                                          start=True, stop=True, skip_group_check=True),
                     reads=["Eall", ("selT", tt // 4)], writes=[("psum", 6)])
            P.op("act", lambda e: e.copy(msk[mb][:, 0:n, :], C.bank(6)[:, 0:n * 128].rearrange("p (a c) -> p a c", a=n)), reads=[("psum", 6)], writes=[("msk", mb)])
            if kk[-1] == tt:
                P.op("pool", lambda e: e.tensor_tensor(msk[mb][:, n - 1, :], msk[mb][:, n - 1, :], K["tri"], ALU.mult), reads=[("msk", mb), "tri"], writes=[("msk", mb)])
            return kk, mb

        for tt in range(NT):
            ngrp = tt // 4 + 1
            yb_b = tt % 2
            ybreg = "ybt%d" % yb_b
            ybt_b = ybt[yb_b]
            P.dma(ybt_b, A["yb"][tt * 128:(tt + 1) * 128, g * 512:(g + 1) * 512], reads=[("ybd", tt, hh) for hh in range(4)], writes=[(ybreg, hh) for hh in range(4)])
            P.dma(gbt[yb_b], A["gates"][tt * 128:(tt + 1) * 128, 1024 + g * 512:1024 + (g + 1) * 512], writes=[("gbt", yb_b)])
            nxt = make_mask(tt, 0)
            kts = [kt for kt in (tt - 2, tt - 1, tt) if kt >= 0]
            j0 = 3 - len(kts)
            for hh in range(4):
                ob = 2 + hh % 2
                pf = (lambda hh=hh, ob=ob, ybt_b=ybt_b, ybreg=ybreg, tt=tt: nsa_post(C, P, C.bank(ob), ("psum", ob), ybt_b, ybreg, ng, tt, hh, g * 4 + hh, 2, rdn, wgt))
                unit(kts, kwT, "kwT", vw, "vw", hh, tt, K["wmask"][:, j0:3, :], ["wmask"], C.bank(ob)[:, 0:130], ("psum", ob), True, True, pf)
            for gi in range(ngrp):
                kk, mb = nxt
                if gi + 1 < ngrp:
                    nxt = make_mask(tt, gi + 1)
                for hh in range(4):
                    ob = (2, 3, 5, 7)[hh]
                    lastg = (gi == ngrp - 1)
                    pf = None
                    if lastg:
                        pf = (lambda hh=hh, ob=ob, ybt_b=ybt_b, ybreg=ybreg, tt=tt: nsa_post(C, P, C.bank(ob)[:, 256:512], ("psum", ob), ybt_b, ybreg, ng, tt, hh, g * 4 + hh, 1, rdn, wgt))
                    unit(kk, ksT, "ksT", vs, "vs", hh, tt, msk[mb][:, 0:len(kk), :], [("msk", mb)], C.bank(ob)[:, 256:386], ("psum", ob), gi == 0, lastg, pf)
            def tail(tt=tt, ybt_b=ybt_b, ybreg=ybreg, yb_b=yb_b):
                P.op("pool", lambda e, gbt_b=gbt[yb_b]: e.tensor_tensor(ybt_b, ybt_b, gbt_b, ALU.mult),
                     reads=[(ybreg, hh) for hh in range(4)] + [("gbt", yb_b)], writes=[(ybreg, hh) for hh in range(4)])
                P.dma(A["yb"][tt * 128:(tt + 1) * 128, g * 512:(g + 1) * 512], ybt_b, reads=[(ybreg, hh) for hh in range(4)], writes=[("ybd", tt, hh) for hh in range(4)])

            state["q"][-1][2].append(tail)
        flush()

    for g in range(2):
        nsa_group(g)
    P.fence()
    C.release(m0)


def nsa_post(C, P, ps, psreg, ybt, ybreg, ng, tt, hh, h, br, rdn, wgt):
    c = 2 + br
    P.op("dve", lambda e: e.tensor_scalar(rdn[:, c:c + 1], ps[:, 128:129], 1e-30, None, ALU.max), reads=[psreg], writes=[("rdn", c)])
    P.op("dve", lambda e: e.reciprocal(rdn[:, c:c + 1], rdn[:, c:c + 1]), reads=[("rdn", c)], writes=[("rdn", c)])
    P.op("dve", lambda e: e.tensor_tensor(wgt[:, c:c + 1], rdn[:, c:c + 1], ng[:, tt, h * 3 + br:h * 3 + br + 1], ALU.mult), reads=[("rdn", c), "ng"], writes=[("wgt", c)])
    P.op("dve", lambda e: e.scalar_tensor_tensor(ybt[:, hh * 128:(hh + 1) * 128], ps[:, 0:128], wgt[:, c:c + 1], ybt[:, hh * 128:(hh + 1) * 128], ALU.mult, ALU.add),
         reads=[psreg, ("wgt", c), (ybreg, hh)], writes=[(ybreg, hh)])


LN_QSCALE = float(np.log(128.0 ** -0.5))


def phase_A3(C, A):
    P = C.P
    P.label = "A3set"
    P.fence()
    m0 = C.mark()
    ident = C.ident
    xT = C.xT
    U64 = C.sb([64, 64], F32)
    SU64 = C.sb([64, 64], F32)
    mS = C.sb([64, 64], F32)
    ones64 = C.sb([64, 128], F32)
    one1 = C.sb([128, 1], F32)
    epsr = C.sb([128, 1], F32)
    lnq = C.sb([128, 1], F32)
    zer1 = C.sb([128, 1], F32)
    zeros = C.sb([128, 32], F32)
    P.op("pool", lambda e: e.memset(U64, 1.0), writes=["U64"])
    P.op("pool", lambda e: e.affine_select(out=U64, in_=U64, pattern=[[1, 64]], compare_op=ALU.is_ge, fill=0.0, base=0, channel_multiplier=-1), reads=["U64"], writes=["U64"])
    P.op("pool", lambda e: e.tensor_scalar(SU64, U64, -1.0, 1.0, ALU.mult, ALU.add), reads=["U64"], writes=["SU64"])
    P.op("pool", lambda e: e.memset(mS, 1.0), writes=["mS"])
    P.op("pool", lambda e: e.affine_select(out=mS, in_=mS, pattern=[[1, 64]], compare_op=ALU.is_ge, fill=0.0, base=-1, channel_multiplier=-1), reads=["mS"], writes=["mS"])
    P.op("pool", lambda e: e.memset(ones64, 1.0), writes=["ones64"])
    P.op("pool", lambda e: e.memset(one1, 1.0), writes=["one1"])
    P.op("pool", lambda e: e.memset(epsr, RMS_EPS), writes=["epsr"])
    P.op("pool", lambda e: e.memset(lnq, LN_QSCALE), writes=["lnq"])
    P.op("pool", lambda e: e.memset(zer1, 0.0), writes=["zer1"])
    P.op("pool", lambda e: e.memset(zeros, 0.0), writes=["zeros"])
    wgb = C.sb([128, 8, 16], F32R)
    P.dma(wgb, A["w_gb"], writes=["wgb"], eng="pool")
    for n in range(32):
        for kc in range(8):
            P.op("pe", lambda e, n=n, kc=kc: e.matmul(C.bank(0)[0:64, n * 16:(n + 1) * 16], xT[:, kc, n * 64:(n + 1) * 64], wgb[:, kc, :], start=(kc == 0), stop=(kc == 7)),
                 reads=[("xT", n // 2, kc // 4), "wgb"], writes=[("psum", 0)])
    gb = C.sb([64, 32, 16], F32)
    P.op("dve", lambda e: e.tensor_copy(gb.rearrange("p n c -> p (n c)"), C.bank(0)[0:64, :]), reads=[("psum", 0)], writes=["gb"])
    beta = C.sb([64, 32, 8], F32)
    negb = C.sb([64, 32, 8], F32)
    gg = C.sb([64, 32, 8], F32)
    egc = C.sb([64, 32, 8], F32)
    ekd = C.sb([64, 32, 8], F32)
    eglb = C.sb([128, 32, 8], F32)
    dtb = C.sb([64, 8], F32)
    nea = C.sb([64, 8], F32)
    nw = C.sb([64, 128], F32)
    P.dma(dtb, A["gdn_dt_bias"].partition_broadcast(64), writes=["dtb"])
    P.dma(nea, A["gdn_a_log"].partition_broadcast(64), writes=["nea"])
    P.dma(nw, A["gdn_norm_w"].partition_broadcast(64), writes=["nw"])
    P.op("act", lambda e: e.activation(beta, gb[:, :, 0:8], AF.Sigmoid), reads=["gb"], writes=["beta"])
    P.op("dve", lambda e: e.tensor_scalar(negb, beta, -1.0, None, ALU.mult), reads=["beta"], writes=["negb"])
    P.op("act", lambda e: e.activation(nea, nea, AF.Exp), reads=["nea"], writes=["nea"])
    P.op("dve", lambda e: e.tensor_scalar(nea, nea, -1.0, None, ALU.mult), reads=["nea"], writes=["nea"])
    P.op("dve", lambda e: e.tensor_tensor(gg, gb[:, :, 8:16], dtb.unsqueeze(1).to_broadcast([64, 32, 8]), ALU.add), reads=["gb", "dtb"], writes=["gg"])
    P.op("act", lambda e: e.activation(gg, gg, AF.Exp), reads=["gg"], writes=["gg"])
    P.op("act", lambda e: e.activation(gg, gg, AF.Ln, bias=one1[0:64]), reads=["gg", "one1"], writes=["gg"])
    P.op("dve", lambda e: e.tensor_tensor(gg, gg, nea.unsqueeze(1).to_broadcast([64, 32, 8]), ALU.mult), reads=["gg", "nea"], writes=["gg"])
    ggf = gg.rearrange("p n h -> p (n h)")
    P.op("pe", lambda e: e.matmul(C.bank(1)[0:64, 0:256], U64, ggf, start=True, stop=True), reads=["U64", "gg"], writes=[("psum", 1)])
    P.op("pe", lambda e: e.matmul(C.bank(2)[0:64, 0:256], SU64, ggf, start=True, stop=True), reads=["SU64", "gg"], writes=[("psum", 2)])
    P.op("pe", lambda e: e.matmul(C.bank(3)[:, 0:256], ones64, ggf, start=True, stop=True), reads=["ones64", "gg"], writes=[("psum", 3)])
    P.op("act", lambda e: e.activation(egc.rearrange("p n h -> p (n h)"), C.bank(1)[0:64, 0:256], AF.Exp), reads=[("psum", 1)], writes=["egc"])
    P.op("act", lambda e: e.activation(ekd.rearrange("p n h -> p (n h)"), C.bank(2)[0:64, 0:256], AF.Exp), reads=[("psum", 2)], writes=["ekd"])
    P.op("act", lambda e: e.activation(eglb.rearrange("p n h -> p (n h)"), C.bank(3)[:, 0:256], AF.Exp), reads=[("psum", 3)], writes=["eglb"])
    C.dbg("gg", gg, ["gg"])
    C.dbg("beta", beta, ["beta"])
    C.dbg("egc", egc, ["egc"])
    gw = [C.sb([128, 8, 384], F32R) for _ in range(2)]
    P.dma(gw[0], A["w_gdn"][0], writes=[("gw", 0)], eng="pool")
    m1 = C.mark()

    def gdn_head(h):
        P.label = "A3S1"
        C.release(m1)
        qT = C.sb([128, S], BF16)
        kT = C.sb([128, S], BF16)
        vT = C.sb([128, S], BF16)
        qkT = (qT, kT)
        E = C.sb([64, 32, 64], F32)
        EMI = C.sb([64, 32, 64], F32)
        EMS = C.sb([64, 32, 64], F32)
        GU = EMS
        ms1 = C.mark()
        w = gw[h % 2]
        cw = C.sb([128, 3, 4], F32)
        P.dma(cw, A["gdn_cw"][h], writes=["cw"])
        dg = C.sb([128, 3, 4, 128], F32R)
        for i in range(3):
            for k in range(4):
                P.op("pool" if (i * 4 + k) % 2 else "dve", lambda e, i=i, k=k: e.tensor_scalar(dg[:, i, k, :], ident, cw[:, i, k:k + 1], None, ALU.mult),
                     reads=["ident", "cw"], writes=[("dg", i, k)])
        raw = C.sb([128, 3, 2056], F32R)
        P.op("pool", lambda e: e.tensor_copy(raw[:, :, 0:8], zeros[:, 0:24].rearrange("p (a b) -> p a b", a=3)), reads=["zeros"], writes=[("rawpad")])
        qk32 = [C.sb([128, S], F32) for _ in range(2)]
        sqr = C.sb([128, S], F32R)
        lt = [C.sb([128, 512], F32) for _ in range(2)]
        P.op("dve", lambda e: e.tensor_tensor(GU, U64.unsqueeze(1).to_broadcast([64, 32, 64]), gg[:, :, h:h + 1].to_broadcast([64, 32, 64]), ALU.mult),
             reads=["U64", "gg"], writes=["GU"])
        def emit_E():
            for q4 in range(4):
                eb_ = 6 + q4 % 2
                P.op("pe", lambda e, q4=q4, eb_=eb_: e.matmul(C.bank(eb_)[0:64, :], SU64, GU[:, q4 * 8:(q4 + 1) * 8, :].rearrange("p a b -> p (a b)"), start=True, stop=True),
                     reads=["SU64", "GU"], writes=[("psum", eb_)])
                P.op("act", lambda e, q4=q4, eb_=eb_: e.activation(E[:, q4 * 8:(q4 + 1) * 8, :].rearrange("p a b -> p (a b)"), C.bank(eb_)[0:64, :], AF.Exp), reads=[("psum", eb_)], writes=[("E", q4)])
            rdE = [("E", q4) for q4 in range(4)]
            P.op("pool", lambda e: e.tensor_tensor(EMI, E, U64.unsqueeze(1).to_broadcast([64, 32, 64]), ALU.mult), reads=rdE + ["U64"], writes=["EMI"])
            P.op("pool", lambda e: e.tensor_tensor(EMS, E, mS.unsqueeze(1).to_broadcast([64, 32, 64]), ALU.mult), reads=rdE + ["mS", "GU"], writes=["EMS", "GU"])
            P.op("dve", lambda e: e.tensor_tensor(EMS, EMS, negb[:, :, h:h + 1].to_broadcast([64, 32, 64]), ALU.mult), reads=["EMS", "negb"], writes=["EMS"])

        for i in range(3):
            for tb in range(4):
                if i == 1 and tb == 0:
                    emit_E()
                bk = tb % 2
                for kc in range(8):
                    P.op("pe", lambda e, bk=bk, kc=kc, tb=tb, i=i: e.matmul(C.bank(bk), w[:, kc, i * 128:(i + 1) * 128], xT[:, kc, tb * 512:(tb + 1) * 512],
                                                                          start=(kc == 0), stop=(kc == 7)),
                         reads=[("gw", h % 2)] + C.rd_xT_tb[tb], writes=[("psum", bk)])
                copy_op(P, C.evac_eng(), raw[:, i, 8 + tb * 512:8 + (tb + 1) * 512], C.bank(bk), reads=[("psum", bk)], writes=[("raw", i, tb)])
        if h + 1 < 8:
            P.dma(gw[(h + 1) % 2], A["w_gdn"][h + 1], writes=[("gw", (h + 1) % 2)], eng="pool")
        for i in range(3):
            for tb in range(4):
                bk = 2 + tb % 2
                for k in range(4):
                    P.op("pe", lambda e, bk=bk, k=k, tb=tb, i=i: e.matmul(C.bank(bk), dg[:, i, k, :], raw[:, i, 5 + k + tb * 512:5 + k + (tb + 1) * 512],
                                                                        start=(k == 0), stop=(k == 3)),
                         reads=[("dg", i, k), ("raw", i, tb), "rawpad"] + ([("raw", i, tb - 1)] if tb > 0 else []), writes=[("psum", bk)])
                if i < 2:
                    P.op("act", lambda e, bk=bk, tb=tb, i=i: e.activation(qk32[i][:, tb * 512:(tb + 1) * 512], C.bank(bk), AF.Silu), reads=[("psum", bk)], writes=[("qk32", i, tb)])
                else:
                    P.op("act", lambda e, bk=bk, tb=tb: e.activation(vT[:, tb * 512:(tb + 1) * 512], C.bank(bk), AF.Silu), reads=[("psum", bk)], writes=[("vT", tb)])
        for i in range(2):
            for tb in range(4):
                P.op("act", lambda e, tb=tb, i=i: e.activation(sqr[:, tb * 512:(tb + 1) * 512], qk32[i][:, tb * 512:(tb + 1) * 512], AF.Square),
                     reads=[("qk32", i, tb)], writes=[("sqr", tb)])
            for tb in range(4):
                bk = 4 + tb % 2
                b = tb % 2
                P.op("pe", lambda e, bk=bk, tb=tb: e.matmul(C.bank(bk), C.onesr, sqr[:, tb * 512:(tb + 1) * 512], start=True, stop=True),
                     reads=["onesr", ("sqr", tb)], writes=[("psum", bk)])
                P.op("act", lambda e, bk=bk, b=b: e.activation(lt[b], C.bank(bk), AF.Ln, bias=epsr), reads=[("psum", bk), "epsr"], writes=[("lt", b)])
                P.op("act", lambda e, b=b, i=i: e.activation(lt[b], lt[b], AF.Exp, bias=(lnq if i == 0 else zer1), scale=-0.5), reads=[("lt", b), "lnq", "zer1"], writes=[("lt", b)])
                P.op("dve", lambda e, b=b, tb=tb, i=i: e.tensor_tensor(qkT[i][:, tb * 512:(tb + 1) * 512], qk32[i][:, tb * 512:(tb + 1) * 512], lt[b], ALU.mult),
                     reads=[("lt", b), ("qk32", i, tb)], writes=[("qkT", i, tb)])
        if h == 0:
            C.dbg("qT0", qT, [("qkT", 0, tb) for tb in range(4)])
            C.dbg("kT0", kT, [("qkT", 1, tb) for tb in range(4)])
            C.dbg("vT0", vT, [("vT", tb) for tb in range(4)])
        C.release(ms1)
        P.label = "A3pre"
        YT = C.sb([64, 32, 2, 64], BF16)
        Xp = C.sb([64, 32, 64], BF16)
        qk_o = C.sb([64, 32, 64], F32R)
        rdq = [("qkT", 0, tb) for tb in range(4)]
        rdk = [("qkT", 1, tb) for tb in range(4)]
        rdv = [("vT", tb) for tb in range(4)]
        for n in range(32):
            P.op("pe", lambda e, n=n: e.matmul(C.bank(4 + n // 8)[0:64, (n % 8) * 64:(n % 8 + 1) * 64], kT[:, n * 64:(n + 1) * 64], kT[:, n * 64:(n + 1) * 64], start=True, stop=True),
                 reads=[("qkT", 1, n // 8)], writes=[("psum", 4 + n // 8)])
        for q4 in range(4):
            P.op("dve", lambda e, q4=q4: e.tensor_tensor(YT[:, q4 * 8:(q4 + 1) * 8, 0, :], C.bank(4 + q4)[0:64, :].rearrange("p (a b) -> p a b", a=8), EMS[:, q4 * 8:(q4 + 1) * 8, :], ALU.mult),
                 reads=[("psum", 4 + q4), "EMS"], writes=[("YT", q4)])
            P.op("pool", lambda e, q4=q4: e.tensor_copy(YT[:, q4 * 8:(q4 + 1) * 8, 1, :], ident[0:64, 0:64].unsqueeze(1).to_broadcast([64, 8, 64])),
                 reads=[("YT", q4), "ident"], writes=[("YT", q4)])
        for n in range(32):
            P.op("pe", lambda e, n=n: e.matmul(C.bank(n // 8)[0:64, (n % 8) * 64:(n % 8 + 1) * 64], kT[:, n * 64:(n + 1) * 64], qT[:, n * 64:(n + 1) * 64], start=True, stop=True),
                 reads=[("qkT", 1, n // 8), ("qkT", 0, n // 8)], writes=[("psum", n // 8)])
        for q4 in range(4):
            P.op("dve", lambda e, q4=q4: e.tensor_tensor(qk_o[:, q4 * 8:(q4 + 1) * 8, :], C.bank(q4)[0:64, :].rearrange("p (a b) -> p a b", a=8), EMI[:, q4 * 8:(q4 + 1) * 8, :], ALU.mult),
                 reads=[("psum", q4), "EMI"], writes=[("qk_o", q4)])
        P.dma(A["sc_qk"][:, :, h, :].rearrange("n m c -> m n c"), qk_o, reads=[("qk_o", q4) for q4 in range(4)], eng="pool")
        for n in range(32):
            P.op("pe", lambda e, n=n: e.transpose(C.bankb(4 + n // 8)[0:64, (n % 8) * 64:(n % 8 + 1) * 64], YT[:, n, 0, :], C.identb[0:64, 0:64]),
                 reads=[("YT", n // 8), "identb"], writes=[("psum", 4 + n // 8)])
        for q4 in range(4):
            copy_op(P, C.evac_eng(), Xp[:, q4 * 8:(q4 + 1) * 8, :], C.bankb(4 + q4)[0:64, 0:512].rearrange("p (a b) -> p a b", a=8), reads=[("psum", 4 + q4)], writes=[("Xp", q4)])
        P.label = "A3chain"
        for itn in range(6):
            last = itn == 5
            for q4 in range(4):
                a0 = 3 * (q4 % 2)
                for j in range(8):
                    n = q4 * 8 + j
                    P.op("pe", lambda e, a0=a0, j=j, n=n: e.matmul(C.bank(a0 + j // 4)[0:64, (j % 4) * 128:(j % 4 + 1) * 128], Xp[:, n, :], YT[:, n, :, :].rearrange("p a b -> p (a b)"),
                                                                 start=True, stop=True),
                         reads=[("Xp", q4), ("YT", q4)], writes=[("psum", a0 + j // 4)])
                    if not last:
                        P.op("pe", lambda e, a0=a0, j=j, n=n: e.matmul(C.bank(a0 + 2)[0:64, j * 64:(j + 1) * 64], YT[:, n, 0, :], Xp[:, n, :], start=True, stop=True),
                             reads=[("Xp", q4), ("YT", q4)], writes=[("psum", a0 + 2)])
                Av = C.bank(a0, 2)[0:64, :].rearrange("p (a b) -> p a b", a=8)
                if not last:
                    P.op("act", lambda e, q4=q4, Av=Av: e.copy(YT[:, q4 * 8:(q4 + 1) * 8, 0, :], Av[:, :, 0:64]), reads=[("psum", a0), ("psum", a0 + 1)], writes=[("YT", q4)])
                P.op("dve", lambda e, q4=q4, Av=Av: e.tensor_tensor(YT[:, q4 * 8:(q4 + 1) * 8, 1, :], Av[:, :, 64:128], YT[:, q4 * 8:(q4 + 1) * 8, 1, :], ALU.add),
                     reads=[("psum", a0), ("psum", a0 + 1), ("YT", q4)], writes=[("YT", q4)])
                if not last:
                    P.op("act", lambda e, q4=q4, a0=a0: e.copy(Xp[:, q4 * 8:(q4 + 1) * 8, :], C.bank(a0 + 2)[0:64, :].rearrange("p (a b) -> p a b", a=8)),
                         reads=[("psum", a0 + 2)], writes=[("Xp", q4)])
        P.label = "A3post"
        kg = C.sb([64, 8, 128], BF16)
        kd_o = C.sb([64, 8, 128], F32R)
        vtok = C.sb([64, 8, 128], BF16)
        bu_o = C.sb([64, 8, 128], F32)
        wT_o = C.sb([128, 8, 64], F32R)
        qd_tok = C.sb([64, 8, 128], BF16)
        qd_o = C.sb([128, 8, 64], F32R)
        for q4 in range(4):
            n0 = q4 * 8
            bc = lambda t, n0=n0: t[:, n0:n0 + 8, h:h + 1].to_broadcast([64, 8, 128])
            for j in range(8):
                n = n0 + j
                P.op("pe", lambda e, j=j, n=n: e.transpose(C.bankb(6)[0:64, j * 128:(j + 1) * 128], kT[:, n * 64:(n + 1) * 64], C.identb),
                     reads=[("qkT", 1, n // 8), "identb"], writes=[("psum", 6)])
            kv = C.bankb(6)[0:64, :].rearrange("p (a b) -> p a b", a=8)
            P.op("dve", lambda e, kv=kv, bc=bc: e.tensor_tensor(kg, kv, bc(egc), ALU.mult), reads=[("psum", 6), "egc"], writes=["kg"])
            P.op("dve", lambda e, kv=kv, bc=bc: e.tensor_tensor(kd_o, kv, bc(ekd), ALU.mult), reads=[("psum", 6), "ekd"], writes=["kd_o"])
            P.dma(A["sc_kd"][n0:n0 + 8, :, h, :].rearrange("n c d -> c n d"), kd_o, reads=["kd_o"], eng="pool")
            for j in range(8):
                n = n0 + j
                P.op("pe", lambda e, j=j, n=n: e.transpose(C.bankb(7)[0:64, j * 128:(j + 1) * 128], vT[:, n * 64:(n + 1) * 64], C.identb),
                     reads=[("vT", n // 8), "identb"], writes=[("psum", 7)])
            P.op("act", lambda e: e.copy(vtok, C.bankb(7)[0:64, :].rearrange("p (a b) -> p a b", a=8)), reads=[("psum", 7)], writes=["vtok"])
            for j in range(8):
                n = n0 + j
                P.op("pe", lambda e, j=j, n=n: e.matmul(C.bank(j // 4)[0:64, (j % 4) * 128:(j % 4 + 1) * 128], YT[:, n, 1, :], vtok[:, j, :], start=True, stop=True),
                     reads=[("YT", q4), "vtok"], writes=[("psum", j // 4)])
            uv = C.bank(0, 2)[0:64, :].rearrange("p (a b) -> p a b", a=8)
            P.op("dve", lambda e, uv=uv, bc=bc: e.tensor_tensor(bu_o, uv, bc(beta), ALU.mult), reads=[("psum", 0), ("psum", 1), "beta"], writes=["bu_o"])
            P.dma(A["sc_bu"][n0:n0 + 8, :, h, :].rearrange("n c d -> c n d"), bu_o, reads=["bu_o"])
            for j in range(8):
                n = n0 + j
                P.op("pe", lambda e, j=j, n=n: e.matmul(C.bank(2)[:, j * 64:(j + 1) * 64], kg[:, j, :], YT[:, n, 1, :], start=True, stop=True),
                     reads=[("YT", q4), "kg"], writes=[("psum", 2)])
            P.op("act", lambda e: e.copy(wT_o, C.bank(2).rearrange("p (a b) -> p a b", a=8)), reads=[("psum", 2)], writes=["wT_o"])
            P.dma(A["sc_wT"][n0:n0 + 8, :, h, :].rearrange("n p c -> p n c"), wT_o, reads=["wT_o"], eng="pool")
            for j in range(8):
                n = n0 + j
                P.op("pe", lambda e, j=j, n=n: e.transpose(C.bankb(3)[0:64, j * 128:(j + 1) * 128], qT[:, n * 64:(n + 1) * 64], C.identb),
                     reads=[("qkT", 0, n // 8), "identb"], writes=[("psum", 3)])
            qv = C.bankb(3)[0:64, :].rearrange("p (a b) -> p a b", a=8)
            P.op("dve", lambda e, qv=qv, bc=bc: e.tensor_tensor(qd_tok, qv, bc(egc), ALU.mult), reads=[("psum", 3), "egc"], writes=["qd_tok"])
            for j in range(8):
                P.op("pe", lambda e, j=j: e.transpose(C.bankb(5)[:, j * 64:(j + 1) * 64], qd_tok[:, j, :], C.identb[0:64, 0:64]),
                     reads=["qd_tok", "identb"], writes=[("psum", 5)])
            P.op("act", lambda e: e.copy(qd_o, C.bankb(5)[:, 0:512].rearrange("p (a b) -> p a b", a=8)), reads=[("psum", 5)], writes=["qd_o"])
            P.dma(A["sc_qd"][n0:n0 + 8, :, h, :].rearrange("n p c -> p n c"), qd_o, reads=["qd_o"], eng="pool")

    for h in range(8):
        gdn_head(h)

    P.label = "A3scan"
    C.release(m1)
    Sr2 = [C.sb([128, 8, 128], F32R) for _ in range(2)]
    tmp2 = C.sb([128, 8, 128], F32)
    P.op("pool", lambda e: e.memset(tmp2, 0.0), writes=["tmp2"])
    P.op("pool", lambda e: e.tensor_copy(Sr2[0], tmp2), reads=["tmp2"], writes=[("Sr", 0)])
    NB = 3
    bu = [C.sb([64, 8, 128], F32) for _ in range(NB)]
    wT = [C.sb([128, 8, 64], F32R) for _ in range(NB)]
    qd = [C.sb([128, 8, 64], F32R) for _ in range(NB)]
    qk = [C.sb([64, 8, 64], F32R) for _ in range(NB)]
    kd = [C.sb([64, 8, 128], F32R) for _ in range(NB)]
    zsb = [C.sb([64, 1024], F32) for _ in range(3)]
    ybb = [C.sb([64, 1024], F32) for _ in range(3)]
    on2 = [C.sb([64, 8, 128], F32) for _ in range(2)]
    tmp = C.sb([64, 8, 128], F32)
    vn = C.sb([64, 8, 128], F32R)
    sq = C.sb([64, 8, 128], F32)
    on = C.sb([64, 8, 128], F32)
    ssum = C.sb([64, 8], F32)

    def loads(n):
        b = n % NB
        P.dma(wT[b].bitcast(F32), A["sc_wT"][n], writes=[("wT", b)])
        P.dma(bu[b], A["sc_bu"][n], writes=[("bu", b)])
        P.dma(kd[b].bitcast(F32), A["sc_kd"][n], writes=[("kd", b)])
        P.dma(qd[b].bitcast(F32), A["sc_qd"][n], writes=[("qd", b)])
        P.dma(qk[b].bitcast(F32), A["sc_qk"][n], writes=[("qk", b)])

    def loads_post(n):
        b3 = n % 3
        P.dma(zsb[b3], A["zs"][n * 64:(n + 1) * 64, :], writes=[("zsb", b3)], eng="pool")
        P.dma(ybb[b3], A["yb"][n * 64:(n + 1) * 64, :], writes=[("ybb", b3)], eng="pool")

    def crit(n):
        b = n % NB
        b2 = n % 2
        Sc = Sr2[n % 2]
        Sn = Sr2[(n + 1) % 2]
        rSc = ("Sr", n % 2)
        rSn = ("Sr", (n + 1) % 2)
        for h in range(8):
            P.op("pe", lambda e, h=h: e.matmul(C.bank(h // 4)[0:64, (h % 4) * 128:(h % 4 + 1) * 128], wT[b][:, h, :], Sc[:, h, :], start=True, stop=True),
                 reads=[("wT", b), rSc], writes=[("psum", h // 4)])
        for h in range(8):
            P.op("pe", lambda e, h=h: e.matmul(C.bank(2 + h // 4)[0:64, (h % 4) * 128:(h % 4 + 1) * 128], qd[b][:, h, :], Sc[:, h, :], start=(h % 4 == 0), stop=False, skip_group_check=True),
                 reads=[("qd", b), rSc], writes=[("psum", 2 + h // 4)])
        p1 = C.bank(0, 2)[0:64, :].rearrange("p (a b) -> p a b", a=8)
        P.op("dve", lambda e: e.tensor_tensor(tmp, p1, negb[:, n, :].unsqueeze(2).to_broadcast([64, 8, 128]), ALU.mult), reads=[("psum", 0), ("psum", 1), "negb"], writes=["tmp"])
        P.op("dve", lambda e: e.tensor_tensor(vn, tmp, bu[b], ALU.add), reads=["tmp", ("bu", b)], writes=["vn"])
        for h in range(8):
            P.op("pe", lambda e, h=h: e.matmul(C.bank(4 + h // 4)[:, (h % 4) * 128:(h % 4 + 1) * 128], kd[b][:, h, :], vn[:, h, :], start=True, stop=True),
                 reads=[("kd", b), "vn"], writes=[("psum", 4 + h // 4)])
        for h in range(8):
            P.op("pe", lambda e, h=h: e.matmul(C.bank(2 + h // 4)[0:64, (h % 4) * 128:(h % 4 + 1) * 128], qk[b][:, h, :], vn[:, h, :], start=False, stop=True, skip_group_check=True),
                 reads=[("qk", b), "vn"], writes=[("psum", 2 + h // 4)])
        if n < 31:
            P.op("dve", lambda e: e.tensor_tensor(Sn, C.bank(4, 2).rearrange("p (a b) -> p a b", a=8), tmp2, ALU.add), reads=[("psum", 4), ("psum", 5), "tmp2"], writes=[rSn])
        if n < 30:
            P.op("dve", lambda e: e.tensor_tensor(tmp2, Sn.bitcast(F32), eglb[:, n + 1, :].unsqueeze(2).to_broadcast([128, 8, 128]), ALU.mult), reads=[rSn, "eglb"], writes=["tmp2"])
        ov_ = C.bank(2, 2)[0:64, :].rearrange("p (a b) -> p a b", a=8)
        P.op("act", lambda e: e.copy(on2[b2], ov_), reads=[("psum", 2), ("psum", 3)], writes=[("on2", b2)])

    def post(n):
        b2 = n % 2
        b3 = n % 3
        P.op("act", lambda e: e.activation(sq, on2[b2], AF.Square), reads=[("on2", b2)], writes=["sq"])
        P.op("dve", lambda e: e.tensor_reduce(ssum, sq, AX.X, ALU.add), reads=["sq"], writes=["ssum"])
        P.op("act", lambda e: e.activation(ssum, ssum, AF.Sqrt, bias=epsr[0:64], scale=1.0 / 128.0), reads=["ssum", "epsr"], writes=["ssum"])
        P.op("dve", lambda e: e.reciprocal(ssum, ssum), reads=["ssum"], writes=["ssum"])
        for h in range(8):
            P.op("act", lambda e, h=h: e.activation(on[:, h, :], on2[b2][:, h, :], AF.Identity, scale=ssum[:, h:h + 1]), reads=[("on2", b2), "ssum"], writes=["on"])
        onf = on.rearrange("p a b -> p (a b)")
        P.op("pool", lambda e: e.tensor_tensor(onf, onf, zsb[b3], ALU.mult), reads=["on", ("zsb", b3)], writes=["on"])
        P.op("pool", lambda e: e.tensor_tensor(ybb[b3], ybb[b3], onf, ALU.add), reads=[("ybb", b3), "on"], writes=[("ybb", b3)])
        P.dma(A["merged"][n * 64:(n + 1) * 64, :], ybb[b3], reads=[("ybb", b3)], eng="pool")

    loads(0)
    loads(1)
    loads_post(0)
    for n in range(32):
        crit(n)
        if n >= 1:
            post(n - 1)
        if n + 1 < 32:
            loads_post(n + 1)
        if n + 2 < 32:
            loads(n + 2)
    post(31)
    C.release(m0)


def kmaj(w):
    K, N = w.shape
    return np.ascontiguousarray(w.reshape(K // 128, 128, N).transpose(1, 0, 2))


IN_OFF = dict(g_q=0, g_k=1024, g_v=2048, g_z=3072, g_b=4096, g_a=4104, n_q=4112, c_k=5136, c_v=5392, s_k=5648, s_v=5904,
              w_k=6160, w_v=6416, n_g=6672, m_g=6696)

WEIGHT_SPECS = {
    "w_gdn": ([8, 128, 8, 384], F32), "w_gb": ([128, 8, 16], F32), "gdn_cw": ([8, 128, 3, 4], F32),
    "gdn_a_log": ([8], F32), "gdn_dt_bias": ([8], F32), "gdn_norm_w": ([128], F32),
    "inv_freq": ([128, 1], F32),
    "w_mga": ([128, 8, 1024], F32), "w_mgb": ([128, 8, 1024], F32), "w_z": ([128, 8, 1024], F32), "w_ng": ([128, 8, 24], F32),
    "w_nsa_c": ([2, 128, 8, 256], F32), "w_nsa_k": ([2, 128, 8, 256], F32), "w_nsa_v": ([2, 128, 8, 256], F32), "w_nsa_q": ([2, 128, 8, 512], F32),
    "cmp_pe": ([2, 32, 128], F32), "cmp_w1": ([2, 128, 32, 256], F32), "cmp_b1": ([128, 2, 2], F32), "cmp_w2": ([2, 256, 128], F32),
    "w_out": ([128, 8, 1024], F32),
    "ln1_g": ([1024], F32), "ln1_b": ([1024], F32),
    "xa_wq": ([128, 8, 1024], F32), "xa_wk": ([128, 8, 1024], F32), "xa_wv": ([128, 8, 1024], F32), "xa_wo": ([128, 8, 1024], F32),
    "ln2_g": ([1024], F32), "ln2_b": ([1024], F32),
    "moe_wr": ([128, 8, 36], F32), "moe_br": ([36], F32),
    "moe_wg": ([32, 128, 8, 256], F32), "moe_wu": ([32, 128, 8, 256], F32), "moe_wd": ([32, 128, 2, 1024], F32),
    "ln3_g": ([1024], F32), "ln3_b": ([1024], F32),
}


def prep_weights(inp):
    l = 0
    w = {}
    wi = inp["w_in"][l]
    O = IN_OFF
    w["inv_freq"] = np.tile((10000.0 ** (-np.arange(64, dtype=np.float32) / np.float32(64))).astype(np.float32), 2).reshape(128, 1)
    w["w_gdn"] = np.stack([kmaj(np.concatenate([wi[:, O[a] + h * 128:O[a] + (h + 1) * 128] for a in ("g_q", "g_k", "g_v")], axis=1)) for h in range(8)])
    w["w_gb"] = kmaj(wi[:, O["g_b"]:O["g_b"] + 16])
    w["gdn_cw"] = np.ascontiguousarray(inp["gdn_conv_w"][l].reshape(4, 3, 8, 128).transpose(2, 3, 1, 0))
    w["gdn_a_log"] = np.ascontiguousarray(inp["gdn_a_log"][l])
    w["gdn_dt_bias"] = np.ascontiguousarray(inp["gdn_dt_bias"][l])
    w["gdn_norm_w"] = np.ascontiguousarray(inp["gdn_norm_w"][l])
    w["w_mga"] = kmaj(wi[:, O["m_g"]:O["m_g"] + 1024])
    w["w_mgb"] = kmaj(wi[:, O["m_g"] + 1024:O["m_g"] + 2048])
    w["w_z"] = kmaj(wi[:, O["g_z"]:O["g_z"] + 1024])
    w["w_ng"] = kmaj(wi[:, O["n_g"]:O["n_g"] + 24])
    def grp(a, b):
        return np.stack([kmaj(np.concatenate([wi[:, O[a] + g * 128:O[a] + (g + 1) * 128], wi[:, O[b] + g * 128:O[b] + (g + 1) * 128]], axis=1)) for g in range(2)])
    w["w_nsa_c"] = grp("c_k", "c_v")
    w["w_nsa_k"] = grp("s_k", "w_k")
    w["w_nsa_v"] = grp("s_v", "w_v")
    w["w_nsa_q"] = np.stack([kmaj(wi[:, O["n_q"] + g * 512:O["n_q"] + (g + 1) * 512]) for g in range(2)])
    w["cmp_pe"] = np.ascontiguousarray(inp["cmp_pe"][l])
    w["cmp_w1"] = np.ascontiguousarray(inp["cmp_w1"][l].reshape(2, 32, 128, 256).transpose(0, 2, 1, 3))
    w["cmp_b1"] = np.ascontiguousarray(inp["cmp_b1"][l].reshape(2, 2, 128).transpose(2, 0, 1))
    w["cmp_w2"] = np.ascontiguousarray(inp["cmp_w2"][l])
    w["w_out"] = kmaj(inp["w_out"][l])
    for k in ("ln1_g", "ln1_b", "ln2_g", "ln2_b", "ln3_g", "ln3_b"):
        w[k] = np.ascontiguousarray(inp[k][l])
    w["xa_wq"] = kmaj(inp["xa_wq"][l])
    w["xa_wk"] = kmaj(inp["xa_wkv"][l][:, :1024])
    w["xa_wv"] = kmaj(inp["xa_wkv"][l][:, 1024:])
    w["xa_wo"] = kmaj(inp["xa_wo"][l])
    w["moe_wr"] = kmaj(np.concatenate([inp["moe_w_group"][l], inp["moe_w_expert"][l]], axis=1))
    w["moe_br"] = np.concatenate([inp["moe_b_group"][l], inp["moe_b_expert"][l]], axis=0)
    w["moe_wg"] = np.ascontiguousarray(inp["moe_w_gate"][l].reshape(32, 8, 128, 256).transpose(0, 2, 1, 3))
    w["moe_wu"] = np.ascontiguousarray(inp["moe_w_up"][l].reshape(32, 8, 128, 256).transpose(0, 2, 1, 3))
    w["moe_wd"] = np.ascontiguousarray(inp["moe_w_down"][l].reshape(32, 2, 128, 1024).transpose(0, 2, 1, 3))
    return w


def build(phases="BCD", ext=()):
    nc = bass.Bass("TRN2", target_bir_lowering=False)
    A = {}
    A["x"] = nc.dram_tensor("x", [S, D], F32, kind="ExternalInput").ap()
    A["mem"] = nc.dram_tensor("mem", [256, D], F32, kind="ExternalInput").ap()
    A["positions"] = nc.dram_tensor("positions", [S], I32, kind="ExternalInput").ap()
    for k, (shape, dt) in WEIGHT_SPECS.items():
        A[k] = nc.dram_tensor(k, shape, dt, kind="ExternalInput").ap()
    A["out"] = nc.dram_tensor("out", [S, D], F32, kind="ExternalOutput").ap()
    for k in ("merged", "h1", "h2", "yb", "zs", "gates"):
        kind = "ExternalInput" if k in ext else ("ExternalOutput" if ("dbg" in ext) else "Internal")
        A[k] = nc.dram_tensor(k, [S, 2 * D if k == "gates" else D], F32, kind=kind).ap()
    A["sc_bu"] = nc.dram_tensor("sc_bu", [32, 64, 8, 128], F32, kind="Internal").ap()
    A["sc_kd"] = nc.dram_tensor("sc_kd", [32, 64, 8, 128], F32, kind="Internal").ap()
    A["sc_wT"] = nc.dram_tensor("sc_wT", [32, 128, 8, 64], F32, kind="Internal").ap()
    A["sc_qd"] = nc.dram_tensor("sc_qd", [32, 128, 8, 64], F32, kind="Internal").ap()
    A["sc_qk"] = nc.dram_tensor("sc_qk", [32, 64, 8, 64], F32, kind="Internal").ap()
    C = Ctx(nc)
    C.debug = "dbg" in ext
    build_consts(C)
    if "A" in phases or "N" in phases or "G" in phases:
        phase_A0(C, A)
    if "A" in phases or "1" in phases:
        phase_A1(C, A)
    if "A" in phases or "N" in phases:
        phase_A2(C, A)
    if "A" in phases or "G" in phases:
        phase_A3(C, A)
    if "A" in phases or "N" in phases or "G" in phases:
        C.release(C.mA0)
    if "B" in phases:
        phase_B(C, A)
    if "C" in phases:
        phase_C(C, A)
    if "D" in phases:
        phase_D(C, A)
    global LAST_PROG, LAST_CTX
    LAST_PROG = C.P
    LAST_CTX = C
    C.P.emit()
    return nc


def kernel(**inputs):
    inputs = {k: np.asarray(v) for k, v in inputs.items()}
    w = prep_weights(inputs)
    nc = build(phases="ABCD", ext=())
    in_maps = []
    for b in range(8):
        m = dict(w)
        m["x"] = np.ascontiguousarray(inputs["x"][b], dtype=np.float32)
        m["mem"] = np.ascontiguousarray(inputs["mem"][b], dtype=np.float32)
        m["positions"] = np.ascontiguousarray(inputs["positions"][b]).astype(np.int32)
        in_maps.append(m)
    res = run_bass_kernel_spmd(nc, in_maps, core_ids=list(range(8)))
    return np.stack([np.asarray(r["out"]) for r in res.results]).astype(np.float32)
```
